# Optimizing a Trainium2 kernel written in Bass

```python
import math
import jax, jax.numpy as jnp
from jax import lax
import numpy as np

D_MODEL = 1024
BATCH = 16
SEQ = 2048
DEPTH = 2

MIX_WIDTH = D_MODEL // 2
HY_WIDTH = MIX_WIDTH
HY_ORDER = 2
HY_POS_BANDS = 8
HY_EMB = 1 + 2 * HY_POS_BANDS
HY_FILTER_HIDDEN = 64
HY_DECAY_TARGET = 1e-2
HY_FAST_DECAY = 0.3
HY_SLOW_DECAY = 1.5
HY_MOD_SHIFT = 0.05
HEAD_DIM = 64
N_Q_HEADS = MIX_WIDTH // HEAD_DIM
N_KV_HEADS = N_Q_HEADS // 4
KV_WIDTH = N_KV_HEADS * HEAD_DIM
WINDOW = 128
BLOCK = 128
ROPE_THETA = 10000.0
GM_WIDTH = MIX_WIDTH
GM_GROUPS = 4
GM_CHUNK = 128
GM_GROUP_CH = GM_WIDTH // GM_GROUPS
N_BRANCH = 3
N_EXPERTS = 16
EC_CAPACITY = 2
D_EXPERT = 2 * D_MODEL
EPS = 1e-6
NEG_INF = -1e30

IN_SIZES = (3 * HY_WIDTH, N_Q_HEADS * HEAD_DIM, KV_WIDTH, KV_WIDTH, GM_WIDTH, GM_WIDTH, N_BRANCH * D_MODEL)
IN_COLS = 3 * HY_WIDTH + N_Q_HEADS * HEAD_DIM + 2 * KV_WIDTH + 2 * GM_WIDTH + N_BRANCH * D_MODEL
IN_SPLITS = (
    3 * HY_WIDTH,
    3 * HY_WIDTH + N_Q_HEADS * HEAD_DIM,
    3 * HY_WIDTH + N_Q_HEADS * HEAD_DIM + KV_WIDTH,
    3 * HY_WIDTH + N_Q_HEADS * HEAD_DIM + 2 * KV_WIDTH,
    3 * HY_WIDTH + N_Q_HEADS * HEAD_DIM + 2 * KV_WIDTH + GM_WIDTH,
    3 * HY_WIDTH + N_Q_HEADS * HEAD_DIM + 2 * KV_WIDTH + 2 * GM_WIDTH,
)

kernel_name = "hybrid_hyena_swa_gmlp_ec_block"


def _rmsnorm(x, g):
    xf = x.astype(jnp.float32)
    y = xf * lax.rsqrt(jnp.mean(xf * xf, axis=-1, keepdims=True) + EPS) * g.astype(jnp.float32)
    return y.astype(x.dtype)


def _layernorm(x, g, b):
    xf = x.astype(jnp.float32)
    mu = jnp.mean(xf, axis=-1, keepdims=True)
    var = jnp.mean(jnp.square(xf - mu), axis=-1, keepdims=True)
    y = (xf - mu) * lax.rsqrt(var + EPS) * g.astype(jnp.float32) + b.astype(jnp.float32)
    return y.astype(x.dtype)


def _short_conv(z, w, b):
    zp = jnp.pad(z, ((0, 0), (1, 1), (0, 0)))
    return zp[:, :-2] * w[0] + zp[:, 1:-1] * w[1] + zp[:, 2:] * w[2] + b


def _hyena_filters(L, f1_w, f1_b, f1_freq, f2_w, f2_b, f2_freq, f3_w):
    f32 = jnp.float32
    pos = jnp.arange(L, dtype=f32)
    t = jnp.linspace(0.0, 1.0, L, dtype=f32)
    w = 2.0 * math.pi * pos / L
    bands = jnp.linspace(1e-4, HY_POS_BANDS - 1, HY_POS_BANDS, dtype=f32)
    ang = w[:, None] * bands[None, :]
    feats = jnp.concatenate([t[:, None], jnp.cos(ang), -jnp.sin(ang)], axis=-1)
    a = jnp.sin(f1_freq.astype(f32) * (feats @ f1_w.astype(f32) + f1_b.astype(f32)))
    a = jnp.sin(f2_freq.astype(f32) * (a @ f2_w.astype(f32) + f2_b.astype(f32)))
    h = (a @ f3_w.astype(f32)).reshape(L, 2, HY_ORDER, HY_WIDTH)
    deltas = jnp.abs(jnp.linspace(math.log(HY_DECAY_TARGET) / HY_FAST_DECAY,
                                  math.log(HY_DECAY_TARGET) / HY_SLOW_DECAY, HY_WIDTH, dtype=f32))
    window = jnp.exp(-t[:, None] * deltas[None, :]) + HY_MOD_SHIFT
    h = h * window[:, None, None, :]
    k = jnp.concatenate([h[:, 0], jnp.zeros((1, HY_ORDER, HY_WIDTH), f32), h[:0:-1, 1]], axis=0)
    k = k / jnp.sum(jnp.abs(k), axis=0, keepdims=True)
    return jnp.fft.rfft(k, axis=0)


def _fft_conv(u, kf, bias):
    L = u.shape[1]
    uf = u.astype(jnp.float32)
    y = jnp.fft.irfft(jnp.fft.rfft(uf, n=2 * L, axis=1) * kf[None], n=2 * L, axis=1)[:, :L]
    return (y + uf * bias.astype(jnp.float32)).astype(u.dtype)


def _hyena(z, conv_w, conv_b, f1_w, f1_b, f1_freq, f2_w, f2_b, f2_freq, f3_w, bias):
    z = _short_conv(z, conv_w, conv_b)
    v, x1, x2 = jnp.split(z, 3, axis=-1)
    kf = _hyena_filters(z.shape[1], f1_w, f1_b, f1_freq, f2_w, f2_b, f2_freq, f3_w)
    y = x1 * _fft_conv(v, kf[:, 0], bias[0])
    y = x2 * _fft_conv(y, kf[:, 1], bias[1])
    return y


def _rotary(x, cos, sin):
    x1, x2 = jnp.split(x, 2, axis=-1)
    c = cos[None, :, None, :]
    s = sin[None, :, None, :]
    return jnp.concatenate([x1 * c - x2 * s, x2 * c + x1 * s], axis=-1)


def _window_attention(q, k, v, sink):
    B, S, H, hd = q.shape
    nb = S // BLOCK
    G = H // N_KV_HEADS
    qb = q.reshape(B, nb, BLOCK, N_KV_HEADS, G, hd)

    def band(t):
        tp = jnp.pad(t, ((0, 0), (BLOCK, BLOCK), (0, 0), (0, 0))).reshape(B, nb + 2, BLOCK, N_KV_HEADS, hd)
        return jnp.concatenate([tp[:, :-2], tp[:, 1:-1], tp[:, 2:]], axis=2)

    kb = band(k)
    vb = band(v)
    s = jnp.einsum('bnqkgd,bnskd->bnkgqs', qb, kb).astype(jnp.float32) * (hd ** -0.5)
    blk = jnp.arange(nb)[:, None, None] * BLOCK
    qpos = blk + jnp.arange(BLOCK)[None, :, None]
    kpos = blk - BLOCK + jnp.arange(3 * BLOCK)[None, None, :]
    valid = (jnp.abs(kpos - qpos) <= WINDOW) & (kpos >= 0) & (kpos < S)
    s = jnp.where(valid[None, :, None, None], s, NEG_INF)
    sink_l = jnp.broadcast_to(sink.astype(jnp.float32).reshape(N_KV_HEADS, G)[None, None, :, :, None, None],
                              s.shape[:-1] + (1,))
    p = jax.nn.softmax(jnp.concatenate([s, sink_l], axis=-1), axis=-1)[..., :-1]
    o = jnp.einsum('bnkgqs,bnskd->bnqkgd', p.astype(v.dtype), vb)
    return o.reshape(B, S, H * hd)


def _chunk_sgu(zu, zv, ln_g, ln_b, ws, b):
    B, S, _ = zu.shape
    nc = S // GM_CHUNK
    u = jax.nn.gelu(zu, approximate=False)
    v = _layernorm(jax.nn.gelu(zv, approximate=False), ln_g, ln_b)
    vc = v.reshape(B, nc, GM_CHUNK, GM_GROUPS, GM_GROUP_CH)
    sv = jnp.einsum('gpq,bnqgc->bnpgc', ws, vc) + b.T[None, None, :, :, None]
    return u * sv.reshape(B, S, GM_WIDTH)


def _expert_choice_ffn(h, w_router, w_gate, w_up, w_down):
    B, S, D = h.shape
    cap = EC_CAPACITY * S // N_EXPERTS
    aff = jax.nn.softmax((h @ w_router).astype(jnp.float32), axis=-1)
    gate, idx = lax.top_k(jnp.swapaxes(aff, 1, 2), cap)
    xg = jax.vmap(lambda hb, ib: hb[ib])(h, idx)
    a = jnp.einsum('becd,edf->becf', xg, w_gate)
    u = jnp.einsum('becd,edf->becf', xg, w_up)
    y = jnp.einsum('becf,efd->becd', jax.nn.silu(a) * u, w_down) * gate[..., None].astype(h.dtype)
    seg = (jnp.arange(B)[:, None, None] * S + idx).reshape(-1)
    out = jax.ops.segment_sum(y.reshape(-1, D), seg, num_segments=B * S)
    return out.reshape(B, S, D)


def _normal(k, shape, scale):
    return jax.random.normal(k, shape, jnp.float32) * scale


def setup_inputs(seed: int = 0) -> dict:
    key = jax.random.key(seed)
    ks = jax.random.split(key, 32)
    L = DEPTH
    return {
        "x": _normal(ks[0], (BATCH, SEQ, D_MODEL), 1.0),
        "c": _normal(ks[1], (BATCH, D_MODEL), 1.0),
        "w_mod": _normal(ks[2], (L, D_MODEL, 6 * D_MODEL), D_MODEL ** -0.5),
        "b_mod": _normal(ks[3], (L, 6 * D_MODEL), 0.02),
        "norm1_g": 1.0 + _normal(ks[4], (L, D_MODEL), 0.02),
        "norm2_g": 1.0 + _normal(ks[5], (L, D_MODEL), 0.02),
        "w_in": _normal(ks[6], (L, D_MODEL, IN_COLS), D_MODEL ** -0.5),
        "hy_conv_w": _normal(ks[7], (L, 3, 3 * HY_WIDTH), 3 ** -0.5),
        "hy_conv_b": _normal(ks[8], (L, 3 * HY_WIDTH), 0.02),
        "hy_f1_w": _normal(ks[9], (L, HY_EMB, HY_FILTER_HIDDEN), HY_EMB ** -0.5),
        "hy_f1_b": _normal(ks[10], (L, HY_FILTER_HIDDEN), 0.1),
        "hy_f1_freq": 1.0 + _normal(ks[11], (L, HY_FILTER_HIDDEN), 0.02),
        "hy_f2_w": _normal(ks[12], (L, HY_FILTER_HIDDEN, HY_FILTER_HIDDEN), HY_FILTER_HIDDEN ** -0.5),
        "hy_f2_b": _normal(ks[13], (L, HY_FILTER_HIDDEN), 0.1),
        "hy_f2_freq": 1.0 + _normal(ks[14], (L, HY_FILTER_HIDDEN), 0.02),
        "hy_f3_w": _normal(ks[15], (L, HY_FILTER_HIDDEN, 2 * HY_ORDER * HY_WIDTH), HY_FILTER_HIDDEN ** -0.5),
        "hy_bias": _normal(ks[16], (L, HY_ORDER, HY_WIDTH), 0.5),
        "q_norm_g": 1.0 + _normal(ks[17], (L, HEAD_DIM), 0.02),
        "k_norm_g": 1.0 + _normal(ks[18], (L, HEAD_DIM), 0.02),
        "attn_sink": _normal(ks[19], (L, N_Q_HEADS), 0.5),
        "gm_ln_g": 1.0 + _normal(ks[20], (L, GM_WIDTH), 0.02),
        "gm_ln_b": _normal(ks[21], (L, GM_WIDTH), 0.02),
        "gm_ws": _normal(ks[22], (L, GM_GROUPS, GM_CHUNK, GM_CHUNK), GM_CHUNK ** -0.5),
        "gm_b": 1.0 + _normal(ks[23], (L, GM_GROUPS, GM_CHUNK), 0.02),
        "w_branch": _normal(ks[24], (L, N_BRANCH, MIX_WIDTH, D_MODEL), MIX_WIDTH ** -0.5),
        "w_out": _normal(ks[25], (L, D_MODEL, D_MODEL), D_MODEL ** -0.5),
        "w_router": _normal(ks[26], (L, D_MODEL, N_EXPERTS), D_MODEL ** -0.5),
        "w_e_gate": _normal(ks[27], (L, N_EXPERTS, D_MODEL, D_EXPERT), D_MODEL ** -0.5),
        "w_e_up": _normal(ks[28], (L, N_EXPERTS, D_MODEL, D_EXPERT), D_MODEL ** -0.5),
        "w_e_down": _normal(ks[29], (L, N_EXPERTS, D_EXPERT, D_MODEL), D_EXPERT ** -0.5),
    }


def reference(x, c, w_mod, b_mod, norm1_g, norm2_g, w_in, hy_conv_w, hy_conv_b, hy_f1_w, hy_f1_b,
              hy_f1_freq, hy_f2_w, hy_f2_b, hy_f2_freq, hy_f3_w, hy_bias, q_norm_g, k_norm_g, attn_sink,
              gm_ln_g, gm_ln_b, gm_ws, gm_b, w_branch, w_out, w_router, w_e_gate, w_e_up, w_e_down):
    B, S, _ = x.shape
    pos = jnp.arange(S, dtype=jnp.float32)
    inv = ROPE_THETA ** (-jnp.arange(0, HEAD_DIM, 2, dtype=jnp.float32) / HEAD_DIM)
    ang = pos[:, None] * inv[None, :]
    cos = jnp.cos(ang).astype(x.dtype)
    sin = jnp.sin(ang).astype(x.dtype)
    cond = jax.nn.silu(c)
    for l in range(DEPTH):
        mod = cond @ w_mod[l] + b_mod[l]
        sh1, sc1, gt1, sh2, sc2, gt2 = [m[:, None, :] for m in jnp.split(mod, 6, axis=-1)]
        h = _rmsnorm(x, norm1_g[l]) * (1 + sc1) + sh1
        z = h @ w_in[l]
        z_hy, z_q, z_k, z_v, z_gu, z_gv, z_gate = jnp.split(z, IN_SPLITS, axis=-1)
        y_hy = _hyena(z_hy, hy_conv_w[l], hy_conv_b[l], hy_f1_w[l], hy_f1_b[l], hy_f1_freq[l],
                      hy_f2_w[l], hy_f2_b[l], hy_f2_freq[l], hy_f3_w[l], hy_bias[l])
        q = _rotary(_rmsnorm(z_q.reshape(B, S, N_Q_HEADS, HEAD_DIM), q_norm_g[l]), cos, sin)
        k = _rotary(_rmsnorm(z_k.reshape(B, S, N_KV_HEADS, HEAD_DIM), k_norm_g[l]), cos, sin)
        y_at = _window_attention(q, k, z_v.reshape(B, S, N_KV_HEADS, HEAD_DIM), attn_sink[l])
        y_gm = _chunk_sgu(z_gu, z_gv, gm_ln_g[l], gm_ln_b[l], gm_ws[l], gm_b[l])
        ys = jnp.stack([y_hy, y_at, y_gm], axis=2)
        br = jnp.einsum('bsiw,iwd->bsid', ys, w_branch[l])
        gates = jax.nn.sigmoid(z_gate.reshape(B, S, N_BRANCH, D_MODEL))
        mix = jnp.sum(gates * br, axis=2) @ w_out[l]
        x = x + gt1 * mix
        h = _rmsnorm(x, norm2_g[l]) * (1 + sc2) + sh2
        x = x + gt2 * _expert_choice_ffn(h, w_router[l], w_e_gate[l], w_e_up[l], w_e_down[l])
    return x
```

```python
import math
import contextlib
import numpy as np
import ml_dtypes
import concourse.bass as bass
import concourse.mybir as mybir
from concourse.bass_utils import run_bass_kernel_spmd

F32 = mybir.dt.float32
BF16 = mybir.dt.bfloat16
I32 = mybir.dt.int32
ALU = mybir.AluOpType
AF = mybir.ActivationFunctionType
AX = mybir.AxisListType

T = 2048
D = 1024
NB = 2
NL = 2
NE = 16
CAP = 256
DE = 2048
COL_V, COL_X1, COL_X2, COL_Q, COL_K, COL_GU, COL_GV, COL_GATE = 0, 512, 1024, 1536, 2048, 2304, 2816, 3328
EPS = 1e-6


class Sched:
    NDMASEM = 8

    def __init__(self, nc):
        self.nc = nc
        self.ops = []
        self.st = {}
        self._cap = None

    def _acc(self, key, write, i, deps):
        if isinstance(key, tuple):
            base, sub = key
        else:
            base, sub = key, None
        b = self.st.get(base)
        if b is None:
            b = self.st[base] = {'w': set(), 'r': set(), 'subs': {}}
        if sub is None:
            deps |= b['w']
            for s in b['subs'].values():
                deps |= s['w']
                if write:
                    deps |= s['r']
            if write:
                deps |= b['r']
                b['w'] = {i}
                b['r'] = set()
                b['subs'] = {}
            else:
                b['r'].add(i)
        else:
            s = b['subs'].get(sub)
            if s is None:
                s = b['subs'][sub] = {'w': set(), 'r': set()}
            deps |= b['w']
            deps |= s['w']
            if write:
                deps |= b['r']
                deps |= s['r']
                s['w'] = {i}
                s['r'] = set()
            else:
                s['r'].add(i)

    def fence(self, base):
        assert self._cap is None
        b = self.st.get(base)
        if b is None:
            return
        w = set(b['w']) | set(b['r'])
        for s in b['subs'].values():
            w |= s['w']
            w |= s['r']
        b['w'] = w
        b['r'] = set()
        b['subs'] = {}

    def interleave(self, thunks, group, name=''):
        import os
        on = os.environ.get('IL_ON')
        if on is not None and name not in on.split(','):
            group = 1
        for g0 in range(0, len(thunks), group):
            chains = []
            for th in thunks[g0:g0 + group]:
                self._cap = []
                th()
                chains.append(self._cap)
                self._cap = None
            pos = [0] * len(chains)
            left = sum(len(c) for c in chains)
            while left:
                for ci, c in enumerate(chains):
                    if pos[ci] < len(c):
                        self.add(*c[pos[ci]])
                        pos[ci] += 1
                        left -= 1

    def add(self, eng, fn, reads=(), writes=(), dma=False):
        if self._cap is not None:
            self._cap.append((eng, fn, tuple(reads), tuple(writes), dma))
            return None
        i = len(self.ops)
        deps = set()
        for k in reads:
            kb = k[0] if isinstance(k, tuple) else k
            self._acc(k, isinstance(kb, str) and len(kb) >= 2 and kb[0] == 'P' and (kb[1].isdigit() or kb[1] == 'T'), i, deps)
        for k in writes:
            self._acc(k, True, i, deps)
        deps.discard(i)
        self.ops.append(dict(eng=eng, fn=fn, deps=deps, dma=dma))
        return i

    def pe(self, fn, r=(), w=()):
        return self.add('pe', fn, r, w)

    def act(self, fn, r=(), w=()):
        return self.add('act', fn, r, w)

    def dve(self, fn, r=(), w=()):
        return self.add('dve', fn, r, w)

    def pool(self, fn, r=(), w=()):
        return self.add('pool', fn, r, w)

    def dma(self, q, fn, r=(), w=()):
        return self.add(q, fn, r, w, dma=True)

    def last_writers(self, key):
        deps = set()
        self._acc(key, False, -1, deps)
        return deps

    def emit(self, final_keys=()):
        nc = self.nc
        ops = self.ops
        n = len(ops)
        needed = [False] * n
        for i, o in enumerate(ops):
            for d in o['deps']:
                od = ops[d]
                if od['eng'] == 'pe' and o['eng'] == 'pe' and not o['dma'] and not od['dma']:
                    continue
                needed[d] = True
        finals = set()
        for k in final_keys:
            finals |= self.last_writers(k)
        finals.discard(-1)
        for d in finals:
            needed[d] = True
        engs = ['pe', 'act', 'dve', 'pool', 'sp']
        with contextlib.ExitStack() as es:
            esem = {e: es.enter_context(nc.semaphore('s_' + e)) for e in engs}
            dsem = {e: [es.enter_context(nc.semaphore('d_%s%d' % (e, j))) for j in range(self.NDMASEM)]
                    for e in ('sp', 'act', 'pool')}
            ecount = {e: 0 for e in engs}
            dcount = {e: 0 for e in ('sp', 'act', 'pool')}
            sig = [None] * n
            gate = [None] * n
            for i, o in enumerate(ops):
                e = o['eng']
                if o['dma']:
                    k = dcount[e]
                    dcount[e] += 1
                    s = dsem[e][k % self.NDMASEM]
                    rnd = k // self.NDMASEM
                    sig[i] = (s, 16 * (rnd + 1), 16)
                    if rnd > 0:
                        gate[i] = (s, 16 * rnd)
                elif needed[i]:
                    ecount[e] += 1
                    sig[i] = (esem[e], ecount[e], 1)
            per = {e: [] for e in engs}
            for i, o in enumerate(ops):
                per[o['eng']].append(i)
            self.stats = {e: len(per[e]) for e in engs}
            blk = es.enter_context(nc.Block())

            def run(engname, eobj):
                seen = {}

                def wait(s, v):
                    key = id(s)
                    if seen.get(key, 0) >= v:
                        return
                    seen[key] = v
                    eobj.wait_ge(s, v)

                for i in per[engname]:
                    o = ops[i]
                    for d in sorted(o['deps']):
                        od = ops[d]
                        if od['eng'] == 'pe' and engname == 'pe' and not o['dma'] and not od['dma']:
                            continue
                        s, v, _ = sig[d]
                        wait(s, v)
                    if gate[i] is not None:
                        wait(*gate[i])
                    ins = o['fn'](eobj)
                    if sig[i] is not None:
                        ins.then_inc(sig[i][0], sig[i][2])
                if engname == 'sp':
                    for d in sorted(finals):
                        s, v, _ = sig[d]
                        wait(s, v)
                    for q in ('sp', 'act', 'pool'):
                        for j in range(self.NDMASEM):
                            cnt = (dcount[q] - j + self.NDMASEM - 1) // self.NDMASEM if dcount[q] > j else 0
                            if cnt > 0:
                                wait(dsem[q][j], 16 * cnt)

            @blk.tensor
            def _(e):
                run('pe', e)

            @blk.scalar
            def _(e):
                run('act', e)

            @blk.vector
            def _(e):
                run('dve', e)

            @blk.gpsimd
            def _(e):
                run('pool', e)

            @blk.sync
            def _(e):
                run('sp', e)


_CONSTS = None


def _tile16(M):
    return np.ascontiguousarray(M.reshape(16, 128, 16, 128).transpose(2, 1, 0, 3))


def make_consts():
    global _CONSTS
    if _CONSTS is not None:
        return _CONSTS
    bf = ml_dtypes.bfloat16
    N = 2 * T
    a = np.arange(T, dtype=np.float64)
    ang2 = np.pi * np.outer(2 * a + 1, 2 * a + 1) / (2 * N)
    c = {}
    c['k_c2'] = _tile16(np.cos(ang2)).astype(bf)
    c['k_s2'] = _tile16(np.sin(ang2)).astype(bf)
    angi = np.pi * np.outer(a, 2 * a + 1) / N
    c['k_ci'] = _tile16(np.cos(angi)).astype(bf)
    c['k_si'] = _tile16(-np.sin(angi)).astype(bf)
    del ang2, angi
    pos = np.arange(T, dtype=np.float32)
    inv = (np.float32(10000.0) ** (-(np.arange(0, 64, 2, dtype=np.float32) / np.float32(64)))).astype(np.float32)
    ang = pos[:, None] * inv[None, :]
    c['k_cos'] = np.ascontiguousarray(np.cos(ang).astype(np.float32).reshape(16, 128, 32).transpose(1, 0, 2))
    c['k_sin'] = np.ascontiguousarray(np.sin(ang).astype(np.float32).reshape(16, 128, 32).transpose(1, 0, 2))
    s_i = np.arange(128)[:, None]
    q_i = np.arange(128)[None, :]
    c['k_trige'] = (s_i >= q_i).astype(bf)
    c['k_trile'] = (s_i <= q_i).astype(bf)
    c['k_idb'] = np.eye(128).astype(bf)
    c['k_idf'] = np.eye(128).astype(np.float32)
    t = np.linspace(0.0, 1.0, T, dtype=np.float32)
    w = (2.0 * math.pi * pos / T).astype(np.float32)
    bands = np.linspace(1e-4, 7, 8, dtype=np.float32)
    an = w[:, None] * bands[None, :]
    feats = np.concatenate([t[:, None], np.cos(an), -np.sin(an)], axis=-1).astype(np.float32)
    c['k_featsT'] = np.ascontiguousarray(feats.T)
    deltas = np.abs(np.linspace(math.log(1e-2) / 0.3, math.log(1e-2) / 1.5, 512, dtype=np.float32))
    win = (np.exp(-t[:, None] * deltas[None, :]) + np.float32(0.05)).astype(np.float32)
    c['k_win'] = np.ascontiguousarray(win.reshape(16, 128, 512).transpose(1, 0, 2))
    c['k_iota1'] = np.tile(np.arange(1, 257, dtype=np.float32)[None, :], (128, 1)).astype(bf)
    ti = np.zeros((128, 16, 2), np.float32)
    ti[:, :, 0] = np.arange(16)[None, :]
    ti[:, :, 1] = np.arange(128)[:, None]
    c['k_tokidx'] = ti.astype(bf)
    _CONSTS = c
    return c


CONST_SHAPES = {
    'k_c2': ([16, 128, 16, 128], BF16), 'k_s2': ([16, 128, 16, 128], BF16),
    'k_ci': ([16, 128, 16, 128], BF16), 'k_si': ([16, 128, 16, 128], BF16),
    'k_cos': ([128, 16, 32], F32), 'k_sin': ([128, 16, 32], F32),
    'k_trige': ([128, 128], BF16), 'k_trile': ([128, 128], BF16),
    'k_idb': ([128, 128], BF16), 'k_idf': ([128, 128], F32),
    'k_featsT': ([17, 2048], F32), 'k_win': ([128, 16, 512], F32),
    'k_iota1': ([128, 256], BF16), 'k_tokidx': ([128, 16, 2], BF16),
}

IN_SHAPES = {
    'x': [NB, T, D], 'c': [NB, D], 'w_mod': [NL, D, 6 * D], 'b_mod': [NL, 6 * D],
    'norm1_g': [NL, D], 'norm2_g': [NL, D], 'w_in': [NL, D, 6400], 'hy_conv_w': [NL, 3, 1536],
    'hy_conv_b': [NL, 1536], 'hy_f1_w': [NL, 17, 64], 'hy_f1_b': [NL, 64], 'hy_f1_freq': [NL, 64],
    'hy_f2_w': [NL, 64, 64], 'hy_f2_b': [NL, 64], 'hy_f2_freq': [NL, 64], 'hy_f3_w': [NL, 64, 2048],
    'hy_bias': [NL, 2, 512], 'q_norm_g': [NL, 64], 'k_norm_g': [NL, 64], 'attn_sink': [NL, 8],
    'gm_ln_g': [NL, 512], 'gm_ln_b': [NL, 512], 'gm_ws': [NL, 4, 128, 128], 'gm_b': [NL, 4, 128],
    'w_branch': [NL, 3, 512, D], 'w_out': [NL, D, D], 'w_router': [NL, D, NE],
    'w_e_gate': [NL, NE, D, DE], 'w_e_up': [NL, NE, D, DE], 'w_e_down': [NL, NE, DE, D],
}


def build(nlayers=NL, stage=None, dumps=()):
    nc = bass.Bass("TRN2", target_bir_lowering=False)
    din = {}
    for name, shp in IN_SHAPES.items():
        if stage is not None and name.startswith('w_e_'):
            shp = [NL, 1, 8, 8]
        din[name] = nc.dram_tensor(name, list(shp), F32, kind="ExternalInput").ap()
    for name, (shp, dt) in CONST_SHAPES.items():
        din[name] = nc.dram_tensor(name, list(shp), dt, kind="ExternalInput").ap()
    out_d = nc.dram_tensor("out", [NB, T, D], F32, kind="ExternalOutput").ap()
    xs0 = nc.dram_tensor("xs0", [NB, T, D], F32, kind="Internal").ap()
    h2d = nc.dram_tensor("h2d", [NB, T, D], BF16, kind="Internal").ap()
    modd = nc.dram_tensor("modd", [NL, NB, 6 * D], F32, kind="Internal").ap()
    pqd = nc.dram_tensor("pqd", [2, 2, T, 512], F32, kind="Internal").ap()
    dump_d = {}
    S = Sched(nc)
    PI = math.pi

    with contextlib.ExitStack() as es:
        def sb(name, shape, dt):
            return es.enter_context(nc.sbuf_tensor("s_" + name, list(shape), dt))

        def psum(name, shape, dt):
            return es.enter_context(nc.psum_tensor("p_" + name, list(shape), dt))

        HT = sb("HT", [128, 8, T + 2], BF16)
        KB = sb("KB", [128, 7, 8192], BF16)
        FS = sb("FS", [128, 6144], F32)
        WP = sb("WP", [128, 2, 8, 512], BF16)
        WBA = sb("WBA", [128, 2, 8, 128], BF16)
        idb = sb("idb", [128, 128], BF16)
        idf = sb("idf", [128, 128], F32)
        trige = sb("trige", [128, 128], BF16)
        trile = sb("trile", [128, 128], BF16)
        ropec = sb("ropec", [128, 16, 32], F32)
        ropes = sb("ropes", [128, 16, 32], F32)
        iota1 = sb("iota1", [128, 256], BF16)
        tokidx = sb("tokidx", [128, 16, 2], BF16)
        onesb = sb("onesb", [128, 128], BF16)
        onesf = sb("onesf", [128, 128], F32)
        epsb = sb("epsb", [128, 1], F32)
        condT = sb("condT", [128, NB, 8], BF16)
        cTf = sb("cTf", [128, NB, 8], F32)
        G1T = sb("G1T", [128, NB, 8], F32)
        SH1T = sb("SH1T", [128, NB, 8], F32)
        n1g = sb("n1g", [128, 8], F32)
        small = sb("small", [128, 256], F32)
        affall = sb("affall", [128, 16, 32], F32)
        qg_bc = sb("qg_bc", [128, 64], F32)
        kg_bc = sb("kg_bc", [128, 64], F32)
        esink = sb("esink", [128, 8], F32)
        wr_sb = sb("wr_sb", [128, 8, NE], BF16)
        idx_sb = sb("idx_sb", [128, NE, 4], I32)
        gate_sb = sb("gate_sb", [128, NE, 4], F32)

        PS = [psum("ps%d" % i, [128, 512], F32) for i in range(6)]
        PTs = [psum("pt%d" % i, [128, 8, 128], BF16) for i in range(2)]
        ptc = [0]

        def nextpt():
            i = ptc[0] % 2
            ptc[0] += 1
            return PTs[i], 'PT%d' % i

        def K(i, n=1):
            if n == 1:
                return KB[:, i, :]
            return KB[:, i:i + n, :].rearrange("p a b -> p (a b)")

        def dump(name, ap, shape, dt, rkeys):
            d = nc.dram_tensor("dbg_" + name, list(shape), dt, kind="ExternalOutput").ap()
            dump_d["dbg_" + name] = d
            S.dma('sp', lambda e: e.dma_start(out=d, in_=ap), r=rkeys, w=['dbg_' + name])

        def ld(dst, src, key):
            S.dma('sp', lambda e: e.dma_start(out=dst, in_=src), w=[key])

        ld(idb[:], din['k_idb'][:, :], 'idb')
        ld(idf[:], din['k_idf'][:, :], 'idf')
        ld(trige[:], din['k_trige'][:, :], 'trige')
        ld(trile[:], din['k_trile'][:, :], 'trile')
        ld(ropec[:], din['k_cos'][:, :, :], 'ropec')
        ld(ropes[:], din['k_sin'][:, :, :], 'ropes')
        ld(iota1[:], din['k_iota1'][:, :], 'iota1')
        ld(tokidx[:], din['k_tokidx'][:, :, :], 'tokidx')
        S.dve(lambda e: e.memset(onesb[:], 1.0), w=['onesb'])
        S.dve(lambda e: e.memset(onesf[:], 1.0), w=['onesf'])
        S.dve(lambda e: e.memset(epsb[:], EPS), w=['epsb'])
        S.dve(lambda e: e.memset(HT[:, :, 0:1], 0.0), w=[('HT', 'pad0')])
        S.dve(lambda e: e.memset(HT[:, :, T + 1:T + 2], 0.0), w=[('HT', 'pad1')])

        wp_ctr = [0]

        def wp_load(src_ap, ncols=512):
            slot = wp_ctr[0] % 2
            wp_ctr[0] += 1
            dst = WP[:, slot, :, 0:ncols]
            S.dma('pool', lambda e: e.dma_start(out=dst, in_=src_ap.rearrange("(k p) n -> p k n", p=128)),
                  w=[('WP', slot)])
            return slot

        for b_ in range(NB):
            S.dma('sp', lambda e, b_=b_: e.dma_start(out=cTf[:, b_, :], in_=din['c'][b_, :].rearrange("(k p) -> p k", p=128),
                                                    allow_slow_non_contiguous=True), w=[('cTf', b_)])
        S.act(lambda e: e.activation(out=condT[:], in_=cTf[:], func=AF.Silu), r=['cTf'], w=['condT'])
        modrow = FS[0:NB, 0:6144]
        for l in range(nlayers):
            S.dma('sp', lambda e, l=l: e.dma_start(out=FS[0:NB, 0:6144], in_=din['b_mod'][l:l + 1, :].partition_broadcast(NB)),
                  w=['FS'])
            for j in range(12):
                slot = wp_load(din['w_mod'][l, :, j * 512:(j + 1) * 512])
                bank = PS[j % 2]

                def mmf(e, slot=slot, bank=bank):
                    ins = None
                    for k in range(8):
                        ins = e.matmul(bank[0:NB, :], lhsT=condT[:, :, k], rhs=WP[:, slot, k, :], start=(k == 0), stop=(k == 7))
                    return ins
                S.pe(mmf, r=[('WP', slot), 'condT'], w=['P%d' % (j % 2)])
                S.dve(lambda e, j=j, bank=bank: e.tensor_tensor(out=FS[0:NB, j * 512:(j + 1) * 512], in0=bank[0:NB, :],
                                                                 in1=FS[0:NB, j * 512:(j + 1) * 512], op=ALU.add),
                      r=['P%d' % (j % 2), ('FS', j)], w=[('FS', j)])
            S.dma('sp', lambda e, l=l: e.dma_start(out=modd[l, :, :], in_=FS[0:NB, 0:6144]), r=['FS'], w=[('modd', l)])
        S.fence('FS')
        if stage == 'mod':
            dump('modd', modd[0, :, :], [NB, 6 * D], F32, [('modd', 0)])
            S.emit(final_keys=['modd', 'dbg_modd'])
            return nc, dump_d


        fw1 = sb("fw1", [17, 64], F32)
        fw2 = sb("fw2", [64, 64], F32)
        fpar = sb("fpar", [64, 4], F32)
        KF = [KB[:, i, :].bitcast(F32) for i in range(7)]

        def layer_prep(l):
            S.dma('sp', lambda e: e.dma_start(out=n1g[:], in_=din['norm1_g'][l, :].rearrange("(k p) -> p k", p=128),
                                              allow_slow_non_contiguous=True), w=['n1g'])
            for b_ in range(NB):
                S.dma('sp', lambda e, b_=b_: e.dma_start(out=SH1T[:, b_, :], in_=modd[l, b_, 0:1024].rearrange("(k p) -> p k", p=128),
                                                        allow_slow_non_contiguous=True), r=[('modd', l)], w=[('SH1T', b_)])
                S.dma('sp', lambda e, b_=b_: e.dma_start(out=G1T[:, b_, :], in_=modd[l, b_, 1024:2048].rearrange("(k p) -> p k", p=128),
                                                        allow_slow_non_contiguous=True), r=[('modd', l)], w=[('G1T', b_)])
            S.dve(lambda e: e.scalar_tensor_tensor(out=G1T[:], in0=G1T[:], scalar=1.0, in1=n1g[:].unsqueeze(1).to_broadcast([128, NB, 8]),
                                                   op0=ALU.add, op1=ALU.mult), r=['G1T', 'n1g'], w=['G1T'])
            S.dma('sp', lambda e: e.dma_start(out=qg_bc[:], in_=din['q_norm_g'][l:l + 1, :].partition_broadcast(128)), w=['qg_bc'])
            S.dma('sp', lambda e: e.dma_start(out=kg_bc[:], in_=din['k_norm_g'][l:l + 1, :].partition_broadcast(128)), w=['kg_bc'])
            S.dma('sp', lambda e: e.dma_start(out=esink[:], in_=din['attn_sink'][l:l + 1, :].partition_broadcast(128)), w=['esink'])
            S.act(lambda e: e.activation(out=esink[:], in_=esink[:], func=AF.Exp), r=['esink'], w=['esink'])
            S.dma('pool', lambda e: e.dma_start(out=wr_sb[:], in_=din['w_router'][l].rearrange("(k p) n -> p k n", p=128)), w=['wr_sb'])

        def filter_phase(l):
            for i in range(7):
                S.fence('K%d' % i)
            S.fence('FS')
            featsT = FS[0:17, 0:2048]
            a1T = FS[0:64, 2048:4096]
            a2T = FS[0:64, 4096:6144]
            gplus = K(0, 2).rearrange("p (a b) -> p a b", a=16)
            gminus = K(2, 2).rearrange("p (a b) -> p a b", a=16)
            f3w = KF[4][0:64, 0:2048]
            absum = KF[4][:, 2048:3072]
            wint = [KF[4][:, 3072:3584], KF[4][:, 3584:4096]]
            tmpf, tmpb, tab1, tab2 = (KF[5][:, i * 512:(i + 1) * 512] for i in range(4))
            stg = [KF[5][:, 2048 + i * 512:2048 + (i + 1) * 512] for i in range(4)]
            dblk = K(6).rearrange("p (s m k j) -> p s m k j", s=2, m=2, k=16)
            S.dma('sp', lambda e: e.dma_start(out=featsT, in_=din['k_featsT'][:, :]), w=[('FS', 'feats')])
            S.dma('sp', lambda e: e.dma_start(out=fw1[:], in_=din['hy_f1_w'][l, :, :]), w=['fw1'])
            S.dma('sp', lambda e: e.dma_start(out=fw2[:], in_=din['hy_f2_w'][l, :, :]), w=['fw2'])
            for j, nm in enumerate(('hy_f1_b', 'hy_f1_freq', 'hy_f2_b', 'hy_f2_freq')):
                S.dma('sp', lambda e, j=j, nm=nm: e.dma_start(out=fpar[:, j:j + 1], in_=din[nm][l, :].rearrange("(p o) -> p o", o=1)),
                      w=[('fpar', j)])
            S.dma('sp', lambda e: e.dma_start(out=f3w, in_=din['hy_f3_w'][l, :, :]), w=[('K4', 'f3w')])
            S.pool(lambda e: e.memset(absum, 0.0), w=[('K4', 'absum')])

            def sin_mlp(wt, kdim, src, dst, pb, pf, lname):
                for nb in range(4):
                    cs = slice(nb * 512, (nb + 1) * 512)
                    bank = PS[nb % 2]
                    bk = 'P%d' % (nb % 2)
                    S.pe(lambda e, bank=bank, cs=cs: e.matmul(bank[0:64, :], lhsT=wt[0:kdim, :], rhs=src[0:kdim, cs], start=True, stop=True),
                         r=[lname, ('FS', lname + 'src')], w=[bk])
                    S.dve(lambda e, bank=bank, cs=cs: e.tensor_scalar(out=dst[:, cs], in0=bank[0:64, :], scalar1=fpar[:, pb:pb + 1],
                                                                       scalar2=fpar[:, pf:pf + 1], op0=ALU.add, op1=ALU.mult),
                          r=[bk, 'fpar'], w=[('FS', lname + 'dst%d' % nb)])
                    t1 = tmpf[0:64, :]
                    t2 = tmpb[0:64, :]
                    S.act(lambda e, cs=cs: e.activation(out=t1, in_=dst[:, cs], func=AF.Sin, scale=1.0 / 3), r=[('FS', lname + 'dst%d' % nb)], w=[('K5', 't1')])
                    S.act(lambda e: e.activation(out=t2, in_=t1, func=AF.Square), r=[('K5', 't1')], w=[('K5', 't2')])
                    S.dve(lambda e: e.tensor_scalar(out=t2, in0=t2, scalar1=-4.0, scalar2=3.0, op0=ALU.mult, op1=ALU.add), r=[('K5', 't2')], w=[('K5', 't2')])
                    S.dve(lambda e, cs=cs: e.tensor_tensor(out=dst[:, cs], in0=t1, in1=t2, op=ALU.mult), r=[('K5', 't1'), ('K5', 't2')],
                          w=[('FS', lname + 'dst%d' % nb)])
            S.fence('FS')
            sin_mlp(fw1, 17, featsT, a1T, 0, 1, 'fw1')
            S.fence('FS')
            sin_mlp(fw2, 64, a1T, a2T, 2, 3, 'fw2')
            S.fence('FS')
            S.fence('K5')
            if stage == 'f1':
                dump('a2T', FS[0:64, 4096:6144], [64, 2048], F32, ['FS'])
                return
            for tt in range(16):
                wt = wint[tt % 2]
                wk = ('K4', 'win%d' % (tt % 2))
                S.dma('sp', lambda e, wt=wt, tt=tt: e.dma_start(out=wt, in_=din['k_win'][:, tt, :]), w=[wk])
                for cb in range(4):
                    S.pe(lambda e, cb=cb, tt=tt: e.matmul(PS[cb][:, :], lhsT=a2T[:, tt * 128:(tt + 1) * 128], rhs=f3w[:, cb * 512:(cb + 1) * 512],
                                                          start=True, stop=True), r=['FS', ('K4', 'f3w')], w=['P%d' % cb])
                for o in range(2):
                    S.dve(lambda e, o=o, wt=wt: e.tensor_tensor(out=tmpf, in0=PS[o][:, :], in1=wt, op=ALU.mult), r=['P%d' % o, wk], w=[('K5', 'tf')])
                    S.dve(lambda e, o=o, wt=wt: e.tensor_tensor(out=tmpb, in0=PS[2 + o][:, :], in1=wt, op=ALU.mult), r=['P%d' % (2 + o), wk], w=[('K5', 'tb')])
                    if tt == 0:
                        S.dve(lambda e: e.memset(tmpb[0:1, :], 0.0), r=[('K5', 'tb')], w=[('K5', 'tb')])
                    oc = slice(o * 512, (o + 1) * 512)
                    S.dve(lambda e, tt=tt, oc=oc: e.tensor_tensor(out=gplus[:, tt, oc], in0=tmpf, in1=tmpb, op=ALU.add),
                          r=[('K5', 'tf'), ('K5', 'tb')], w=[('K0', tt * 2 + o), ('K1', tt * 2 + o)])
                    S.dve(lambda e, tt=tt, oc=oc: e.tensor_tensor(out=gminus[:, tt, oc], in0=tmpf, in1=tmpb, op=ALU.subtract),
                          r=[('K5', 'tf'), ('K5', 'tb')], w=[('K2', tt * 2 + o), ('K3', tt * 2 + o)])
                    S.act(lambda e: e.activation(out=tab1, in_=tmpf, func=AF.Abs), r=[('K5', 'tf')], w=[('K5', 'a1')])
                    S.act(lambda e: e.activation(out=tab2, in_=tmpb, func=AF.Abs), r=[('K5', 'tb')], w=[('K5', 'a2')])
                    S.pool(lambda e: e.tensor_tensor(out=tab1, in0=tab1, in1=tab2, op=ALU.add), r=[('K5', 'a1'), ('K5', 'a2')], w=[('K5', 'a1')])
                    S.pool(lambda e, oc=oc: e.tensor_tensor(out=absum[:, oc], in0=absum[:, oc], in1=tab1, op=ALU.add),
                           r=[('K5', 'a1'), ('K4', 'absum')], w=[('K4', 'absum')])
            S.fence('FS')
            rn_bc = FS[:, 0:1024]
            hb_bc = FS[:, 1024:2048]
            S.dma('sp', lambda e: e.dma_start(out=hb_bc, in_=din['hy_bias'][l:l + 1, :, :].rearrange("a o c -> a (o c)").partition_broadcast(128)),
                  w=[('FS', 'hb_bc')])
            S.dve(lambda e: e.tensor_scalar(out=hb_bc, in0=hb_bc, scalar1=2.0 / (2 * T), scalar2=None, op0=ALU.mult), r=[('FS', 'hb_bc')], w=[('FS', 'hb_bc')])
            for o in range(2):
                oc = slice(o * 512, (o + 1) * 512)
                S.pe(lambda e, o=o, oc=oc: e.matmul(PS[4 + o][:, :], lhsT=onesf[:], rhs=absum[:, oc], start=True, stop=True),
                     r=[('K4', 'absum'), 'onesf'], w=['P%d' % (4 + o)])
                S.dve(lambda e, o=o, oc=oc: e.reciprocal(out=rn_bc[:, oc], in_=PS[4 + o][:, :]), r=['P%d' % (4 + o)], w=[('FS', ('rn', o))])
                S.dve(lambda e, oc=oc: e.tensor_scalar(out=rn_bc[:, oc], in0=rn_bc[:, oc], scalar1=2.0 / (2 * T), scalar2=None, op0=ALU.mult),
                      r=[('FS', ('rn', o))], w=[('FS', ('rn', o))])
            for i in range(6):
                S.fence('K%d' % i)
            if stage == 'f2':
                dump('rn', FS[:, 0:2048], [128, 2048], F32, ['FS'])
                dump('gplus', K(0, 2), [128, 16384], BF16, ['K0', 'K1'])
                return
            def fl_load(ft):
                sl = ft % 2
                S.dma('sp', lambda e: e.dma_start(out=dblk[:, sl, 0, :, :], in_=din['k_ci'][ft, :, :, :]), w=[('K6', (sl, 0))])
                S.dma('sp', lambda e: e.dma_start(out=dblk[:, sl, 1, :, :], in_=din['k_si'][ft, :, :, :]), w=[('K6', (sl, 1))])
            fl_load(0)
            for ft in range(16):
                sl = ft % 2
                if ft + 1 < 16:
                    fl_load(ft + 1)
                for o in range(2):
                    oc = slice(o * 512, (o + 1) * 512)
                    for m, gsrc, gk, gk2 in ((0, gplus, 'K0', 'K1'), (1, gminus, 'K2', 'K3')):
                        bank = PS[m * 2 + o]

                        def mmf(e, bank=bank, sl=sl, m=m, gsrc=gsrc, oc=oc):
                            ins = None
                            for jc in range(16):
                                ins = e.matmul(bank[:, :], lhsT=dblk[:, sl, m, jc, :], rhs=gsrc[:, jc, oc], start=(jc == 0), stop=(jc == 15))
                            return ins
                        S.pe(mmf, r=[('K6', (sl, m)), gk, gk2], w=['P%d' % (m * 2 + o)])
                        st_ = stg[m * 2 + o]
                        sk = ('K5', 'stg%d' % (m * 2 + o))
                        S.dve(lambda e, bank=bank, st_=st_, oc=oc: e.tensor_tensor(out=st_, in0=bank[:, :], in1=rn_bc[:, oc], op=ALU.mult),
                              r=['P%d' % (m * 2 + o), ('FS', ('rn', o))], w=[sk])
                        if m == 0:
                            S.dve(lambda e, st_=st_, oc=oc: e.tensor_tensor(out=st_, in0=st_, in1=hb_bc[:, oc], op=ALU.add), r=[sk, ('FS', 'hb_bc')], w=[sk])
                        S.dma('sp', lambda e, st_=st_, m=m, o=o, ft=ft: e.dma_start(out=pqd[m, o, ft * 128:(ft + 1) * 128, :], in_=st_),
                              r=[sk], w=[('pqd', (m, o, ft))])
            for i in range(7):
                S.fence('K%d' % i)
            S.fence('FS')

        def mixer(l, b, xin, xin_key, xout, xout_key):
            XIO = [FS[:, 0:1024], FS[:, 1024:2048]]
            TMP = FS[:, 2048:3072]
            w_in = din['w_in'][l]
            TMPs = [FS[:, 2048:3072], FS[:, 3072:4096]]
            def ht_tile(tt):
                p_ = tt % 2
                xt = XIO[p_]
                xk = ('FS', 'xio%d' % p_)
                tmp_ = TMPs[p_]
                tk = ('FS', 'tmp%d' % p_)
                c_ssq = small[:, 4 * p_:4 * p_ + 1]
                c_rstd = small[:, 4 * p_ + 1:4 * p_ + 2]
                sk0 = ('small', ('a0', p_))
                sk1 = ('small', ('a1', p_))
                S.dma('sp', lambda e, xt=xt, tt=tt: e.dma_start(out=xt, in_=xin[b, tt * 128:(tt + 1) * 128, :]), r=[xin_key], w=[xk])
                S.act(lambda e, xt=xt, tmp_=tmp_, c_ssq=c_ssq: e.activation(out=tmp_, in_=xt, func=AF.Square, accum_out=c_ssq), r=[xk], w=[tk, sk0])
                S.act(lambda e, c_ssq=c_ssq, c_rstd=c_rstd: e.activation(out=c_rstd, in_=c_ssq, func=AF.Sqrt, scale=1.0 / D, bias=epsb[:, 0:1]),
                      r=[sk0, 'epsb'], w=[sk1])
                S.dve(lambda e, c_rstd=c_rstd: e.reciprocal(out=c_rstd, in_=c_rstd), r=[sk1], w=[sk1])
                xn = tmp_.bitcast(BF16)[:, 0:1024]
                S.dve(lambda e, xt=xt, xn=xn, c_rstd=c_rstd: e.tensor_scalar(out=xn, in0=xt, scalar1=c_rstd, scalar2=None, op0=ALU.mult),
                      r=[xk, sk1, tk], w=[tk])
                pt, ptk = nextpt()

                def trf(e, xn=xn, pt=pt):
                    ins = None
                    for k in range(8):
                        ins = e.transpose(out=pt[:, k, :], in_=xn[:, k * 128:(k + 1) * 128], identity=idb[:])
                    return ins
                S.pe(trf, r=[tk, 'idb'], w=[ptk])
                for k in range(8):
                    if tt % 2 == 0:
                        S.act(lambda e, k=k, tt=tt, pt=pt: e.activation(out=HT[:, k, 1 + tt * 128:1 + (tt + 1) * 128], in_=pt[:, k, :], func=AF.Identity,
                                                                        scale=G1T[:, b, k:k + 1], bias=SH1T[:, b, k:k + 1]),
                              r=[ptk, 'G1T', 'SH1T'], w=[('HT', (tt, k))])
                    else:
                        S.dve(lambda e, k=k, tt=tt, pt=pt: e.tensor_scalar(out=HT[:, k, 1 + tt * 128:1 + (tt + 1) * 128], in0=pt[:, k, :],
                                                                           scalar1=G1T[:, b, k:k + 1], scalar2=SH1T[:, b, k:k + 1], op0=ALU.mult, op1=ALU.add),
                              r=[ptk, 'G1T', 'SH1T'], w=[('HT', (tt, k))])
            S.interleave([lambda tt=tt: ht_tile(tt) for tt in range(16)], 2, 'ht')
            S.fence('HT')
            S.fence('FS')
            if stage == 'ht':
                return

            cw_bc = FS[:, 3072:4608].rearrange("p (j c) -> p j c", j=3)
            cb_bc = FS[:, 4608:5120]
            WFs = [K(3, 2).rearrange("p (j k c) -> p j k c", j=4, k=8), K(5, 2).rearrange("p (j k c) -> p j k c", j=4, k=8)]
            hy_tm = [K(0).rearrange("p (a c) -> p a c", a=16), K(1).rearrange("p (a c) -> p a c", a=16), K(2).rearrange("p (a c) -> p a c", a=16)]
            for seg in range(3):
                c0 = seg * 512
                WF = WFs[seg % 2]
                wfk = ['K3', 'K4'] if seg % 2 == 0 else ['K5', 'K6']
                slot = wp_load(w_in[:, c0:c0 + 512])
                S.dma('sp', lambda e, c0=c0: e.dma_start(out=cw_bc, in_=din['hy_conv_w'][l:l + 1, :, c0:c0 + 512].partition_broadcast(128)),
                      w=[('FS', 'cw')])
                S.dma('sp', lambda e, c0=c0: e.dma_start(out=cb_bc, in_=din['hy_conv_b'][l:l + 1, c0:c0 + 512].partition_broadcast(128)),
                      w=[('FS', 'cb')])
                for j in range(3):
                    S.dve(lambda e, j=j, slot=slot, WF=WF: e.tensor_tensor(out=WF[:, j, :, :], in0=WP[:, slot, :, :],
                                                                            in1=cw_bc[:, j, :].unsqueeze(1).to_broadcast([128, 8, 512]), op=ALU.mult),
                          r=[('WP', slot), ('FS', 'cw')], w=[(wfk[0], j), (wfk[1], j)])
                for tt in range(16):
                    bank = PS[tt % 4]
                    bk = 'P%d' % (tt % 4)

                    def mmf(e, bank=bank, tt=tt, WF=WF):
                        ins = None
                        n = 0
                        for j in range(3):
                            for k in range(8):
                                ins = e.matmul(bank[:, :], lhsT=HT[:, k, tt * 128 + j:tt * 128 + j + 128], rhs=WF[:, j, k, :],
                                               start=(n == 0), stop=(n == 23))
                                n += 1
                        return ins
                    S.pe(mmf, r=['HT'] + wfk, w=[bk])
                    S.dve(lambda e, bank=bank, tt=tt, seg=seg: e.tensor_tensor(out=hy_tm[seg][:, tt, :], in0=bank[:, :], in1=cb_bc, op=ALU.add),
                          r=[bk, ('FS', 'cb')], w=[('K%d' % seg, tt)])
            for i in (3, 4, 5, 6):
                S.fence('K%d' % i)
            S.fence('FS')
            if stage == 'hyproj':
                dump('v', K(0), [128, 8192], BF16, ['K0'])
                dump('x1', K(1), [128, 8192], BF16, ['K1'])
                dump('x2', K(2), [128, 8192], BF16, ['K2'])
                return

            Wr = K(3).rearrange("p (a c) -> p a c", a=16)
            Wi = K(4).rearrange("p (a c) -> p a c", a=16)
            dblk = K(5).rearrange("p (s m k j) -> p s m k j", s=2, m=2, k=16)
            pqs = FS[:, 3072:5120].rearrange("p (s m c) -> p s m c", s=2, m=2)
            t1 = FS[:, 0:512]
            t2 = FS[:, 512:1024]
            blk_ctr = [0]

            def load_blk(i):
                sl = blk_ctr[0] % 2
                blk_ctr[0] += 1
                S.dma('sp', lambda e: e.dma_start(out=dblk[:, sl, 0, :, :], in_=din['k_c2'][i, :, :, :]), w=[('K5', (sl, 0))])
                S.dma('sp', lambda e: e.dma_start(out=dblk[:, sl, 1, :, :], in_=din['k_s2'][i, :, :, :]), w=[('K5', (sl, 1))])
                return sl
            for s_ in range(2):
                u = hy_tm[0]
                gsrc = hy_tm[1 + s_]
                for ft in range(16):
                    sl = load_blk(ft)
                    ps_ = ft % 2
                    for m in range(2):
                        S.dma('sp', lambda e, m=m, ft=ft, ps_=ps_, s_=s_: e.dma_start(out=pqs[:, ps_, m, :], in_=pqd[m, s_, ft * 128:(ft + 1) * 128, :]),
                              r=[('pqd', (m, s_, ft))], w=[('FS', ('pq', ps_, m))])
                    bA = PS[(ft % 2) * 2]
                    bB = PS[(ft % 2) * 2 + 1]
                    kA = 'P%d' % ((ft % 2) * 2)
                    kB = 'P%d' % ((ft % 2) * 2 + 1)
                    for m, bank, bk in ((0, bA, kA), (1, bB, kB)):
                        def mmf(e, bank=bank, sl=sl, m=m):
                            ins = None
                            for tc in range(16):
                                ins = e.matmul(bank[:, :], lhsT=dblk[:, sl, m, tc, :], rhs=u[:, tc, :], start=(tc == 0), stop=(tc == 15))
                            return ins
                        S.pe(mmf, r=[('K5', (sl, m)), 'K0'], w=[bk])
                    Pt = pqs[:, ps_, 0, :]
                    Qt = pqs[:, ps_, 1, :]
                    pk = ('FS', ('pq', ps_, 0))
                    qk = ('FS', ('pq', ps_, 1))
                    S.dve(lambda e, bA=bA, Pt=Pt: e.tensor_tensor(out=t1, in0=bA[:, :], in1=Pt, op=ALU.mult), r=[kA, pk], w=[('FS', 't1')])
                    S.dve(lambda e, bB=bB, Qt=Qt: e.tensor_tensor(out=t2, in0=bB[:, :], in1=Qt, op=ALU.mult), r=[kB, qk], w=[('FS', 't2')])
                    S.pool(lambda e, ft=ft: e.tensor_tensor(out=Wr[:, ft, :], in0=t1, in1=t2, op=ALU.add), r=[('FS', 't1'), ('FS', 't2')], w=[('K3', ft)])
                    t3 = FS[:, 1024:1536]
                    t4 = FS[:, 1536:2048]
                    S.dve(lambda e, bB=bB, Pt=Pt, t3=t3: e.tensor_tensor(out=t3, in0=bB[:, :], in1=Pt, op=ALU.mult), r=[kB, pk], w=[('FS', 't3')])
                    S.dve(lambda e, bA=bA, Qt=Qt, t4=t4: e.tensor_tensor(out=t4, in0=bA[:, :], in1=Qt, op=ALU.mult), r=[kA, qk], w=[('FS', 't4')])
                    S.pool(lambda e, ft=ft, t3=t3, t4=t4: e.tensor_tensor(out=Wi[:, ft, :], in0=t3, in1=t4, op=ALU.subtract),
                           r=[('FS', 't3'), ('FS', 't4')], w=[('K4', ft)])
                S.fence('K3')
                S.fence('K4')
                S.fence('K0')
                for tt in range(16):
                    sl = load_blk(tt)
                    bank = PS[4 + tt % 2]
                    bk = 'P%d' % (4 + tt % 2)

                    def mmf(e, bank=bank, sl=sl):
                        ins = None
                        for fc in range(16):
                            e.matmul(bank[:, :], lhsT=dblk[:, sl, 0, fc, :], rhs=Wr[:, fc, :], start=(fc == 0), stop=False)
                            ins = e.matmul(bank[:, :], lhsT=dblk[:, sl, 1, fc, :], rhs=Wi[:, fc, :], start=False, stop=(fc == 15))
                        return ins
                    S.pe(mmf, r=[('K5', (sl, 0)), ('K5', (sl, 1)), 'K3', 'K4'], w=[bk])
                    S.dve(lambda e, bank=bank, tt=tt, gsrc=gsrc: e.tensor_tensor(out=u[:, tt, :], in0=bank[:, :], in1=gsrc[:, tt, :], op=ALU.mult),
                          r=[bk, ('K%d' % (1 + s_), tt)], w=[('K0', tt)])
                S.fence('K0')
            y_hyT = K(3).rearrange("p (a t) -> p a t", a=4)
            S.fence('K3')
            for tt in range(16):
                pt, ptk = nextpt()

                def trf(e, tt=tt, pt=pt):
                    ins = None
                    for cc in range(4):
                        ins = e.transpose(out=pt[:, cc, :], in_=hy_tm[0][:, tt, cc * 128:(cc + 1) * 128], identity=idb[:])
                    return ins
                S.pe(trf, r=['K0', 'idb'], w=[ptk])
                S.act(lambda e, tt=tt, pt=pt: e.activation(out=y_hyT[:, :, tt * 128:(tt + 1) * 128], in_=pt[:, 0:4, :], func=AF.Copy),
                      r=[ptk], w=[('K3', tt)])
            S.fence('K3')
            S.fence('FS')
            if stage == 'hyena':
                dump('yhy', K(3), [128, 8192], BF16, ['K3'])
                return

            uT = K(0).rearrange("p (a t) -> p a t", a=4)
            vln = K(1).rearrange("p (a c) -> p a c", a=16)
            y_gmT = K(4).rearrange("p (a t) -> p a t", a=4)
            wsT = K(2).rearrange("p (g q) -> p g q", g=64)[:, 0:4, :]
            lng_bc = FS[:, 3072:3584]
            lnb_bc = FS[:, 3584:4096]
            gmb_bc = FS[:, 4096:4608]
            gt = [FS[:, 512 + i * 512:1024 + i * 512] for i in range(4)]
            wsf = FS[:, 0:512].rearrange("p (g q) -> p g q", g=4)
            for i in (0, 1, 2, 4):
                S.fence('K%d' % i)
            S.dma('sp', lambda e: e.dma_start(out=lng_bc, in_=din['gm_ln_g'][l:l + 1, :].partition_broadcast(128)), w=[('FS', 'lng')])
            S.dma('sp', lambda e: e.dma_start(out=lnb_bc, in_=din['gm_ln_b'][l:l + 1, :].partition_broadcast(128)), w=[('FS', 'lnb')])
            S.dma('sp', lambda e: e.dma_start(out=gmb_bc, in_=din['gm_b'][l:l + 1, :, :].rearrange("a g p -> a (g p)").partition_broadcast(128)),
                  w=[('FS', 'gmb')])
            S.dma('sp', lambda e: e.dma_start(out=wsf, in_=din['gm_ws'][l].rearrange("g p q -> p g q")), w=[('FS', 'wsf')])
            for g in range(4):
                S.pe(lambda e, g=g: e.transpose(out=PS[5][:, g * 128:(g + 1) * 128], in_=wsf[:, g, :], identity=idf[:]), r=[('FS', 'wsf'), 'idf'], w=['P5'])
            S.act(lambda e: e.activation(out=wsT, in_=PS[5][:, :].rearrange("p (g q) -> p g q", g=4), func=AF.Copy), r=['P5'], w=[('K2', 'wsT')])
            slot = wp_load(w_in[:, COL_GU:COL_GU + 512])
            for cc in range(4):
                for tb in range(4):
                    bank = PS[(cc * 4 + tb) % 4]
                    bk = 'P%d' % ((cc * 4 + tb) % 4)

                    def mmf(e, bank=bank, cc=cc, tb=tb, slot=slot):
                        ins = None
                        for k in range(8):
                            ins = e.matmul(bank[:, :], lhsT=WP[:, slot, k, cc * 128:(cc + 1) * 128], rhs=HT[:, k, 1 + tb * 512:1 + (tb + 1) * 512],
                                           start=(k == 0), stop=(k == 7))
                        return ins
                    S.pe(mmf, r=[('WP', slot), 'HT'], w=[bk])
                    S.act(lambda e, bank=bank, cc=cc, tb=tb: e.activation(out=uT[:, cc, tb * 512:(tb + 1) * 512], in_=bank[:, :], func=AF.Gelu),
                          r=[bk], w=[('K0', (cc, tb))])
            slot = wp_load(w_in[:, COL_GV:COL_GV + 512])
            def gv_tile(tt):
                bank = PS[tt % 4]
                bk = 'P%d' % (tt % 4)
                g_ = gt[tt % 4]
                gk = ('FS', 'g%d' % (tt % 4))

                def mmf(e, bank=bank, tt=tt, slot=slot):
                    ins = None
                    for k in range(8):
                        ins = e.matmul(bank[:, :], lhsT=HT[:, k, 1 + tt * 128:1 + (tt + 1) * 128], rhs=WP[:, slot, k, :], start=(k == 0), stop=(k == 7))
                    return ins
                S.pe(mmf, r=[('WP', slot), 'HT'], w=[bk])
                S.act(lambda e, bank=bank, g_=g_: e.activation(out=g_, in_=bank[:, :], func=AF.Gelu), r=[bk], w=[gk])
                p_ = tt % 4
                bn6 = small[:, 64 + 8 * p_:64 + 8 * p_ + 6]
                mv_ = small[:, 128 + 2 * p_:128 + 2 * p_ + 2]
                bnk = ('small', ('bn', p_))
                mvk = ('small', ('mv', p_))
                S.dve(lambda e, g_=g_, bn6=bn6: e.bn_stats(out=bn6, in_=g_), r=[gk], w=[bnk])
                S.dve(lambda e, bn6=bn6, mv_=mv_: e.bn_aggr(out=mv_, in_=bn6), r=[bnk], w=[mvk])
                S.act(lambda e, mv_=mv_: e.activation(out=mv_[:, 1:2], in_=mv_[:, 1:2], func=AF.Sqrt, bias=epsb[:, 0:1]), r=[mvk, 'epsb'], w=[mvk])
                S.dve(lambda e, mv_=mv_: e.reciprocal(out=mv_[:, 1:2], in_=mv_[:, 1:2]), r=[mvk], w=[mvk])
                S.dve(lambda e, g_=g_, mv_=mv_: e.scalar_tensor_tensor(out=g_, in0=g_, scalar=mv_[:, 0:1], in1=lng_bc, op0=ALU.subtract, op1=ALU.mult),
                      r=[gk, mvk, ('FS', 'lng')], w=[gk])
                S.dve(lambda e, g_=g_, mv_=mv_, tt=tt: e.scalar_tensor_tensor(out=vln[:, tt, :], in0=g_, scalar=mv_[:, 1:2], in1=lnb_bc, op0=ALU.mult, op1=ALU.add),
                      r=[gk, mvk, ('FS', 'lnb')], w=[('K1', tt)])
            S.interleave([lambda tt=tt: gv_tile(tt) for tt in range(16)], 4, 'gv')

            def sp_tile(n):
                bank = PS[4 + n % 2]
                bk = 'P%d' % (4 + n % 2)

                def mmf(e, bank=bank, n=n):
                    ins = None
                    for g in range(4):
                        ins = e.matmul(bank[:, g * 128:(g + 1) * 128], lhsT=vln[:, n, g * 128:(g + 1) * 128], rhs=wsT[:, g, :], start=True, stop=True)
                    return ins
                S.pe(mmf, r=[('K1', n), ('K2', 'wsT')], w=[bk])
                g_ = gt[n % 4]
                gk = ('FS', 'g%d' % (n % 4))
                S.dve(lambda e, bank=bank, g_=g_: e.tensor_tensor(out=g_, in0=bank[:, :], in1=gmb_bc, op=ALU.add), r=[bk, ('FS', 'gmb')], w=[gk])
                S.dve(lambda e, g_=g_, n=n: e.tensor_tensor(out=y_gmT[:, :, n * 128:(n + 1) * 128], in0=g_.rearrange("p (g q) -> p g q", g=4),
                                                           in1=uT[:, :, n * 128:(n + 1) * 128], op=ALU.mult), r=[gk, 'K0'], w=[('K4', n)])
            S.interleave([lambda n=n: sp_tile(n) for n in range(16)], 2, 'sp')
            for i in (0, 1, 2, 4):
                S.fence('K%d' % i)
            S.fence('FS')
            if stage == 'gmlp':
                dump('ygm', K(4), [128, 8192], BF16, ['K4'])
                return

            QT = K(0).rearrange("p (j t) -> p j t", j=4)
            KT = K(1)[:, 0:2048]
            Vat = K(1)[:, 2048:4096].rearrange("p (a c) -> p a c", a=16)
            y_atT = K(5, 2).rearrange("p (h t) -> p h t", h=8)
            for i in (0, 1, 5, 6):
                S.fence('K%d' % i)
            slotq = wp_ctr[0] % 2
            wp_ctr[0] += 1
            for j in range(4):
                for a_ in range(2):
                    hc = COL_Q + (a_ * 4 + j) * 64
                    S.dma('pool', lambda e, j=j, a_=a_, hc=hc: e.dma_start(out=WP[:, slotq, :, j * 128 + a_ * 64:j * 128 + (a_ + 1) * 64],
                                                                          in_=w_in[:, hc:hc + 64].rearrange("(k p) d -> p k d", p=128)),
                          w=[('WP', slotq)])
            slotk = wp_load(w_in[:, COL_K:COL_K + 256], 256)

            def qk_norm_rope(bank, bk, nh, gbc, tt, p_):
                w_ = nh * 64
                h_ = nh * 32
                o_ = p_ * 2304 if p_ < 2 else 4608 + (p_ - 2) * 576
                sq = FS[:, o_:o_ + w_]
                qn = FS[:, o_ + w_:o_ + 2 * w_]
                qr = FS[:, o_ + 2 * w_:o_ + 2 * w_ + h_].bitcast(BF16)
                tA, tB, tC, tD = (FS[:, o_ + 2 * w_ + h_ + i * h_:o_ + 2 * w_ + h_ + (i + 1) * h_] for i in range(4))
                st_ = small[:, 96 + 8 * p_:96 + 8 * p_ + nh]
                kq = lambda n_: ('FS', (n_, p_))
                sk = ('small', ('qs', p_))
                S.act(lambda e: e.activation(out=sq[:, 0:w_], in_=bank[:, 0:w_], func=AF.Square), r=[bk], w=[kq('sq')])
                S.dve(lambda e: e.tensor_reduce(out=st_, in_=sq[:, 0:w_].rearrange("p (h d) -> p h d", h=nh), axis=AX.X, op=ALU.add),
                      r=[kq('sq')], w=[sk])
                S.act(lambda e: e.activation(out=st_, in_=st_, func=AF.Sqrt, scale=1.0 / 64, bias=epsb[:, 0:1]), r=[sk, 'epsb'], w=[sk])
                S.dve(lambda e: e.reciprocal(out=st_, in_=st_), r=[sk], w=[sk])
                q3 = qn[:, 0:w_].rearrange("p (h d) -> p h d", h=nh)
                S.dve(lambda e: e.tensor_tensor(out=q3, in0=bank[:, 0:w_].rearrange("p (h d) -> p h d", h=nh),
                                                in1=st_.unsqueeze(2).to_broadcast([128, nh, 64]), op=ALU.mult),
                      r=[bk, sk], w=[kq('qn')])
                S.pool(lambda e: e.tensor_tensor(out=q3, in0=q3, in1=gbc[:].unsqueeze(1).to_broadcast([128, nh, 64]), op=ALU.mult),
                       r=[kq('qn'), 'qg_bc', 'kg_bc'], w=[kq('qn')])
                cb_ = ropec[:, tt, :].unsqueeze(1).to_broadcast([128, nh, 32])
                sb_ = ropes[:, tt, :].unsqueeze(1).to_broadcast([128, nh, 32])
                x1_ = q3[:, :, 0:32]
                x2_ = q3[:, :, 32:64]
                r3 = qr[:, 0:w_].rearrange("p (h d) -> p h d", h=nh)
                v = lambda t_: t_[:, 0:nh * 32].rearrange("p (h d) -> p h d", h=nh)
                S.dve(lambda e: e.tensor_tensor(out=v(tA), in0=x1_, in1=cb_, op=ALU.mult), r=[kq('qn'), 'ropec'], w=[kq('tA')])
                S.pool(lambda e: e.tensor_tensor(out=v(tB), in0=x2_, in1=sb_, op=ALU.mult), r=[kq('qn'), 'ropes'], w=[kq('tB')])
                S.dve(lambda e: e.tensor_tensor(out=v(tC), in0=x2_, in1=cb_, op=ALU.mult), r=[kq('qn'), 'ropec'], w=[kq('tC')])
                S.pool(lambda e: e.tensor_tensor(out=v(tD), in0=x1_, in1=sb_, op=ALU.mult), r=[kq('qn'), 'ropes'], w=[kq('tD')])
                S.dve(lambda e: e.tensor_tensor(out=r3[:, :, 0:32], in0=v(tA), in1=v(tB), op=ALU.subtract), r=[kq('tA'), kq('tB')], w=[kq('qr')])
                S.pool(lambda e: e.tensor_tensor(out=r3[:, :, 32:64], in0=v(tC), in1=v(tD), op=ALU.add), r=[kq('tC'), kq('tD')], w=[kq('qr2')])
                return qr, [kq('qr'), kq('qr2')]

            def q_tile(tt):
                bank = PS[tt % 2]
                bk = 'P%d' % (tt % 2)
                pt = PTs[tt % 2]
                ptn = 'PT%d' % (tt % 2)

                def mmf(e, bank=bank, tt=tt):
                    ins = None
                    for k in range(8):
                        ins = e.matmul(bank[:, :], lhsT=HT[:, k, 1 + tt * 128:1 + (tt + 1) * 128], rhs=WP[:, slotq, k, :], start=(k == 0), stop=(k == 7))
                    return ins
                S.pe(mmf, r=[('WP', slotq), 'HT'], w=[bk])
                qr, qrk = qk_norm_rope(bank, bk, 8, qg_bc, tt, tt % 2)

                def trf(e, qr=qr, pt=pt):
                    ins = None
                    for j in range(4):
                        ins = e.transpose(out=pt[:, j, :], in_=qr[:, j * 128:(j + 1) * 128], identity=idb[:])
                    return ins
                S.pe(trf, r=qrk + ['idb'], w=[ptn])
                S.act(lambda e, tt=tt, pt=pt: e.activation(out=QT[:, :, tt * 128:(tt + 1) * 128], in_=pt[:, 0:4, :], func=AF.Copy),
                      r=[ptn], w=[('K0', tt)])

            def k_tile(tt):
                pt = PTs[(tt + 1) % 2]
                ptn = 'PT%d' % ((tt + 1) % 2)
                bank2 = PS[2 + tt % 2]
                bk2 = 'P%d' % (2 + tt % 2)

                def mmf2(e, bank2=bank2, tt=tt):
                    ins = None
                    for k in range(8):
                        ins = e.matmul(bank2[:, 0:256], lhsT=HT[:, k, 1 + tt * 128:1 + (tt + 1) * 128], rhs=WP[:, slotk, k, 0:256], start=(k == 0), stop=(k == 7))
                    return ins
                S.pe(mmf2, r=[('WP', slotk), 'HT'], w=[bk2])
                S.act(lambda e, bank2=bank2, tt=tt: e.activation(out=Vat[:, tt, :], in_=bank2[:, 128:256], func=AF.Copy), r=[bk2], w=[('K1', ('v', tt))])
                kr, krk = qk_norm_rope(bank2, bk2, 2, kg_bc, tt, 2 + tt % 2)
                S.pe(lambda e, kr=kr, pt=pt: e.transpose(out=pt[:, 4, :], in_=kr[:, 0:128], identity=idb[:]), r=krk + ['idb'], w=[ptn])
                S.act(lambda e, tt=tt, pt=pt: e.activation(out=KT[:, tt * 128:(tt + 1) * 128], in_=pt[:, 4, :], func=AF.Copy),
                      r=[ptn], w=[('K1', ('k', tt))])
            thunks = []
            for tt in range(16):
                thunks.append(lambda tt=tt: q_tile(tt))
                thunks.append(lambda tt=tt: k_tile(tt))
            S.interleave(thunks, 4, 'qk')
            S.fence('K0')
            S.fence('K1')
            S.fence('FS')
            S.fence('PT0')
            S.fence('PT1')
            pbuf = [FS[:, i * 256:(i + 1) * 256].bitcast(BF16) for i in range(6)]
            dens = [FS[0:64, 1536:2048], FS[0:64, 2048:2560]]
            itc = [0, 0]

            def core(g, n):
                if True:
                    it = itc[0]
                    c_ = itc[1]
                    gp = slice(g * 64, (g + 1) * 64)
                    ms = [m for m in (n - 1, n, n + 1) if 0 <= m < 16]
                    bO = PS[2 + 2 * (c_ % 2)]
                    bD = PS[3 + 2 * (c_ % 2)]
                    kO = 'P%d' % (2 + 2 * (c_ % 2))
                    kD = 'P%d' % (3 + 2 * (c_ % 2))
                    den = dens[c_ % 2]
                    dk = ('FS', ('den', c_ % 2))
                    itc[1] += 1
                    for mi, m in enumerate(ms):
                        bS = PS[c_ % 2]
                        kS = 'P%d' % (c_ % 2)
                        pb = pbuf[(c_ % 2) * 3 + mi]
                        pk = ('FS', 'pb%d' % ((c_ % 2) * 3 + mi))
                        it += 1
                        itc[0] = it
                        S.pe(lambda e, bS=bS, m=m, n=n, gp=gp: e.matmul(bS[:, :], lhsT=KT[gp, m * 128:(m + 1) * 128], rhs=QT[gp, :, n * 128:(n + 1) * 128],
                                                                        start=True, stop=True), r=['K0', 'K1'], w=[kS])
                        S.act(lambda e, bS=bS, pb=pb: e.activation(out=pb, in_=bS[:, :], func=AF.Exp, scale=0.125), r=[kS], w=[pk])
                        if m != n:
                            msk = trige if m < n else trile
                            S.pool(lambda e, pb=pb, msk=msk: e.tensor_tensor(out=pb.rearrange("p (j q) -> p j q", j=4), in0=pb.rearrange("p (j q) -> p j q", j=4),
                                                                             in1=msk[:].unsqueeze(1).to_broadcast([128, 4, 128]), op=ALU.mult),
                                   r=[pk, 'trige', 'trile'], w=[pk])

                        def mmf(e, pb=pb, m=m, mi=mi, g=g, last=(mi == len(ms) - 1), bO=bO, bD=bD):
                            e.matmul(bO[0:64, :], lhsT=Vat[:, m, g * 64:(g + 1) * 64], rhs=pb, start=(mi == 0), stop=last)
                            return e.matmul(bD[0:64, :], lhsT=onesb[:, 0:64], rhs=pb, start=(mi == 0), stop=last)
                        S.pe(mmf, r=[pk, 'K1', 'onesb'], w=[kO, kD])
                    S.dve(lambda e, g=g, bD=bD, den=den: e.tensor_tensor(out=den.rearrange("p (j q) -> p j q", j=4),
                                                                         in0=bD[0:64, :].rearrange("p (j q) -> p j q", j=4),
                                                                         in1=esink[0:64, g * 4:(g + 1) * 4].unsqueeze(2).to_broadcast([64, 4, 128]), op=ALU.add),
                          r=[kD, 'esink'], w=[dk])
                    S.act(lambda e, den=den: e.activation(out=den, in_=den, func=AF.Ln), r=[dk], w=[dk])
                    S.act(lambda e, den=den: e.activation(out=den, in_=den, func=AF.Exp, scale=-1.0), r=[dk], w=[dk])
                    S.dve(lambda e, g=g, n=n, bO=bO, den=den: e.tensor_tensor(out=y_atT[0:64, g * 4:(g + 1) * 4, n * 128:(n + 1) * 128],
                                                                             in0=bO[0:64, :].rearrange("p (j q) -> p j q", j=4),
                                                                             in1=den.rearrange("p (j q) -> p j q", j=4), op=ALU.mult),
                          r=[kO, dk], w=[('K5', (g, n)), ('K6', (g, n))])
            S.interleave([lambda g=g, n=n: core(g, n) for g in range(2) for n in range(16)], 2, 'core')
            for i in (0, 1, 5, 6):
                S.fence('K%d' % i)
            S.fence('FS')
            if stage == 'attn':
                dump('yat', K(5, 2), [128, 16384], BF16, ['K5', 'K6'])
                return

            mergedT = K(0, 2).rearrange("p (k t) -> p k t", k=8)
            sgs = [[FS[:, (p_ * 6 + i) * 512:(p_ * 6 + i + 1) * 512] for i in range(3)] for p_ in range(2)]
            accs = [FS[:, (p_ * 6 + 3) * 512:(p_ * 6 + 4) * 512] for p_ in range(2)]
            tqs = [FS[:, (p_ * 6 + 4) * 512:(p_ * 6 + 5) * 512] for p_ in range(2)]
            tq2s = [FS[:, (p_ * 6 + 5) * 512:(p_ * 6 + 6) * 512] for p_ in range(2)]
            yT = [K(3).rearrange("p (a t) -> p a t", a=4), y_atT, K(4).rearrange("p (a t) -> p a t", a=4)]
            ykeys = [['K3'], ['K5', 'K6'], ['K4']]
            wbr = din['w_branch'][l]
            S.fence('WP')
            S.fence('WBA')

            def mg_load(dt):
                slot = dt % 2
                mwf = WP[:, slot, :, :].rearrange("p k c -> p (k c)")
                wg = [mwf[:, i * 1024:(i + 1) * 1024].rearrange("p (k c) -> p k c", k=8) for i in range(3)]
                wb_hy = mwf[:, 3072:3584].rearrange("p (k c) -> p k c", k=4)
                wb_gm = mwf[:, 3584:4096].rearrange("p (k c) -> p k c", k=4)
                wb_at = WBA[0:64, slot, :, :]
                dc = slice(dt * 128, (dt + 1) * 128)
                for i in range(3):
                    c0 = COL_GATE + i * 1024 + dt * 128
                    S.dma('pool', lambda e, i=i, c0=c0, wg=wg: e.dma_start(out=wg[i], in_=w_in[:, c0:c0 + 128].rearrange("(k p) n -> p k n", p=128)),
                          w=[('WP', (slot, i))])
                S.dma('pool', lambda e: e.dma_start(out=wb_hy, in_=wbr[0, :, dc].rearrange("(k p) n -> p k n", p=128)), w=[('WP', (slot, 3))])
                S.dma('pool', lambda e: e.dma_start(out=wb_gm, in_=wbr[2, :, dc].rearrange("(k p) n -> p k n", p=128)), w=[('WP', (slot, 4))])
                S.dma('pool', lambda e: e.dma_start(out=wb_at, in_=wbr[1, :, dc].rearrange("(h d) n -> d h n", d=64)), w=[('WBA', slot)])
                return slot, wg, [wb_hy, wb_at, wb_gm]
            mg_next = mg_load(0)
            for dt in range(8):
                slot, wg, wbs = mg_next
                if dt + 1 < 8:
                    mg_next = mg_load(dt + 1)
                mwk = [('WP', (slot, i)) for i in range(5)]
                def mg_tb(tb, dt=dt, slot=slot, wg=wg, wbs=wbs):
                    tcs = slice(tb * 512, (tb + 1) * 512)
                    p_ = tb % 2
                    sg = sgs[p_]
                    acc = accs[p_]
                    tq = tqs[p_]
                    tq2 = tq2s[p_]
                    fk = lambda n_, p_=p_: ('FS', (n_, p_))
                    for i in range(3):
                        uG = 3 * p_ + (2 * i) % 3
                        uB = 3 * p_ + (2 * i + 1) % 3
                        bG = PS[uG]
                        bB = PS[uB]
                        kG = 'P%d' % uG
                        kB_ = 'P%d' % uB

                        def mmg(e, bG=bG, i=i, tb=tb, wg=wg):
                            ins = None
                            for k in range(8):
                                ins = e.matmul(bG[:, :], lhsT=wg[i][:, k, :], rhs=HT[:, k, 1 + tb * 512:1 + (tb + 1) * 512], start=(k == 0), stop=(k == 7))
                            return ins
                        S.pe(mmg, r=[('WP', (slot, i)), 'HT'], w=[kG])
                        S.act(lambda e, bG=bG, i=i, sg=sg: e.activation(out=sg[i], in_=bG[:, :], func=AF.Sigmoid), r=[kG], w=[fk('sg%d' % i)])

                        def mmb(e, bB=bB, i=i, tcs=tcs, wbs=wbs):
                            ins = None
                            if i == 1:
                                for h in range(8):
                                    ins = e.matmul(bB[:, :], lhsT=wbs[1][:, h, :], rhs=y_atT[0:64, h, tcs], start=(h == 0), stop=(h == 7))
                            else:
                                for cc in range(4):
                                    ins = e.matmul(bB[:, :], lhsT=wbs[i][:, cc, :], rhs=yT[i][:, cc, tcs], start=(cc == 0), stop=(cc == 3))
                            return ins
                        S.pe(mmb, r=mwk[3:5] + [('WBA', slot)] + ykeys[i], w=[kB_])
                        if i == 0:
                            S.dve(lambda e, bB=bB, acc=acc, sg=sg: e.tensor_tensor(out=acc, in0=bB[:, :], in1=sg[0], op=ALU.mult), r=[kB_, fk('sg0')], w=[fk('acc')])
                        elif i == 1:
                            S.dve(lambda e, bB=bB, tq=tq, sg=sg: e.tensor_tensor(out=tq, in0=bB[:, :], in1=sg[1], op=ALU.mult), r=[kB_, fk('sg1')], w=[fk('tq')])
                            S.pool(lambda e, acc=acc, tq=tq: e.tensor_tensor(out=acc, in0=acc, in1=tq, op=ALU.add), r=[fk('acc'), fk('tq')], w=[fk('acc')])
                        else:
                            S.dve(lambda e, bB=bB, tq2=tq2, sg=sg: e.tensor_tensor(out=tq2, in0=bB[:, :], in1=sg[2], op=ALU.mult), r=[kB_, fk('sg2')], w=[fk('tq2')])
                            S.pool(lambda e, dt=dt, tcs=tcs, acc=acc, tq2=tq2: e.tensor_tensor(out=mergedT[:, dt, tcs], in0=acc, in1=tq2, op=ALU.add),
                                   r=[fk('acc'), fk('tq2')], w=[('K0', (dt, tcs.start)), ('K1', (dt, tcs.start))])
                S.interleave([lambda tb=tb, mg_tb=mg_tb: mg_tb(tb) for tb in range(4)], 2, 'mg')
            for i in range(7):
                S.fence('K%d' % i)
            S.fence('FS')
            S.fence('HT')
            S.fence('WP')
            S.fence('WBA')
            if stage == 'merge':
                dump('merged', K(0, 2), [128, 16384], BF16, ['K0', 'K1'])
                return

            wout = K(2).rearrange("p (k c) -> p k c", k=8)
            gt1_bc = FS[:, 3072:4096]
            G2_bc = FS[:, 4096:5120]
            SH2_bc = FS[:, 5120:6144]
            h2b = K(3).rearrange("p (s c) -> p s c", s=8)
            h2T = K(4).rearrange("p (s k t) -> p s k t", s=8, k=8)
            for hf in range(2):
                S.dma('pool', lambda e, hf=hf: e.dma_start(out=wout[:, :, hf * 512:(hf + 1) * 512],
                                                           in_=din['w_out'][l, :, hf * 512:(hf + 1) * 512].rearrange("(k p) n -> p k n", p=128)),
                      w=[('K2', hf)])
            S.dma('sp', lambda e: e.dma_start(out=gt1_bc, in_=modd[l, b:b + 1, 2048:3072].partition_broadcast(128)), r=[('modd', l)], w=[('FS', 'gt1')])
            S.dma('sp', lambda e: e.dma_start(out=G2_bc, in_=modd[l, b:b + 1, 4096:5120].partition_broadcast(128)), r=[('modd', l)], w=[('FS', 'G2')])
            S.dma('sp', lambda e: e.dma_start(out=SH2_bc, in_=modd[l, b:b + 1, 3072:4096].partition_broadcast(128)), r=[('modd', l)], w=[('FS', 'SH2')])
            S.dma('sp', lambda e: e.dma_start(out=TMP, in_=din['norm2_g'][l:l + 1, :].partition_broadcast(128)), w=[('FS', 'tmp')])
            S.dve(lambda e: e.scalar_tensor_tensor(out=G2_bc, in0=G2_bc, scalar=1.0, in1=TMP, op0=ALU.add, op1=ALU.mult),
                  r=[('FS', 'G2'), ('FS', 'tmp')], w=[('FS', 'G2')])
            S.dve(lambda e: e.tensor_tensor(out=wout, in0=wout, in1=gt1_bc.unsqueeze(1).to_broadcast([128, 8, 1024]), op=ALU.mult),
                  r=['K2', ('FS', 'gt1')], w=['K2'])
            TMP5 = [FS[:, 2048:3072], FS[:, 3072:4096]]
            TK5 = [('FS', 'tmp'), ('FS', 'gt1')]

            def w5_tile(tt):
                p_ = tt % 2
                TMP = TMP5[p_]
                tk_ = TK5[p_]
                sb_ = 160 + 16 * p_
                xt = XIO[tt % 2]
                xk = ('FS', 'xio%d' % (tt % 2))
                S.dma('sp', lambda e, xt=xt, tt=tt: e.dma_start(out=xt, in_=xin[b, tt * 128:(tt + 1) * 128, :]), r=[xin_key], w=[xk])
                for hf in range(2):
                    bank = PS[(tt * 2 + hf) % 4]
                    bk = 'P%d' % ((tt * 2 + hf) % 4)

                    def mmf(e, bank=bank, tt=tt, hf=hf):
                        ins = None
                        for k in range(8):
                            ins = e.matmul(bank[:, :], lhsT=mergedT[:, k, tt * 128:(tt + 1) * 128], rhs=wout[:, k, hf * 512:(hf + 1) * 512], start=(k == 0), stop=(k == 7))
                        return ins
                    S.pe(mmf, r=['K0', 'K1', 'K2'], w=[bk])
                    S.dve(lambda e, bank=bank, xt=xt, hf=hf: e.tensor_tensor(out=xt[:, hf * 512:(hf + 1) * 512], in0=bank[:, :], in1=xt[:, hf * 512:(hf + 1) * 512], op=ALU.add),
                          r=[bk, xk], w=[xk])
                S.dma('sp', lambda e, xt=xt, tt=tt: e.dma_start(out=xout[b, tt * 128:(tt + 1) * 128, :], in_=xt), r=[xk], w=[(xout_key, (b, tt))])
                S.act(lambda e, xt=xt: e.activation(out=TMP, in_=xt, func=AF.Square, accum_out=small[:, sb_ + 2:sb_ + 3]), r=[xk], w=[tk_, ('small', (2, p_))])
                S.act(lambda e: e.activation(out=small[:, sb_ + 3:sb_ + 4], in_=small[:, sb_ + 2:sb_ + 3], func=AF.Sqrt, scale=1.0 / D, bias=epsb[:, 0:1]),
                      r=[('small', (2, p_)), 'epsb'], w=[('small', (3, p_))])
                S.dve(lambda e: e.reciprocal(out=small[:, sb_ + 3:sb_ + 4], in_=small[:, sb_ + 3:sb_ + 4]), r=[('small', (3, p_))], w=[('small', (3, p_))])
                S.dve(lambda e, xt=xt: e.scalar_tensor_tensor(out=TMP, in0=xt, scalar=small[:, sb_ + 3:sb_ + 4], in1=G2_bc, op0=ALU.mult, op1=ALU.mult),
                      r=[xk, ('small', (3, p_)), ('FS', 'G2')], w=[tk_])
                hs = tt % 2
                S.pool(lambda e, hs=hs: e.tensor_tensor(out=h2b[:, hs, :], in0=TMP, in1=SH2_bc, op=ALU.add), r=[tk_, ('FS', 'SH2')], w=[('K3', hs)])
                S.dma('sp', lambda e, hs=hs, tt=tt: e.dma_start(out=h2d[b, tt * 128:(tt + 1) * 128, :], in_=h2b[:, hs, :]), r=[('K3', hs)], w=[('h2d', (b, tt))])

                pt, ptk = nextpt()

                def trf(e, hs=hs, pt=pt):
                    ins = None
                    for k in range(8):
                        ins = e.transpose(out=pt[:, k, :], in_=h2b[:, hs, k * 128:(k + 1) * 128], identity=idb[:])
                    return ins
                S.pe(trf, r=[('K3', hs), 'idb'], w=[ptk])
                S.act(lambda e, hs=hs, pt=pt: e.activation(out=h2T[:, hs, :, :], in_=pt[:, :, :], func=AF.Copy), r=[ptk], w=[('K4', hs)])
                bank = PS[4 + tt % 2]
                bk = 'P%d' % (4 + tt % 2)

                def mmr(e, bank=bank, hs=hs):
                    ins = None
                    for k in range(8):
                        ins = e.matmul(bank[:, 0:NE], lhsT=h2T[:, hs, k, :], rhs=wr_sb[:, k, :], start=(k == 0), stop=(k == 7))
                    return ins
                S.pe(mmr, r=[('K4', hs), 'wr_sb'], w=[bk])
                S.dve(lambda e, bank=bank: e.tensor_reduce(out=small[:, sb_ + 4:sb_ + 5], in_=bank[:, 0:NE], axis=AX.X, op=ALU.max), r=[bk], w=[('small', (4, p_))])
                S.dve(lambda e: e.tensor_scalar(out=small[:, sb_ + 5:sb_ + 6], in0=small[:, sb_ + 4:sb_ + 5], scalar1=-1.0, scalar2=None, op0=ALU.mult), r=[('small', (4, p_))], w=[('small', (5, p_))])
                S.act(lambda e, bank=bank: e.activation(out=small[:, 32 + sb_:48 + sb_], in_=bank[:, 0:NE], func=AF.Exp, bias=small[:, sb_ + 5:sb_ + 6], accum_out=small[:, sb_ + 6:sb_ + 7]),
                      r=[bk, ('small', (5, p_))], w=[('small', ('e', p_)), ('small', (6, p_))])
                S.dve(lambda e: e.reciprocal(out=small[:, sb_ + 6:sb_ + 7], in_=small[:, sb_ + 6:sb_ + 7]), r=[('small', (6, p_))], w=[('small', (6, p_))])
                S.dve(lambda e, tt=tt: e.tensor_scalar(out=affall[:, tt, b * NE:(b + 1) * NE], in0=small[:, 32 + sb_:48 + sb_], scalar1=small[:, sb_ + 6:sb_ + 7], scalar2=None, op0=ALU.mult),
                      r=[('small', ('e', p_)), ('small', (6, p_))], w=[('affall', (b, tt))])
            S.interleave([lambda tt=tt: w5_tile(tt) for tt in range(16)], 2, 'w5')
            for i in range(7):
                S.fence('K%d' % i)
            S.fence('FS')


        mx8 = sb("mx8", [32, 8], F32)

        def moe(l, xout, xout_key):
            for i in range(7):
                S.fence('K%d' % i)
            S.fence('FS')
            S.fence('WP')
            WPf = WP[:].rearrange("p a k c -> p (a k c)")
            Rall = WPf[:, 2048:4096].rearrange("p (a b c) -> p a b c", a=16, b=32)
            affT = KF[0][0:32, 0:2048]
            work = KF[0][0:32, 2048:4096]
            mask = KF[1][0:32, 0:2048]
            ones_r = KF[1][0:32, 2048:4096]
            keyr = KF[2][0:32, 0:2048]
            for tb in range(4):
                bank = PS[tb % 2]
                bk = 'P%d' % (tb % 2)

                def trf(e, bank=bank, tb=tb):
                    ins = None
                    for j in range(4):
                        ins = e.transpose(out=bank[0:32, j * 128:(j + 1) * 128], in_=affall[:, tb * 4 + j, :], identity=idf[:])
                    return ins
                S.pe(trf, r=['affall', 'idf'], w=[bk])
                S.act(lambda e, bank=bank, tb=tb: e.activation(out=affT[:, tb * 512:(tb + 1) * 512], in_=bank[0:32, :], func=AF.Copy),
                      r=[bk], w=[('K0', ('affT', tb))])
            S.dve(lambda e: e.tensor_copy(out=work, in_=affT), r=['K0'], w=[('K0', 'work')])
            S.pool(lambda e: e.memset(ones_r, 1.0), w=[('K1', 'ones')])
            for r_ in range(CAP // 8):
                S.dve(lambda e: e.max(out=mx8[:], in_=work), r=[('K0', 'work')], w=['mx8'])
                if r_ < CAP // 8 - 1:
                    S.dve(lambda e: e.match_replace(out=work, in_to_replace=mx8[:], in_values=work, imm_value=-1.0),
                          r=[('K0', 'work'), 'mx8'], w=[('K0', 'work')])
            S.dve(lambda e: e.tensor_scalar(out=mask, in0=affT, scalar1=mx8[:, 7:8], scalar2=None, op0=ALU.is_ge), r=['K0', 'mx8'], w=[('K1', 'mask')])
            S.dve(lambda e: e.tensor_tensor_scan(out=keyr, data0=ones_r, data1=mask, initial=0.0, op0=ALU.mult, op1=ALU.add),
                  r=[('K1', 'mask'), ('K1', 'ones')], w=[('K2', 'key')])
            S.dve(lambda e: e.tensor_tensor(out=keyr, in0=keyr, in1=mask, op=ALU.mult), r=[('K2', 'key'), ('K1', 'mask')], w=[('K2', 'key')])

            def trk(e):
                ins = None
                for tt in range(16):
                    ins = e.transpose(out=PS[2][:, tt * 32:(tt + 1) * 32], in_=keyr[:, tt * 128:(tt + 1) * 128], identity=idf[0:32, 0:32])
                return ins
            S.pe(trk, r=[('K2', 'key'), 'idf'], w=['P2'])
            keyb = WPf[:, 4096:4608].rearrange("p (a b) -> p a b", a=16)
            S.act(lambda e: e.activation(out=keyb.rearrange("p a b -> p (a b)"), in_=PS[2][:, :], func=AF.Copy), r=['P2'], w=[('WP', 'keyb')])
            S.dve(lambda e: e.tensor_copy(out=Rall[:, :, :, 0:2], in_=tokidx[:].unsqueeze(2).to_broadcast([128, 16, 32, 2])), r=['tokidx'], w=[('WP', ('Rall', 0))])
            S.dve(lambda e: e.tensor_copy(out=Rall[:, :, :, 2], in_=affall[:]), r=['affall'], w=[('WP', ('Rall', 2))])
            S.dve(lambda e: e.tensor_tensor(out=Rall[:, :, :, 3], in0=affall[:], in1=Rall[:, :, :, 2], op=ALU.subtract),
                  r=['affall', ('WP', ('Rall', 2))], w=[('WP', ('Rall', 3))])
            S.fence('HT')
            HTf = HT[:].rearrange("p k t -> p (k t)")
            Poh = [HTf[:, i * 4096:(i + 1) * 4096].rearrange("p (t c) -> p t c", t=16) for i in range(2)]
            idxf = small[:, 48:56]
            rkeys = [('WP', ('Rall', 0)), ('WP', ('Rall', 2)), ('WP', ('Rall', 3))]

            def route(e_, b_, part):
                if True:
                    be = b_ * NE + e_
                    po = Poh[b_]
                    if part == 0:
                        S.dve(lambda e, po=po, be=be: e.tensor_tensor(out=po, in0=iota1[:].unsqueeze(1).to_broadcast([128, 16, 256]),
                                                                     in1=keyb[:, :, be].unsqueeze(2).to_broadcast([128, 16, 256]), op=ALU.is_equal),
                              r=['iota1', ('WP', 'keyb')], w=[('HT', ('poh', b_))])
                        return
                    for st in range(2):
                        bank = PS[5]
                        bk = 'P5'

                        def mmf(e, bank=bank, po=po, st=st, be=be):
                            ins = None
                            for tt in range(16):
                                ins = e.matmul(bank[:, 0:4], lhsT=po[:, tt, st * 128:(st + 1) * 128], rhs=Rall[:, tt, be, :], start=(tt == 0), stop=(tt == 15))
                            return ins
                        S.pe(mmf, r=[('HT', ('poh', b_))] + rkeys, w=[bk])
                        col = b_ * 2 + st
                        S.dve(lambda e, bank=bank: e.tensor_copy(out=idxf[:, 0:4], in_=bank[:, 0:4]), r=[bk], w=[('small', 'i0')])
                        S.dve(lambda e: e.scalar_tensor_tensor(out=idxf[:, 4:5], in0=idxf[:, 0:1], scalar=128.0, in1=idxf[:, 1:2], op0=ALU.mult, op1=ALU.add),
                              r=[('small', 'i0')], w=[('small', 'i1')])
                        S.dve(lambda e, col=col, b_=b_: e.tensor_scalar(out=idx_sb[:, e_, col:col + 1], in0=idxf[:, 4:5], scalar1=float(T * b_), scalar2=None,
                                                                        op0=ALU.add),
                              r=[('small', 'i1')], w=[('idx', (e_, col))])
                        S.dve(lambda e, col=col: e.tensor_tensor(out=gate_sb[:, e_, col:col + 1], in0=idxf[:, 2:3], in1=idxf[:, 3:4], op=ALU.add),
                              r=[('small', 'i0')], w=[('gate', (e_, col))])
            for i in (0, 1, 2, 3):
                S.fence('K%d' % i)
            if stage == 'route':
                for e_ in range(NE):
                    for b_ in range(2):
                        route(e_, b_, 0)
                        route(e_, b_, 1)
                return
            gt2_bc = FS[:, 0:2048].rearrange("p (b c) -> p b c", b=2)
            ystg = [FS[:, 2048 + i * 1024:2048 + (i + 1) * 1024] for i in range(4)]
            xg = K(4).rearrange("p (s q c) -> p s q c", s=2, q=4)
            xgT = K(5).rearrange("p (s k c) -> p s k c", s=2, k=8)
            actT = K(6).rearrange("p (f c) -> p f c", f=16)
            sgt = [WP[:].rearrange("p a k c -> p (a k c)").bitcast(F32)[:, i * 512:(i + 1) * 512] for i in range(2)]
            for b_ in range(2):
                S.dma('sp', lambda e, b_=b_: e.dma_start(out=gt2_bc[:, b_, :], in_=modd[l, b_:b_ + 1, 5120:6144].partition_broadcast(128)),
                      r=[('modd', l)], w=[('FS', ('gt2', b_))])
            wg_d = din['w_e_gate'][l]
            wu_d = din['w_e_up'][l]
            wd_d = din['w_e_down'][l]
            pieces = [(e_, q) for e_ in range(NE) for q in range(6)]

            def load_piece(pi):
                e_, q = pieces[pi]
                sl = pi % 4
                if q < 4:
                    dst = K(sl).rearrange("p (m k f) -> p m k f", m=2, k=8)
                    S.dma('pool', lambda e: e.dma_start(out=dst[:, 0, :, :], in_=wg_d[e_, :, q * 512:(q + 1) * 512].rearrange("(k p) f -> p k f", p=128)),
                          w=[('K%d' % sl, 'a')])
                    S.dma('pool', lambda e: e.dma_start(out=dst[:, 1, :, :], in_=wu_d[e_, :, q * 512:(q + 1) * 512].rearrange("(k p) f -> p k f", p=128)),
                          w=[('K%d' % sl, 'b')])
                else:
                    h = q - 4
                    dst = K(sl).rearrange("p (f c) -> p f c", f=16)
                    for hh in range(2):
                        S.dma('pool', lambda e, hh=hh: e.dma_start(out=dst[:, hh * 8:(hh + 1) * 8, :],
                                                                   in_=wd_d[e_, hh * 1024:(hh + 1) * 1024, h * 512:(h + 1) * 512].rearrange("(f p) c -> p f c", p=128)),
                              w=[('K%d' % sl, 'a' if hh == 0 else 'b')])

            def gathers(e_):
                par = e_ % 2
                for bs in range(4):
                    b_ = bs // 2
                    S.dma('pool', lambda e, bs=bs, b_=b_: e.indirect_dma_start(out=xg[:, par, bs, :], out_offset=None, in_=h2d.rearrange("b t d -> (b t) d"),
                                                                               in_offset=bass.IndirectOffsetOnAxis(ap=idx_sb[:, e_, bs:bs + 1], axis=0)),
                          r=['h2d', ('idx', (e_, bs))], w=[('K4', (par, bs))])

            def expert(e_):
                par = e_ % 2
                if e_ + 2 < NE:
                    route(e_ + 2, 0, 0)
                if e_ + 1 < NE:
                    gathers(e_ + 1)
                for bs in range(4):
                    pt, ptk = nextpt()

                    def trf(e, bs=bs, pt=pt):
                        ins = None
                        for k in range(8):
                            ins = e.transpose(out=pt[:, k, :], in_=xg[:, par, bs, k * 128:(k + 1) * 128], identity=idb[:])
                        return ins
                    S.pe(trf, r=[('K4', (par, bs)), 'idb'], w=[ptk])
                    S.act(lambda e, bs=bs, pt=pt: e.activation(out=xgT[:, par, :, bs * 128:(bs + 1) * 128], in_=pt[:, :, :], func=AF.Copy),
                          r=[ptk], w=[('K5', (par, bs))])
                for q in range(6):
                    pi = e_ * 6 + q
                    if pi + 3 < len(pieces):
                        load_piece(pi + 3)
                    if e_ + 2 < NE:
                        if q == 2:
                            route(e_ + 2, 0, 1)
                            route(e_ + 2, 1, 0)
                        elif q == 4:
                            route(e_ + 2, 1, 1)
                    sl = pi % 4
                    wk = 'K%d' % sl
                    if q < 4:
                        wsl = K(sl).rearrange("p (m k f) -> p m k f", m=2, k=8)
                        for fl in range(4):
                            ft = q * 4 + fl
                            bA = PS[(ft % 2) * 2]
                            bU = PS[(ft % 2) * 2 + 1]
                            kA = 'P%d' % ((ft % 2) * 2)
                            kU = 'P%d' % ((ft % 2) * 2 + 1)
                            for m, bank, bk in ((0, bA, kA), (1, bU, kU)):
                                def mmf(e, m=m, bank=bank, fl=fl, wsl=wsl):
                                    ins = None
                                    for k in range(8):
                                        ins = e.matmul(bank[:, :], lhsT=wsl[:, m, k, fl * 128:(fl + 1) * 128], rhs=xgT[:, par, k, :], start=(k == 0), stop=(k == 7))
                                    return ins
                                S.pe(mmf, r=[wk, 'K5'], w=[bk])
                            st_ = sgt[ft % 2]
                            S.act(lambda e, bA=bA, st_=st_: e.activation(out=st_, in_=bA[:, :], func=AF.Silu), r=[kA], w=[('WP', ('sg', ft % 2))])
                            S.dve(lambda e, bU=bU, st_=st_, ft=ft: e.tensor_tensor(out=actT[:, ft, :], in0=bU[:, :], in1=st_, op=ALU.mult),
                                  r=[kU, ('WP', ('sg', ft % 2))], w=[('K6', ft)])
                    else:
                        h = q - 4
                        wsl = K(sl).rearrange("p (f c) -> p f c", f=16)
                        for bs in range(4):
                            b_ = bs // 2
                            bank = PS[4 + bs % 2]
                            bk = 'P%d' % (4 + bs % 2)

                            def mmf(e, bank=bank, bs=bs, wsl=wsl):
                                ins = None
                                for ft in range(16):
                                    ins = e.matmul(bank[:, :], lhsT=actT[:, ft, bs * 128:(bs + 1) * 128], rhs=wsl[:, ft, :], start=(ft == 0), stop=(ft == 15))
                                return ins
                            S.pe(mmf, r=[wk, 'K6'], w=[bk])
                            S.dve(lambda e, bank=bank, bs=bs, b_=b_, h=h: e.scalar_tensor_tensor(out=ystg[bs][:, h * 512:(h + 1) * 512], in0=bank[:, :],
                                                                                                  scalar=gate_sb[:, e_, bs:bs + 1],
                                                                                                  in1=gt2_bc[:, b_, h * 512:(h + 1) * 512],
                                                                                                  op0=ALU.mult, op1=ALU.mult),
                                  r=[bk, ('gate', (e_, bs)), ('FS', ('gt2', b_))], w=[('FS', ('y', bs))])
                for bs in range(4):
                    b_ = bs // 2
                    S.dma('pool', lambda e, bs=bs, b_=b_: e.indirect_dma_start(out=xout.rearrange("b t d -> (b t) d"),
                                                                               out_offset=bass.IndirectOffsetOnAxis(ap=idx_sb[:, e_, bs:bs + 1], axis=0),
                                                                               in_=ystg[bs], in_offset=None, compute_op=ALU.add),
                          r=[('FS', ('y', bs)), ('idx', (e_, bs))], w=[xout_key])

            for pi in range(3):
                load_piece(pi)
            for e_ in range(2):
                for b_ in range(2):
                    route(e_, b_, 0)
                    route(e_, b_, 1)
            gathers(0)
            for e_ in range(NE):
                expert(e_)
            S.fence('HT')
            S.dve(lambda e: e.memset(HT[:, :, 0:1], 0.0), w=['HT'])
            S.dve(lambda e: e.memset(HT[:, :, T + 1:T + 2], 0.0), w=['HT'])
            for i in range(7):
                S.fence('K%d' % i)
            S.fence('FS')
            S.fence('WP')

        for l in range(nlayers):
            xin = din['x'] if l == 0 else xs0
            xout = xs0 if (l == 0 and nlayers > 1) else out_d
            xin_key = 'xin%d' % l
            xout_key = 'xin%d' % (l + 1)
            layer_prep(l)
            if stage == 'prep':
                dump('G1T', G1T[:], [128, NB, 8], F32, ['G1T'])
                dump('esink', esink[:], [128, 8], F32, ['esink'])
                break
            filter_phase(l)
            if stage in ('f1', 'f2'):
                break
            if stage == 'filter':
                dump('pqd', pqd, [2, 2, T, 512], F32, ['pqd'])
                break
            stop = False
            for b in range(NB):
                mixer(l, b, xin, xin_key, xout, xout_key)
                if stage in ('ht', 'hyproj', 'hyena', 'gmlp', 'attn', 'merge'):
                    stop = True
                    break
            if stop:
                break
            if stage == 'mixer':
                dump('h2d', h2d, [NB, T, D], BF16, ['h2d'])
                dump('aff', affall[:], [128, 16, 32], F32, ['affall'])
                break
            moe(l, xout, xout_key)
            if stage == 'route':
                dump('idx', idx_sb[:], [128, NE, 4], I32, ['idx'])
                dump('gate', gate_sb[:], [128, NE, 4], F32, ['gate'])
                break
        fk = ['xin%d' % nlayers] + list(dump_d.keys())
        if stage is not None:
            fk += ['pqd', 'h2d', 'xin1', 'idx', 'gate', 'HT', 'affall']
        S.emit(final_keys=fk)
    return nc, dump_d


_NC_CACHE = {}


def kernel(**inputs):
    consts = make_consts()
    if 'full' not in _NC_CACHE:
        _NC_CACHE['full'] = build()
    nc, _ = _NC_CACHE['full']
    shared = {k: np.ascontiguousarray(np.asarray(v, dtype=np.float32)) for k, v in inputs.items() if k not in ('x', 'c')}
    shared.update(consts)
    x = np.asarray(inputs['x'], dtype=np.float32)
    c = np.asarray(inputs['c'], dtype=np.float32)
    in_maps = []
    for i in range(8):
        m = dict(shared)
        m['x'] = np.ascontiguousarray(x[2 * i:2 * i + 2])
        m['c'] = np.ascontiguousarray(c[2 * i:2 * i + 2])
        in_maps.append(m)
    res = run_bass_kernel_spmd(nc, in_maps, core_ids=list(range(8)))
    return np.concatenate([r['out'] for r in res.results], axis=0).astype(np.float32)
```

```python
import math
import contextlib
import numpy as np
import ml_dtypes
import concourse.bass as bass
import concourse.mybir as mybir
from concourse.bass_utils import run_bass_kernel_spmd

F32 = mybir.dt.float32
BF16 = mybir.dt.bfloat16
I32 = mybir.dt.int32
ALU = mybir.AluOpType
AF = mybir.ActivationFunctionType
AX = mybir.AxisListType

T = 2048
D = 1024
NB = 2
NL = 2
NE = 16
CAP = 256
DE = 2048
COL_V, COL_X1, COL_X2, COL_Q, COL_K, COL_GU, COL_GV, COL_GATE = 0, 512, 1024, 1536, 2048, 2304, 2816, 3328
EPS = 1e-6


class Sched:
    NDMASEM = 8

    def __init__(self, nc):
        self.nc = nc
        self.ops = []
        self.st = {}
        self._cap = None

    def _acc(self, key, write, i, deps):
        if isinstance(key, tuple):
            base, sub = key
        else:
            base, sub = key, None
        b = self.st.get(base)
        if b is None:
            b = self.st[base] = {'w': set(), 'r': set(), 'subs': {}}
        if sub is None:
            deps |= b['w']
            for s in b['subs'].values():
                deps |= s['w']
                if write:
                    deps |= s['r']
            if write:
                deps |= b['r']
                b['w'] = {i}
                b['r'] = set()
                b['subs'] = {}
            else:
                b['r'].add(i)
        else:
            s = b['subs'].get(sub)
            if s is None:
                s = b['subs'][sub] = {'w': set(), 'r': set()}
            deps |= b['w']
            deps |= s['w']
            if write:
                deps |= b['r']
                deps |= s['r']
                s['w'] = {i}
                s['r'] = set()
            else:
                s['r'].add(i)

    def fence(self, base):
        assert self._cap is None
        b = self.st.get(base)
        if b is None:
            return
        w = set(b['w']) | set(b['r'])
        for s in b['subs'].values():
            w |= s['w']
            w |= s['r']
        b['w'] = w
        b['r'] = set()
        b['subs'] = {}

    def interleave(self, thunks, group, name=''):
        import os
        on = os.environ.get('IL_ON')
        if on is not None and name not in on.split(','):
            group = 1
        for g0 in range(0, len(thunks), group):
            chains = []
            for th in thunks[g0:g0 + group]:
                self._cap = []
                th()
                chains.append(self._cap)
                self._cap = None
            pos = [0] * len(chains)
            left = sum(len(c) for c in chains)
            while left:
                for ci, c in enumerate(chains):
                    if pos[ci] < len(c):
                        self.add(*c[pos[ci]])
                        pos[ci] += 1
                        left -= 1

    def add(self, eng, fn, reads=(), writes=(), dma=False):
        if self._cap is not None:
            self._cap.append((eng, fn, tuple(reads), tuple(writes), dma))
            return None
        i = len(self.ops)
        deps = set()
        for k in reads:
            kb = k[0] if isinstance(k, tuple) else k
            self._acc(k, isinstance(kb, str) and len(kb) >= 2 and kb[0] == 'P' and (kb[1].isdigit() or kb[1] == 'T'), i, deps)
        for k in writes:
            self._acc(k, True, i, deps)
        deps.discard(i)
        self.ops.append(dict(eng=eng, fn=fn, deps=deps, dma=dma))
        return i

    def pe(self, fn, r=(), w=()):
        return self.add('pe', fn, r, w)

    def act(self, fn, r=(), w=()):
        return self.add('act', fn, r, w)

    def dve(self, fn, r=(), w=()):
        return self.add('dve', fn, r, w)

    def pool(self, fn, r=(), w=()):
        return self.add('pool', fn, r, w)

    def dma(self, q, fn, r=(), w=()):
        return self.add(q, fn, r, w, dma=True)

    def last_writers(self, key):
        deps = set()
        self._acc(key, False, -1, deps)
        return deps

    def emit(self, final_keys=()):
        nc = self.nc
        ops = self.ops
        n = len(ops)
        needed = [False] * n
        for i, o in enumerate(ops):
            for d in o['deps']:
                od = ops[d]
                if od['eng'] == 'pe' and o['eng'] == 'pe' and not o['dma'] and not od['dma']:
                    continue
                needed[d] = True
        finals = set()
        for k in final_keys:
            finals |= self.last_writers(k)
        finals.discard(-1)
        for d in finals:
            needed[d] = True
        engs = ['pe', 'act', 'dve', 'pool', 'sp']
        with contextlib.ExitStack() as es:
            esem = {e: es.enter_context(nc.semaphore('s_' + e)) for e in engs}
            dsem = {e: [es.enter_context(nc.semaphore('d_%s%d' % (e, j))) for j in range(self.NDMASEM)]
                    for e in ('sp', 'act', 'pool')}
            ecount = {e: 0 for e in engs}
            dcount = {e: 0 for e in ('sp', 'act', 'pool')}
            sig = [None] * n
            gate = [None] * n
            for i, o in enumerate(ops):
                e = o['eng']
                if o['dma']:
                    k = dcount[e]
                    dcount[e] += 1
                    s = dsem[e][k % self.NDMASEM]
                    rnd = k // self.NDMASEM
                    sig[i] = (s, 16 * (rnd + 1), 16)
                    if rnd > 0:
                        gate[i] = (s, 16 * rnd)
                elif needed[i]:
                    ecount[e] += 1
                    sig[i] = (esem[e], ecount[e], 1)
            per = {e: [] for e in engs}
            for i, o in enumerate(ops):
                per[o['eng']].append(i)
            self.stats = {e: len(per[e]) for e in engs}
            blk = es.enter_context(nc.Block())

            def run(engname, eobj):
                seen = {}

                def wait(s, v):
                    key = id(s)
                    if seen.get(key, 0) >= v:
                        return
                    seen[key] = v
                    eobj.wait_ge(s, v)

                for i in per[engname]:
                    o = ops[i]
                    for d in sorted(o['deps']):
                        od = ops[d]
                        if od['eng'] == 'pe' and engname == 'pe' and not o['dma'] and not od['dma']:
                            continue
                        s, v, _ = sig[d]
                        wait(s, v)
                    if gate[i] is not None:
                        wait(*gate[i])
                    ins = o['fn'](eobj)
                    if sig[i] is not None:
                        ins.then_inc(sig[i][0], sig[i][2])
                if engname == 'sp':
                    for d in sorted(finals):
                        s, v, _ = sig[d]
                        wait(s, v)
                    for q in ('sp', 'act', 'pool'):
                        for j in range(self.NDMASEM):
                            cnt = (dcount[q] - j + self.NDMASEM - 1) // self.NDMASEM if dcount[q] > j else 0
                            if cnt > 0:
                                wait(dsem[q][j], 16 * cnt)

            @blk.tensor
            def _(e):
                run('pe', e)

            @blk.scalar
            def _(e):
                run('act', e)

            @blk.vector
            def _(e):
                run('dve', e)

            @blk.gpsimd
            def _(e):
                run('pool', e)

            @blk.sync
            def _(e):
                run('sp', e)


_CONSTS = None


def _tile16(M):
    return np.ascontiguousarray(M.reshape(16, 128, 16, 128).transpose(2, 1, 0, 3))


def make_consts():
    global _CONSTS
    if _CONSTS is not None:
        return _CONSTS
    bf = ml_dtypes.bfloat16
    N = 2 * T
    a = np.arange(T, dtype=np.float64)
    ang2 = np.pi * np.outer(2 * a + 1, 2 * a + 1) / (2 * N)
    c = {}
    c['k_c2'] = _tile16(np.cos(ang2)).astype(bf)
    c['k_s2'] = _tile16(np.sin(ang2)).astype(bf)
    angi = np.pi * np.outer(a, 2 * a + 1) / N
    c['k_ci'] = _tile16(np.cos(angi)).astype(bf)
    c['k_si'] = _tile16(-np.sin(angi)).astype(bf)
    del ang2, angi
    pos = np.arange(T, dtype=np.float32)
    inv = (np.float32(10000.0) ** (-(np.arange(0, 64, 2, dtype=np.float32) / np.float32(64)))).astype(np.float32)
    ang = pos[:, None] * inv[None, :]
    c['k_cos'] = np.ascontiguousarray(np.cos(ang).astype(np.float32).reshape(16, 128, 32).transpose(1, 0, 2))
    c['k_sin'] = np.ascontiguousarray(np.sin(ang).astype(np.float32).reshape(16, 128, 32).transpose(1, 0, 2))
    s_i = np.arange(128)[:, None]
    q_i = np.arange(128)[None, :]
    c['k_trige'] = (s_i >= q_i).astype(bf)
    c['k_trile'] = (s_i <= q_i).astype(bf)
    c['k_idb'] = np.eye(128).astype(bf)
    c['k_idf'] = np.eye(128).astype(np.float32)
    t = np.linspace(0.0, 1.0, T, dtype=np.float32)
    w = (2.0 * math.pi * pos / T).astype(np.float32)
    bands = np.linspace(1e-4, 7, 8, dtype=np.float32)
    an = w[:, None] * bands[None, :]
    feats = np.concatenate([t[:, None], np.cos(an), -np.sin(an)], axis=-1).astype(np.float32)
    c['k_featsT'] = np.ascontiguousarray(feats.T)
    deltas = np.abs(np.linspace(math.log(1e-2) / 0.3, math.log(1e-2) / 1.5, 512, dtype=np.float32))
    win = (np.exp(-t[:, None] * deltas[None, :]) + np.float32(0.05)).astype(np.float32)
    c['k_win'] = np.ascontiguousarray(win.reshape(16, 128, 512).transpose(1, 0, 2))
    c['k_iota1'] = np.tile(np.arange(1, 257, dtype=np.float32)[None, :], (128, 1)).astype(bf)
    ti = np.zeros((128, 16, 2), np.float32)
    ti[:, :, 0] = np.arange(16)[None, :]
    ti[:, :, 1] = np.arange(128)[:, None]
    c['k_tokidx'] = ti.astype(bf)
    _CONSTS = c
    return c


CONST_SHAPES = {
    'k_c2': ([16, 128, 16, 128], BF16), 'k_s2': ([16, 128, 16, 128], BF16),
    'k_ci': ([16, 128, 16, 128], BF16), 'k_si': ([16, 128, 16, 128], BF16),
    'k_cos': ([128, 16, 32], F32), 'k_sin': ([128, 16, 32], F32),
    'k_trige': ([128, 128], BF16), 'k_trile': ([128, 128], BF16),
    'k_idb': ([128, 128], BF16), 'k_idf': ([128, 128], F32),
    'k_featsT': ([17, 2048], F32), 'k_win': ([128, 16, 512], F32),
    'k_iota1': ([128, 256], BF16), 'k_tokidx': ([128, 16, 2], BF16),
}

IN_SHAPES = {
    'x': [NB, T, D], 'c': [NB, D], 'w_mod': [NL, D, 6 * D], 'b_mod': [NL, 6 * D],
    'norm1_g': [NL, D], 'norm2_g': [NL, D], 'w_in': [NL, D, 6400], 'hy_conv_w': [NL, 3, 1536],
    'hy_conv_b': [NL, 1536], 'hy_f1_w': [NL, 17, 64], 'hy_f1_b': [NL, 64], 'hy_f1_freq': [NL, 64],
    'hy_f2_w': [NL, 64, 64], 'hy_f2_b': [NL, 64], 'hy_f2_freq': [NL, 64], 'hy_f3_w': [NL, 64, 2048],
    'hy_bias': [NL, 2, 512], 'q_norm_g': [NL, 64], 'k_norm_g': [NL, 64], 'attn_sink': [NL, 8],
    'gm_ln_g': [NL, 512], 'gm_ln_b': [NL, 512], 'gm_ws': [NL, 4, 128, 128], 'gm_b': [NL, 4, 128],
    'w_branch': [NL, 3, 512, D], 'w_out': [NL, D, D], 'w_router': [NL, D, NE],
    'w_e_gate': [NL, NE, D, DE], 'w_e_up': [NL, NE, D, DE], 'w_e_down': [NL, NE, DE, D],
}


def build(nlayers=NL, stage=None, dumps=()):
    nc = bass.Bass("TRN2", target_bir_lowering=False)
    din = {}
    for name, shp in IN_SHAPES.items():
        if stage is not None and name.startswith('w_e_'):
            shp = [NL, 1, 8, 8]
        din[name] = nc.dram_tensor(name, list(shp), F32, kind="ExternalInput").ap()
    for name, (shp, dt) in CONST_SHAPES.items():
        din[name] = nc.dram_tensor(name, list(shp), dt, kind="ExternalInput").ap()
    out_d = nc.dram_tensor("out", [NB, T, D], F32, kind="ExternalOutput").ap()
    xs0 = nc.dram_tensor("xs0", [NB, T, D], F32, kind="Internal").ap()
    h2d = nc.dram_tensor("h2d", [NB, T, D], BF16, kind="Internal").ap()
    modd = nc.dram_tensor("modd", [NL, NB, 6 * D], F32, kind="Internal").ap()
    pqd = nc.dram_tensor("pqd", [2, 2, T, 512], F32, kind="Internal").ap()
    dump_d = {}
    S = Sched(nc)
    PI = math.pi

    with contextlib.ExitStack() as es:
        def sb(name, shape, dt):
            return es.enter_context(nc.sbuf_tensor("s_" + name, list(shape), dt))

        def psum(name, shape, dt):
            return es.enter_context(nc.psum_tensor("p_" + name, list(shape), dt))

        HT = sb("HT", [128, 8, T + 2], BF16)
        KB = sb("KB", [128, 7, 8192], BF16)
        FS = sb("FS", [128, 6144], F32)
        WP = sb("WP", [128, 2, 8, 512], BF16)
        WBA = sb("WBA", [128, 2, 8, 128], BF16)
        idb = sb("idb", [128, 128], BF16)
        idf = sb("idf", [128, 128], F32)
        trige = sb("trige", [128, 128], BF16)
        trile = sb("trile", [128, 128], BF16)
        ropec = sb("ropec", [128, 16, 32], F32)
        ropes = sb("ropes", [128, 16, 32], F32)
        iota1 = sb("iota1", [128, 256], BF16)
        tokidx = sb("tokidx", [128, 16, 2], BF16)
        onesb = sb("onesb", [128, 128], BF16)
        onesf = sb("onesf", [128, 128], F32)
        epsb = sb("epsb", [128, 1], F32)
        condT = sb("condT", [128, NB, 8], BF16)
        cTf = sb("cTf", [128, NB, 8], F32)
        G1T = sb("G1T", [128, NB, 8], F32)
        SH1T = sb("SH1T", [128, NB, 8], F32)
        n1g = sb("n1g", [128, 8], F32)
        small = sb("small", [128, 256], F32)
        affall = sb("affall", [128, 16, 32], F32)
        qg_bc = sb("qg_bc", [128, 64], F32)
        kg_bc = sb("kg_bc", [128, 64], F32)
        esink = sb("esink", [128, 8], F32)
        wr_sb = sb("wr_sb", [128, 8, NE], BF16)
        idx_sb = sb("idx_sb", [128, NE, 4], I32)
        gate_sb = sb("gate_sb", [128, NE, 4], F32)

        PS = [psum("ps%d" % i, [128, 512], F32) for i in range(6)]
        PTs = [psum("pt%d" % i, [128, 8, 128], BF16) for i in range(2)]
        ptc = [0]

        def nextpt():
            i = ptc[0] % 2
            ptc[0] += 1
            return PTs[i], 'PT%d' % i

        def K(i, n=1):
            if n == 1:
                return KB[:, i, :]
            return KB[:, i:i + n, :].rearrange("p a b -> p (a b)")

        def dump(name, ap, shape, dt, rkeys):
            d = nc.dram_tensor("dbg_" + name, list(shape), dt, kind="ExternalOutput").ap()
            dump_d["dbg_" + name] = d
            S.dma('sp', lambda e: e.dma_start(out=d, in_=ap), r=rkeys, w=['dbg_' + name])

        def ld(dst, src, key):
            S.dma('sp', lambda e: e.dma_start(out=dst, in_=src), w=[key])

        ld(idb[:], din['k_idb'][:, :], 'idb')
        ld(idf[:], din['k_idf'][:, :], 'idf')
        ld(trige[:], din['k_trige'][:, :], 'trige')
        ld(trile[:], din['k_trile'][:, :], 'trile')
        ld(ropec[:], din['k_cos'][:, :, :], 'ropec')
        ld(ropes[:], din['k_sin'][:, :, :], 'ropes')
        ld(iota1[:], din['k_iota1'][:, :], 'iota1')
        ld(tokidx[:], din['k_tokidx'][:, :, :], 'tokidx')
        S.dve(lambda e: e.memset(onesb[:], 1.0), w=['onesb'])
        S.dve(lambda e: e.memset(onesf[:], 1.0), w=['onesf'])
        S.dve(lambda e: e.memset(epsb[:], EPS), w=['epsb'])
        S.dve(lambda e: e.memset(HT[:, :, 0:1], 0.0), w=[('HT', 'pad0')])
        S.dve(lambda e: e.memset(HT[:, :, T + 1:T + 2], 0.0), w=[('HT', 'pad1')])

        wp_ctr = [0]

        def wp_load(src_ap, ncols=512):
            slot = wp_ctr[0] % 2
            wp_ctr[0] += 1
            dst = WP[:, slot, :, 0:ncols]
            S.dma('pool', lambda e: e.dma_start(out=dst, in_=src_ap.rearrange("(k p) n -> p k n", p=128)),
                  w=[('WP', slot)])
            return slot

        for b_ in range(NB):
            S.dma('sp', lambda e, b_=b_: e.dma_start(out=cTf[:, b_, :], in_=din['c'][b_, :].rearrange("(k p) -> p k", p=128),
                                                    allow_slow_non_contiguous=True), w=[('cTf', b_)])
        S.act(lambda e: e.activation(out=condT[:], in_=cTf[:], func=AF.Silu), r=['cTf'], w=['condT'])
        modrow = FS[0:NB, 0:6144]
        for l in range(nlayers):
            S.dma('sp', lambda e, l=l: e.dma_start(out=FS[0:NB, 0:6144], in_=din['b_mod'][l:l + 1, :].partition_broadcast(NB)),
                  w=['FS'])
            for j in range(12):
                slot = wp_load(din['w_mod'][l, :, j * 512:(j + 1) * 512])
                bank = PS[j % 2]

                def mmf(e, slot=slot, bank=bank):
                    ins = None
                    for k in range(8):
                        ins = e.matmul(bank[0:NB, :], lhsT=condT[:, :, k], rhs=WP[:, slot, k, :], start=(k == 0), stop=(k == 7))
                    return ins
                S.pe(mmf, r=[('WP', slot), 'condT'], w=['P%d' % (j % 2)])
                S.dve(lambda e, j=j, bank=bank: e.tensor_tensor(out=FS[0:NB, j * 512:(j + 1) * 512], in0=bank[0:NB, :],
                                                                 in1=FS[0:NB, j * 512:(j + 1) * 512], op=ALU.add),
                      r=['P%d' % (j % 2), ('FS', j)], w=[('FS', j)])
            S.dma('sp', lambda e, l=l: e.dma_start(out=modd[l, :, :], in_=FS[0:NB, 0:6144]), r=['FS'], w=[('modd', l)])
        S.fence('FS')
        if stage == 'mod':
            dump('modd', modd[0, :, :], [NB, 6 * D], F32, [('modd', 0)])
            S.emit(final_keys=['modd', 'dbg_modd'])
            return nc, dump_d


        fw1 = sb("fw1", [17, 64], F32)
        fw2 = sb("fw2", [64, 64], F32)
        fpar = sb("fpar", [64, 4], F32)
        KF = [KB[:, i, :].bitcast(F32) for i in range(7)]

        def layer_prep(l):
            S.dma('sp', lambda e: e.dma_start(out=n1g[:], in_=din['norm1_g'][l, :].rearrange("(k p) -> p k", p=128),
                                              allow_slow_non_contiguous=True), w=['n1g'])
            for b_ in range(NB):
                S.dma('sp', lambda e, b_=b_: e.dma_start(out=SH1T[:, b_, :], in_=modd[l, b_, 0:1024].rearrange("(k p) -> p k", p=128),
                                                        allow_slow_non_contiguous=True), r=[('modd', l)], w=[('SH1T', b_)])
                S.dma('sp', lambda e, b_=b_: e.dma_start(out=G1T[:, b_, :], in_=modd[l, b_, 1024:2048].rearrange("(k p) -> p k", p=128),
                                                        allow_slow_non_contiguous=True), r=[('modd', l)], w=[('G1T', b_)])
            S.dve(lambda e: e.scalar_tensor_tensor(out=G1T[:], in0=G1T[:], scalar=1.0, in1=n1g[:].unsqueeze(1).to_broadcast([128, NB, 8]),
                                                   op0=ALU.add, op1=ALU.mult), r=['G1T', 'n1g'], w=['G1T'])
            S.dma('sp', lambda e: e.dma_start(out=qg_bc[:], in_=din['q_norm_g'][l:l + 1, :].partition_broadcast(128)), w=['qg_bc'])
            S.dma('sp', lambda e: e.dma_start(out=kg_bc[:], in_=din['k_norm_g'][l:l + 1, :].partition_broadcast(128)), w=['kg_bc'])
            S.dma('sp', lambda e: e.dma_start(out=esink[:], in_=din['attn_sink'][l:l + 1, :].partition_broadcast(128)), w=['esink'])
            S.act(lambda e: e.activation(out=esink[:], in_=esink[:], func=AF.Exp), r=['esink'], w=['esink'])
            S.dma('pool', lambda e: e.dma_start(out=wr_sb[:], in_=din['w_router'][l].rearrange("(k p) n -> p k n", p=128)), w=['wr_sb'])

        def filter_phase(l):
            for i in range(7):
                S.fence('K%d' % i)
            S.fence('FS')
            featsT = FS[0:17, 0:2048]
            a1T = FS[0:64, 2048:4096]
            a2T = FS[0:64, 4096:6144]
            gplus = K(0, 2).rearrange("p (a b) -> p a b", a=16)
            gminus = K(2, 2).rearrange("p (a b) -> p a b", a=16)
            f3w = KF[4][0:64, 0:2048]
            absum = KF[4][:, 2048:3072]
            wint = [KF[4][:, 3072:3584], KF[4][:, 3584:4096]]
            tmpf, tmpb, tab1, tab2 = (KF[5][:, i * 512:(i + 1) * 512] for i in range(4))
            stg = [KF[5][:, 2048 + i * 512:2048 + (i + 1) * 512] for i in range(4)]
            dblk = K(6).rearrange("p (s m k j) -> p s m k j", s=2, m=2, k=16)
            S.dma('sp', lambda e: e.dma_start(out=featsT, in_=din['k_featsT'][:, :]), w=[('FS', 'feats')])
            S.dma('sp', lambda e: e.dma_start(out=fw1[:], in_=din['hy_f1_w'][l, :, :]), w=['fw1'])
            S.dma('sp', lambda e: e.dma_start(out=fw2[:], in_=din['hy_f2_w'][l, :, :]), w=['fw2'])
            for j, nm in enumerate(('hy_f1_b', 'hy_f1_freq', 'hy_f2_b', 'hy_f2_freq')):
                S.dma('sp', lambda e, j=j, nm=nm: e.dma_start(out=fpar[:, j:j + 1], in_=din[nm][l, :].rearrange("(p o) -> p o", o=1)),
                      w=[('fpar', j)])
            S.dma('sp', lambda e: e.dma_start(out=f3w, in_=din['hy_f3_w'][l, :, :]), w=[('K4', 'f3w')])
            S.pool(lambda e: e.memset(absum, 0.0), w=[('K4', 'absum')])

            def sin_mlp(wt, kdim, src, dst, pb, pf, lname):
                for nb in range(4):
                    cs = slice(nb * 512, (nb + 1) * 512)
                    bank = PS[nb % 2]
                    bk = 'P%d' % (nb % 2)
                    S.pe(lambda e, bank=bank, cs=cs: e.matmul(bank[0:64, :], lhsT=wt[0:kdim, :], rhs=src[0:kdim, cs], start=True, stop=True),
                         r=[lname, ('FS', lname + 'src')], w=[bk])
                    S.dve(lambda e, bank=bank, cs=cs: e.tensor_scalar(out=dst[:, cs], in0=bank[0:64, :], scalar1=fpar[:, pb:pb + 1],
                                                                       scalar2=fpar[:, pf:pf + 1], op0=ALU.add, op1=ALU.mult),
                          r=[bk, 'fpar'], w=[('FS', lname + 'dst%d' % nb)])
                    t1 = tmpf[0:64, :]
                    t2 = tmpb[0:64, :]
                    S.act(lambda e, cs=cs: e.activation(out=t1, in_=dst[:, cs], func=AF.Sin, scale=1.0 / 3), r=[('FS', lname + 'dst%d' % nb)], w=[('K5', 't1')])
                    S.act(lambda e: e.activation(out=t2, in_=t1, func=AF.Square), r=[('K5', 't1')], w=[('K5', 't2')])
                    S.dve(lambda e: e.tensor_scalar(out=t2, in0=t2, scalar1=-4.0, scalar2=3.0, op0=ALU.mult, op1=ALU.add), r=[('K5', 't2')], w=[('K5', 't2')])
                    S.dve(lambda e, cs=cs: e.tensor_tensor(out=dst[:, cs], in0=t1, in1=t2, op=ALU.mult), r=[('K5', 't1'), ('K5', 't2')],
                          w=[('FS', lname + 'dst%d' % nb)])
            S.fence('FS')
            sin_mlp(fw1, 17, featsT, a1T, 0, 1, 'fw1')
            S.fence('FS')
            sin_mlp(fw2, 64, a1T, a2T, 2, 3, 'fw2')
            S.fence('FS')
            S.fence('K5')
            if stage == 'f1':
                dump('a2T', FS[0:64, 4096:6144], [64, 2048], F32, ['FS'])
                return
            for tt in range(16):
                wt = wint[tt % 2]
                wk = ('K4', 'win%d' % (tt % 2))
                S.dma('sp', lambda e, wt=wt, tt=tt: e.dma_start(out=wt, in_=din['k_win'][:, tt, :]), w=[wk])
                for cb in range(4):
                    S.pe(lambda e, cb=cb, tt=tt: e.matmul(PS[cb][:, :], lhsT=a2T[:, tt * 128:(tt + 1) * 128], rhs=f3w[:, cb * 512:(cb + 1) * 512],
                                                          start=True, stop=True), r=['FS', ('K4', 'f3w')], w=['P%d' % cb])
                for o in range(2):
                    S.dve(lambda e, o=o, wt=wt: e.tensor_tensor(out=tmpf, in0=PS[o][:, :], in1=wt, op=ALU.mult), r=['P%d' % o, wk], w=[('K5', 'tf')])
                    S.dve(lambda e, o=o, wt=wt: e.tensor_tensor(out=tmpb, in0=PS[2 + o][:, :], in1=wt, op=ALU.mult), r=['P%d' % (2 + o), wk], w=[('K5', 'tb')])
                    if tt == 0:
                        S.dve(lambda e: e.memset(tmpb[0:1, :], 0.0), r=[('K5', 'tb')], w=[('K5', 'tb')])
                    oc = slice(o * 512, (o + 1) * 512)
                    S.dve(lambda e, tt=tt, oc=oc: e.tensor_tensor(out=gplus[:, tt, oc], in0=tmpf, in1=tmpb, op=ALU.add),
                          r=[('K5', 'tf'), ('K5', 'tb')], w=[('K0', tt * 2 + o), ('K1', tt * 2 + o)])
                    S.dve(lambda e, tt=tt, oc=oc: e.tensor_tensor(out=gminus[:, tt, oc], in0=tmpf, in1=tmpb, op=ALU.subtract),
                          r=[('K5', 'tf'), ('K5', 'tb')], w=[('K2', tt * 2 + o), ('K3', tt * 2 + o)])
                    S.act(lambda e: e.activation(out=tab1, in_=tmpf, func=AF.Abs), r=[('K5', 'tf')], w=[('K5', 'a1')])
                    S.act(lambda e: e.activation(out=tab2, in_=tmpb, func=AF.Abs), r=[('K5', 'tb')], w=[('K5', 'a2')])
                    S.pool(lambda e: e.tensor_tensor(out=tab1, in0=tab1, in1=tab2, op=ALU.add), r=[('K5', 'a1'), ('K5', 'a2')], w=[('K5', 'a1')])
                    S.pool(lambda e, oc=oc: e.tensor_tensor(out=absum[:, oc], in0=absum[:, oc], in1=tab1, op=ALU.add),
                           r=[('K5', 'a1'), ('K4', 'absum')], w=[('K4', 'absum')])
            S.fence('FS')
            rn_bc = FS[:, 0:1024]
            hb_bc = FS[:, 1024:2048]
            S.dma('sp', lambda e: e.dma_start(out=hb_bc, in_=din['hy_bias'][l:l + 1, :, :].rearrange("a o c -> a (o c)").partition_broadcast(128)),
                  w=[('FS', 'hb_bc')])
            S.dve(lambda e: e.tensor_scalar(out=hb_bc, in0=hb_bc, scalar1=2.0 / (2 * T), scalar2=None, op0=ALU.mult), r=[('FS', 'hb_bc')], w=[('FS', 'hb_bc')])
            for o in range(2):
                oc = slice(o * 512, (o + 1) * 512)
                S.pe(lambda e, o=o, oc=oc: e.matmul(PS[4 + o][:, :], lhsT=onesf[:], rhs=absum[:, oc], start=True, stop=True),
                     r=[('K4', 'absum'), 'onesf'], w=['P%d' % (4 + o)])
                S.dve(lambda e, o=o, oc=oc: e.reciprocal(out=rn_bc[:, oc], in_=PS[4 + o][:, :]), r=['P%d' % (4 + o)], w=[('FS', ('rn', o))])
                S.dve(lambda e, oc=oc: e.tensor_scalar(out=rn_bc[:, oc], in0=rn_bc[:, oc], scalar1=2.0 / (2 * T), scalar2=None, op0=ALU.mult),
                      r=[('FS', ('rn', o))], w=[('FS', ('rn', o))])
            for i in range(6):
                S.fence('K%d' % i)
            if stage == 'f2':
                dump('rn', FS[:, 0:2048], [128, 2048], F32, ['FS'])
                dump('gplus', K(0, 2), [128, 16384], BF16, ['K0', 'K1'])
                return
            def fl_load(ft):
                sl = ft % 2
                S.dma('sp', lambda e: e.dma_start(out=dblk[:, sl, 0, :, :], in_=din['k_ci'][ft, :, :, :]), w=[('K6', (sl, 0))])
                S.dma('sp', lambda e: e.dma_start(out=dblk[:, sl, 1, :, :], in_=din['k_si'][ft, :, :, :]), w=[('K6', (sl, 1))])
            fl_load(0)
            for ft in range(16):
                sl = ft % 2
                if ft + 1 < 16:
                    fl_load(ft + 1)
                for o in range(2):
                    oc = slice(o * 512, (o + 1) * 512)
                    for m, gsrc, gk, gk2 in ((0, gplus, 'K0', 'K1'), (1, gminus, 'K2', 'K3')):
                        bank = PS[m * 2 + o]

                        def mmf(e, bank=bank, sl=sl, m=m, gsrc=gsrc, oc=oc):
                            ins = None
                            for jc in range(16):
                                ins = e.matmul(bank[:, :], lhsT=dblk[:, sl, m, jc, :], rhs=gsrc[:, jc, oc], start=(jc == 0), stop=(jc == 15))
                            return ins
                        S.pe(mmf, r=[('K6', (sl, m)), gk, gk2], w=['P%d' % (m * 2 + o)])
                        st_ = stg[m * 2 + o]
                        sk = ('K5', 'stg%d' % (m * 2 + o))
                        S.dve(lambda e, bank=bank, st_=st_, oc=oc: e.tensor_tensor(out=st_, in0=bank[:, :], in1=rn_bc[:, oc], op=ALU.mult),
                              r=['P%d' % (m * 2 + o), ('FS', ('rn', o))], w=[sk])
                        if m == 0:
                            S.dve(lambda e, st_=st_, oc=oc: e.tensor_tensor(out=st_, in0=st_, in1=hb_bc[:, oc], op=ALU.add), r=[sk, ('FS', 'hb_bc')], w=[sk])
                        S.dma('sp', lambda e, st_=st_, m=m, o=o, ft=ft: e.dma_start(out=pqd[m, o, ft * 128:(ft + 1) * 128, :], in_=st_),
                              r=[sk], w=[('pqd', (m, o, ft))])
            for i in range(7):
                S.fence('K%d' % i)
            S.fence('FS')

        def mixer(l, b, xin, xin_key, xout, xout_key):
            XIO = [FS[:, 0:1024], FS[:, 1024:2048]]
            TMP = FS[:, 2048:3072]
            w_in = din['w_in'][l]
            TMPs = [FS[:, 2048:3072], FS[:, 3072:4096]]
            def ht_tile(tt):
                p_ = tt % 2
                xt = XIO[p_]
                xk = ('FS', 'xio%d' % p_)
                tmp_ = TMPs[p_]
                tk = ('FS', 'tmp%d' % p_)
                c_ssq = small[:, 4 * p_:4 * p_ + 1]
                c_rstd = small[:, 4 * p_ + 1:4 * p_ + 2]
                sk0 = ('small', ('a0', p_))
                sk1 = ('small', ('a1', p_))
                S.dma('sp', lambda e, xt=xt, tt=tt: e.dma_start(out=xt, in_=xin[b, tt * 128:(tt + 1) * 128, :]), r=[xin_key], w=[xk])
                S.act(lambda e, xt=xt, tmp_=tmp_, c_ssq=c_ssq: e.activation(out=tmp_, in_=xt, func=AF.Square, accum_out=c_ssq), r=[xk], w=[tk, sk0])
                S.act(lambda e, c_ssq=c_ssq, c_rstd=c_rstd: e.activation(out=c_rstd, in_=c_ssq, func=AF.Sqrt, scale=1.0 / D, bias=epsb[:, 0:1]),
                      r=[sk0, 'epsb'], w=[sk1])
                S.dve(lambda e, c_rstd=c_rstd: e.reciprocal(out=c_rstd, in_=c_rstd), r=[sk1], w=[sk1])
                xn = tmp_.bitcast(BF16)[:, 0:1024]
                S.dve(lambda e, xt=xt, xn=xn, c_rstd=c_rstd: e.tensor_scalar(out=xn, in0=xt, scalar1=c_rstd, scalar2=None, op0=ALU.mult),
                      r=[xk, sk1, tk], w=[tk])
                pt, ptk = nextpt()

                def trf(e, xn=xn, pt=pt):
                    ins = None
                    for k in range(8):
                        ins = e.transpose(out=pt[:, k, :], in_=xn[:, k * 128:(k + 1) * 128], identity=idb[:])
                    return ins
                S.pe(trf, r=[tk, 'idb'], w=[ptk])
                for k in range(8):
                    if tt % 2 == 0:
                        S.act(lambda e, k=k, tt=tt, pt=pt: e.activation(out=HT[:, k, 1 + tt * 128:1 + (tt + 1) * 128], in_=pt[:, k, :], func=AF.Identity,
                                                                        scale=G1T[:, b, k:k + 1], bias=SH1T[:, b, k:k + 1]),
                              r=[ptk, 'G1T', 'SH1T'], w=[('HT', (tt, k))])
                    else:
                        S.dve(lambda e, k=k, tt=tt, pt=pt: e.tensor_scalar(out=HT[:, k, 1 + tt * 128:1 + (tt + 1) * 128], in0=pt[:, k, :],
                                                                           scalar1=G1T[:, b, k:k + 1], scalar2=SH1T[:, b, k:k + 1], op0=ALU.mult, op1=ALU.add),
                              r=[ptk, 'G1T', 'SH1T'], w=[('HT', (tt, k))])
            S.interleave([lambda tt=tt: ht_tile(tt) for tt in range(16)], 2, 'ht')
            S.fence('HT')
            S.fence('FS')
            if stage == 'ht':
                return

            hy_tm = [K(0).rearrange("p (a c) -> p a c", a=16), K(1).rearrange("p (a c) -> p a c", a=16), K(2).rearrange("p (a c) -> p a c", a=16)]
            zTs = [K(3)[:, 0:T + 2], K(3)[:, 4096:4096 + T + 2]]
            cTs = [K(4)[:, 0:T], K(4)[:, 4096:4096 + T]]
            cwT = FS[:, 3072:3108].rearrange("p (j s) -> p j s", j=3)
            cbT = FS[:, 3112:3124]
            for i in (3, 4):
                S.fence('K%d' % i)
            S.dma('sp', lambda e: e.dma_start(out=cwT, in_=din['hy_conv_w'][l, :, :].rearrange("j (s p) -> p j s", p=128), allow_slow_non_contiguous=True),
                  w=[('FS', 'cwT')])
            S.dma('sp', lambda e: e.dma_start(out=cbT, in_=din['hy_conv_b'][l, :].rearrange("(s p) -> p s", p=128), allow_slow_non_contiguous=True),
                  w=[('FS', 'cbT')])
            for p_ in range(2):
                S.dve(lambda e, p_=p_: e.memset(zTs[p_][:, 0:1], 0.0), w=[('K3', ('pad0', p_))])
                S.dve(lambda e, p_=p_: e.memset(zTs[p_][:, T + 1:T + 2], 0.0), w=[('K3', ('pad1', p_))])
            slots = [wp_load(w_in[:, seg * 512:(seg + 1) * 512]) if seg < 2 else None for seg in range(3)]

            def hy_chain(sc):
                seg, cc = sc // 4, sc % 4
                p_ = sc % 2
                zT = zTs[p_]
                cT = cTs[p_]
                zk = ('K3', ('z', p_))
                ck = ('K4', ('c', p_))
                slot = slots[seg]
                for tb in range(4):
                    bank = PS[2 * p_ + tb % 2]
                    bk = 'P%d' % (2 * p_ + tb % 2)

                    def mmf(e, bank=bank, tb=tb):
                        ins = None
                        for k in range(8):
                            ins = e.matmul(bank[:, :], lhsT=WP[:, slot, k, cc * 128:(cc + 1) * 128], rhs=HT[:, k, 1 + tb * 512:1 + (tb + 1) * 512],
                                           start=(k == 0), stop=(k == 7))
                        return ins
                    S.pe(mmf, r=[('WP', slot), 'HT'], w=[bk])
                    S.act(lambda e, bank=bank, tb=tb: e.activation(out=zT[:, 1 + tb * 512:1 + (tb + 1) * 512], in_=bank[:, :], func=AF.Copy),
                          r=[bk], w=[('K3', ('z', p_, tb))])
                zr = [('K3', ('z', p_, tb)) for tb in range(4)] + [('K3', ('pad0', p_)), ('K3', ('pad1', p_))]
                S.dve(lambda e: e.tensor_scalar(out=cT, in0=zT[:, 1:T + 1], scalar1=cwT[:, 1, sc:sc + 1], scalar2=cbT[:, sc:sc + 1], op0=ALU.mult, op1=ALU.add),
                      r=zr + [('FS', 'cwT'), ('FS', 'cbT')], w=[ck])
                S.dve(lambda e: e.scalar_tensor_tensor(out=cT, in0=zT[:, 0:T], scalar=cwT[:, 0, sc:sc + 1], in1=cT, op0=ALU.mult, op1=ALU.add),
                      r=zr + [('FS', 'cwT'), ck], w=[ck])
                S.dve(lambda e: e.scalar_tensor_tensor(out=cT, in0=zT[:, 2:T + 2], scalar=cwT[:, 2, sc:sc + 1], in1=cT, op0=ALU.mult, op1=ALU.add),
                      r=zr + [('FS', 'cwT'), ck], w=[ck])
                for hb in range(2):
                    pt, ptk = PTs[p_], 'PT%d' % p_

                    def trf(e, pt=pt, hb=hb):
                        ins = None
                        for j in range(8):
                            tt = hb * 8 + j
                            ins = e.transpose(out=pt[:, j, :], in_=cT[:, tt * 128:(tt + 1) * 128], identity=idb[:])
                        return ins
                    S.pe(trf, r=[ck, 'idb'], w=[ptk])
                    dst = hy_tm[seg][:, hb * 8:(hb + 1) * 8, cc * 128:(cc + 1) * 128]
                    if hb == 0:
                        S.act(lambda e, pt=pt, dst=dst: e.activation(out=dst, in_=pt[:, :, :], func=AF.Copy), r=[ptk], w=[('K%d' % seg, (cc, hb))])
                    else:
                        S.dve(lambda e, pt=pt, dst=dst: e.tensor_copy(out=dst, in_=pt[:, :, :]), r=[ptk], w=[('K%d' % seg, (cc, hb))])
            S.interleave([lambda sc=sc: hy_chain(sc) for sc in range(8)], 2, 'hy')
            slots[2] = wp_load(w_in[:, 1024:1536])
            S.interleave([lambda sc=sc: hy_chain(sc) for sc in range(8, 12)], 2, 'hy')
            for i in range(3):
                S.fence('K%d' % i)
            for i in (3, 4, 5, 6):
                S.fence('K%d' % i)
            S.fence('FS')
            if stage == 'hyproj':
                dump('v', K(0), [128, 8192], BF16, ['K0'])
                dump('x1', K(1), [128, 8192], BF16, ['K1'])
                dump('x2', K(2), [128, 8192], BF16, ['K2'])
                return

            Wr = K(3).rearrange("p (a c) -> p a c", a=16)
            Wi = K(4).rearrange("p (a c) -> p a c", a=16)
            dblk = K(5).rearrange("p (s m k j) -> p s m k j", s=2, m=2, k=16)
            pqs = FS[:, 3072:5120].rearrange("p (s m c) -> p s m c", s=2, m=2)
            t1 = FS[:, 0:512]
            t2 = FS[:, 512:1024]
            blk_ctr = [0]

            def load_blk(i):
                sl = blk_ctr[0] % 2
                blk_ctr[0] += 1
                S.dma('sp', lambda e: e.dma_start(out=dblk[:, sl, 0, :, :], in_=din['k_c2'][i, :, :, :]), w=[('K5', (sl, 0))])
                S.dma('sp', lambda e: e.dma_start(out=dblk[:, sl, 1, :, :], in_=din['k_s2'][i, :, :, :]), w=[('K5', (sl, 1))])
                return sl
            for s_ in range(2):
                u = hy_tm[0]
                gsrc = hy_tm[1 + s_]
                for ft in range(16):
                    sl = load_blk(ft)
                    ps_ = ft % 2
                    for m in range(2):
                        S.dma('sp', lambda e, m=m, ft=ft, ps_=ps_, s_=s_: e.dma_start(out=pqs[:, ps_, m, :], in_=pqd[m, s_, ft * 128:(ft + 1) * 128, :]),
                              r=[('pqd', (m, s_, ft))], w=[('FS', ('pq', ps_, m))])
                    bA = PS[(ft % 2) * 2]
                    bB = PS[(ft % 2) * 2 + 1]
                    kA = 'P%d' % ((ft % 2) * 2)
                    kB = 'P%d' % ((ft % 2) * 2 + 1)
                    for m, bank, bk in ((0, bA, kA), (1, bB, kB)):
                        def mmf(e, bank=bank, sl=sl, m=m):
                            ins = None
                            for tc in range(16):
                                ins = e.matmul(bank[:, :], lhsT=dblk[:, sl, m, tc, :], rhs=u[:, tc, :], start=(tc == 0), stop=(tc == 15))
                            return ins
                        S.pe(mmf, r=[('K5', (sl, m)), 'K0'], w=[bk])
                    Pt = pqs[:, ps_, 0, :]
                    Qt = pqs[:, ps_, 1, :]
                    pk = ('FS', ('pq', ps_, 0))
                    qk = ('FS', ('pq', ps_, 1))
                    S.dve(lambda e, bA=bA, Pt=Pt: e.tensor_tensor(out=t1, in0=bA[:, :], in1=Pt, op=ALU.mult), r=[kA, pk], w=[('FS', 't1')])
                    S.dve(lambda e, bB=bB, Qt=Qt: e.tensor_tensor(out=t2, in0=bB[:, :], in1=Qt, op=ALU.mult), r=[kB, qk], w=[('FS', 't2')])
                    S.pool(lambda e, ft=ft: e.tensor_tensor(out=Wr[:, ft, :], in0=t1, in1=t2, op=ALU.add), r=[('FS', 't1'), ('FS', 't2')], w=[('K3', ft)])
                    t3 = FS[:, 1024:1536]
                    t4 = FS[:, 1536:2048]
                    S.dve(lambda e, bB=bB, Pt=Pt, t3=t3: e.tensor_tensor(out=t3, in0=bB[:, :], in1=Pt, op=ALU.mult), r=[kB, pk], w=[('FS', 't3')])
                    S.dve(lambda e, bA=bA, Qt=Qt, t4=t4: e.tensor_tensor(out=t4, in0=bA[:, :], in1=Qt, op=ALU.mult), r=[kA, qk], w=[('FS', 't4')])
                    S.pool(lambda e, ft=ft, t3=t3, t4=t4: e.tensor_tensor(out=Wi[:, ft, :], in0=t3, in1=t4, op=ALU.subtract),
                           r=[('FS', 't3'), ('FS', 't4')], w=[('K4', ft)])
                S.fence('K3')
                S.fence('K4')
                S.fence('K0')
                for tt in range(16):
                    sl = load_blk(tt)
                    bank = PS[4 + tt % 2]
                    bk = 'P%d' % (4 + tt % 2)

                    def mmf(e, bank=bank, sl=sl):
                        ins = None
                        for fc in range(16):
                            e.matmul(bank[:, :], lhsT=dblk[:, sl, 0, fc, :], rhs=Wr[:, fc, :], start=(fc == 0), stop=False)
                            ins = e.matmul(bank[:, :], lhsT=dblk[:, sl, 1, fc, :], rhs=Wi[:, fc, :], start=False, stop=(fc == 15))
                        return ins
                    S.pe(mmf, r=[('K5', (sl, 0)), ('K5', (sl, 1)), 'K3', 'K4'], w=[bk])
                    S.dve(lambda e, bank=bank, tt=tt, gsrc=gsrc: e.tensor_tensor(out=u[:, tt, :], in0=bank[:, :], in1=gsrc[:, tt, :], op=ALU.mult),
                          r=[bk, ('K%d' % (1 + s_), tt)], w=[('K0', tt)])
                S.fence('K0')
            y_hyT = K(3).rearrange("p (a t) -> p a t", a=4)
            S.fence('K3')
            for tt in range(16):
                pt, ptk = nextpt()

                def trf(e, tt=tt, pt=pt):
                    ins = None
                    for cc in range(4):
                        ins = e.transpose(out=pt[:, cc, :], in_=hy_tm[0][:, tt, cc * 128:(cc + 1) * 128], identity=idb[:])
                    return ins
                S.pe(trf, r=['K0', 'idb'], w=[ptk])
                S.act(lambda e, tt=tt, pt=pt: e.activation(out=y_hyT[:, :, tt * 128:(tt + 1) * 128], in_=pt[:, 0:4, :], func=AF.Copy),
                      r=[ptk], w=[('K3', tt)])
            S.fence('K3')
            S.fence('FS')
            if stage == 'hyena':
                dump('yhy', K(3), [128, 8192], BF16, ['K3'])
                return

            uT = K(0).rearrange("p (a t) -> p a t", a=4)
            vln = K(1).rearrange("p (a c) -> p a c", a=16)
            y_gmT = K(4).rearrange("p (a t) -> p a t", a=4)
            wsT = K(2).rearrange("p (g q) -> p g q", g=64)[:, 0:4, :]
            lng_bc = FS[:, 3072:3584]
            lnb_bc = FS[:, 3584:4096]
            gmb_bc = FS[:, 4096:4608]
            gt = [FS[:, 512 + i * 512:1024 + i * 512] for i in range(4)]
            wsf = FS[:, 0:512].rearrange("p (g q) -> p g q", g=4)
            for i in (0, 1, 2, 4):
                S.fence('K%d' % i)
            S.dma('sp', lambda e: e.dma_start(out=lng_bc, in_=din['gm_ln_g'][l:l + 1, :].partition_broadcast(128)), w=[('FS', 'lng')])
            S.dma('sp', lambda e: e.dma_start(out=lnb_bc, in_=din['gm_ln_b'][l:l + 1, :].partition_broadcast(128)), w=[('FS', 'lnb')])
            S.dma('sp', lambda e: e.dma_start(out=gmb_bc, in_=din['gm_b'][l:l + 1, :, :].rearrange("a g p -> a (g p)").partition_broadcast(128)),
                  w=[('FS', 'gmb')])
            S.dma('sp', lambda e: e.dma_start(out=wsf, in_=din['gm_ws'][l].rearrange("g p q -> p g q")), w=[('FS', 'wsf')])
            for g in range(4):
                S.pe(lambda e, g=g: e.transpose(out=PS[5][:, g * 128:(g + 1) * 128], in_=wsf[:, g, :], identity=idf[:]), r=[('FS', 'wsf'), 'idf'], w=['P5'])
            S.act(lambda e: e.activation(out=wsT, in_=PS[5][:, :].rearrange("p (g q) -> p g q", g=4), func=AF.Copy), r=['P5'], w=[('K2', 'wsT')])
            slot = wp_load(w_in[:, COL_GU:COL_GU + 512])
            for cc in range(4):
                for tb in range(4):
                    bank = PS[(cc * 4 + tb) % 4]
                    bk = 'P%d' % ((cc * 4 + tb) % 4)

                    def mmf(e, bank=bank, cc=cc, tb=tb, slot=slot):
                        ins = None
                        for k in range(8):
                            ins = e.matmul(bank[:, :], lhsT=WP[:, slot, k, cc * 128:(cc + 1) * 128], rhs=HT[:, k, 1 + tb * 512:1 + (tb + 1) * 512],
                                           start=(k == 0), stop=(k == 7))
                        return ins
                    S.pe(mmf, r=[('WP', slot), 'HT'], w=[bk])
                    S.act(lambda e, bank=bank, cc=cc, tb=tb: e.activation(out=uT[:, cc, tb * 512:(tb + 1) * 512], in_=bank[:, :], func=AF.Gelu),
                          r=[bk], w=[('K0', (cc, tb))])
            slot = wp_load(w_in[:, COL_GV:COL_GV + 512])
            def gv_tile(tt):
                bank = PS[tt % 4]
                bk = 'P%d' % (tt % 4)
                g_ = gt[tt % 4]
                gk = ('FS', 'g%d' % (tt % 4))

                def mmf(e, bank=bank, tt=tt, slot=slot):
                    ins = None
                    for k in range(8):
                        ins = e.matmul(bank[:, :], lhsT=HT[:, k, 1 + tt * 128:1 + (tt + 1) * 128], rhs=WP[:, slot, k, :], start=(k == 0), stop=(k == 7))
                    return ins
                S.pe(mmf, r=[('WP', slot), 'HT'], w=[bk])
                S.act(lambda e, bank=bank, g_=g_: e.activation(out=g_, in_=bank[:, :], func=AF.Gelu), r=[bk], w=[gk])
                p_ = tt % 4
                bn6 = small[:, 64 + 8 * p_:64 + 8 * p_ + 6]
                mv_ = small[:, 128 + 2 * p_:128 + 2 * p_ + 2]
                bnk = ('small', ('bn', p_))
                mvk = ('small', ('mv', p_))
                S.dve(lambda e, g_=g_, bn6=bn6: e.bn_stats(out=bn6, in_=g_), r=[gk], w=[bnk])
                S.dve(lambda e, bn6=bn6, mv_=mv_: e.bn_aggr(out=mv_, in_=bn6), r=[bnk], w=[mvk])
                S.act(lambda e, mv_=mv_: e.activation(out=mv_[:, 1:2], in_=mv_[:, 1:2], func=AF.Sqrt, bias=epsb[:, 0:1]), r=[mvk, 'epsb'], w=[mvk])
                S.dve(lambda e, mv_=mv_: e.reciprocal(out=mv_[:, 1:2], in_=mv_[:, 1:2]), r=[mvk], w=[mvk])
                S.dve(lambda e, g_=g_, mv_=mv_: e.scalar_tensor_tensor(out=g_, in0=g_, scalar=mv_[:, 0:1], in1=lng_bc, op0=ALU.subtract, op1=ALU.mult),
                      r=[gk, mvk, ('FS', 'lng')], w=[gk])
                S.dve(lambda e, g_=g_, mv_=mv_, tt=tt: e.scalar_tensor_tensor(out=vln[:, tt, :], in0=g_, scalar=mv_[:, 1:2], in1=lnb_bc, op0=ALU.mult, op1=ALU.add),
                      r=[gk, mvk, ('FS', 'lnb')], w=[('K1', tt)])
            S.interleave([lambda tt=tt: gv_tile(tt) for tt in range(16)], 4, 'gv')

            def sp_tile(n):
                bank = PS[4 + n % 2]
                bk = 'P%d' % (4 + n % 2)

                def mmf(e, bank=bank, n=n):
                    ins = None
                    for g in range(4):
                        ins = e.matmul(bank[:, g * 128:(g + 1) * 128], lhsT=vln[:, n, g * 128:(g + 1) * 128], rhs=wsT[:, g, :], start=True, stop=True)
                    return ins
                S.pe(mmf, r=[('K1', n), ('K2', 'wsT')], w=[bk])
                g_ = gt[n % 4]
                gk = ('FS', 'g%d' % (n % 4))
                S.dve(lambda e, bank=bank, g_=g_: e.tensor_tensor(out=g_, in0=bank[:, :], in1=gmb_bc, op=ALU.add), r=[bk, ('FS', 'gmb')], w=[gk])
                S.dve(lambda e, g_=g_, n=n: e.tensor_tensor(out=y_gmT[:, :, n * 128:(n + 1) * 128], in0=g_.rearrange("p (g q) -> p g q", g=4),
                                                           in1=uT[:, :, n * 128:(n + 1) * 128], op=ALU.mult), r=[gk, 'K0'], w=[('K4', n)])
            S.interleave([lambda n=n: sp_tile(n) for n in range(16)], 2, 'sp')
            for i in (0, 1, 2, 4):
                S.fence('K%d' % i)
            S.fence('FS')
            if stage == 'gmlp':
                dump('ygm', K(4), [128, 8192], BF16, ['K4'])
                return

            QT = K(0).rearrange("p (j t) -> p j t", j=4)
            KT = K(1)[:, 0:2048]
            Vat = K(1)[:, 2048:4096].rearrange("p (a c) -> p a c", a=16)
            y_atT = K(5, 2).rearrange("p (h t) -> p h t", h=8)
            for i in (0, 1, 5, 6):
                S.fence('K%d' % i)
            slotq = wp_ctr[0] % 2
            wp_ctr[0] += 1
            for j in range(4):
                for a_ in range(2):
                    hc = COL_Q + (a_ * 4 + j) * 64
                    S.dma('pool', lambda e, j=j, a_=a_, hc=hc: e.dma_start(out=WP[:, slotq, :, j * 128 + a_ * 64:j * 128 + (a_ + 1) * 64],
                                                                          in_=w_in[:, hc:hc + 64].rearrange("(k p) d -> p k d", p=128)),
                          w=[('WP', slotq)])
            slotk = wp_load(w_in[:, COL_K:COL_K + 256], 256)

            def qk_norm_rope(bank, bk, nh, gbc, tt, p_):
                w_ = nh * 64
                h_ = nh * 32
                o_ = p_ * 2304 if p_ < 2 else 4608 + (p_ - 2) * 576
                sq = FS[:, o_:o_ + w_]
                qn = FS[:, o_ + w_:o_ + 2 * w_]
                qr = FS[:, o_ + 2 * w_:o_ + 2 * w_ + h_].bitcast(BF16)
                tA, tB, tC, tD = (FS[:, o_ + 2 * w_ + h_ + i * h_:o_ + 2 * w_ + h_ + (i + 1) * h_] for i in range(4))
                st_ = small[:, 96 + 8 * p_:96 + 8 * p_ + nh]
                kq = lambda n_: ('FS', (n_, p_))
                sk = ('small', ('qs', p_))
                S.act(lambda e: e.activation(out=sq[:, 0:w_], in_=bank[:, 0:w_], func=AF.Square), r=[bk], w=[kq('sq')])
                S.dve(lambda e: e.tensor_reduce(out=st_, in_=sq[:, 0:w_].rearrange("p (h d) -> p h d", h=nh), axis=AX.X, op=ALU.add),
                      r=[kq('sq')], w=[sk])
                S.act(lambda e: e.activation(out=st_, in_=st_, func=AF.Sqrt, scale=1.0 / 64, bias=epsb[:, 0:1]), r=[sk, 'epsb'], w=[sk])
                S.dve(lambda e: e.reciprocal(out=st_, in_=st_), r=[sk], w=[sk])
                q3 = qn[:, 0:w_].rearrange("p (h d) -> p h d", h=nh)
                S.dve(lambda e: e.tensor_tensor(out=q3, in0=bank[:, 0:w_].rearrange("p (h d) -> p h d", h=nh),
                                                in1=st_.unsqueeze(2).to_broadcast([128, nh, 64]), op=ALU.mult),
                      r=[bk, sk], w=[kq('qn')])
                S.pool(lambda e: e.tensor_tensor(out=q3, in0=q3, in1=gbc[:].unsqueeze(1).to_broadcast([128, nh, 64]), op=ALU.mult),
                       r=[kq('qn'), 'qg_bc', 'kg_bc'], w=[kq('qn')])
                cb_ = ropec[:, tt, :].unsqueeze(1).to_broadcast([128, nh, 32])
                sb_ = ropes[:, tt, :].unsqueeze(1).to_broadcast([128, nh, 32])
                x1_ = q3[:, :, 0:32]
                x2_ = q3[:, :, 32:64]
                r3 = qr[:, 0:w_].rearrange("p (h d) -> p h d", h=nh)
                v = lambda t_: t_[:, 0:nh * 32].rearrange("p (h d) -> p h d", h=nh)
                S.dve(lambda e: e.tensor_tensor(out=v(tA), in0=x1_, in1=cb_, op=ALU.mult), r=[kq('qn'), 'ropec'], w=[kq('tA')])
                S.pool(lambda e: e.tensor_tensor(out=v(tB), in0=x2_, in1=sb_, op=ALU.mult), r=[kq('qn'), 'ropes'], w=[kq('tB')])
                S.dve(lambda e: e.tensor_tensor(out=v(tC), in0=x2_, in1=cb_, op=ALU.mult), r=[kq('qn'), 'ropec'], w=[kq('tC')])
                S.pool(lambda e: e.tensor_tensor(out=v(tD), in0=x1_, in1=sb_, op=ALU.mult), r=[kq('qn'), 'ropes'], w=[kq('tD')])
                S.dve(lambda e: e.tensor_tensor(out=r3[:, :, 0:32], in0=v(tA), in1=v(tB), op=ALU.subtract), r=[kq('tA'), kq('tB')], w=[kq('qr')])
                S.pool(lambda e: e.tensor_tensor(out=r3[:, :, 32:64], in0=v(tC), in1=v(tD), op=ALU.add), r=[kq('tC'), kq('tD')], w=[kq('qr2')])
                return qr, [kq('qr'), kq('qr2')]

            def q_tile(tt):
                bank = PS[tt % 2]
                bk = 'P%d' % (tt % 2)
                pt = PTs[tt % 2]
                ptn = 'PT%d' % (tt % 2)

                def mmf(e, bank=bank, tt=tt):
                    ins = None
                    for k in range(8):
                        ins = e.matmul(bank[:, :], lhsT=HT[:, k, 1 + tt * 128:1 + (tt + 1) * 128], rhs=WP[:, slotq, k, :], start=(k == 0), stop=(k == 7))
                    return ins
                S.pe(mmf, r=[('WP', slotq), 'HT'], w=[bk])
                qr, qrk = qk_norm_rope(bank, bk, 8, qg_bc, tt, tt % 2)

                def trf(e, qr=qr, pt=pt):
                    ins = None
                    for j in range(4):
                        ins = e.transpose(out=pt[:, j, :], in_=qr[:, j * 128:(j + 1) * 128], identity=idb[:])
                    return ins
                S.pe(trf, r=qrk + ['idb'], w=[ptn])
                S.act(lambda e, tt=tt, pt=pt: e.activation(out=QT[:, :, tt * 128:(tt + 1) * 128], in_=pt[:, 0:4, :], func=AF.Copy),
                      r=[ptn], w=[('K0', tt)])

            def k_tile(tt):
                pt = PTs[(tt + 1) % 2]
                ptn = 'PT%d' % ((tt + 1) % 2)
                bank2 = PS[2 + tt % 2]
                bk2 = 'P%d' % (2 + tt % 2)

                def mmf2(e, bank2=bank2, tt=tt):
                    ins = None
                    for k in range(8):
                        ins = e.matmul(bank2[:, 0:256], lhsT=HT[:, k, 1 + tt * 128:1 + (tt + 1) * 128], rhs=WP[:, slotk, k, 0:256], start=(k == 0), stop=(k == 7))
                    return ins
                S.pe(mmf2, r=[('WP', slotk), 'HT'], w=[bk2])
                S.act(lambda e, bank2=bank2, tt=tt: e.activation(out=Vat[:, tt, :], in_=bank2[:, 128:256], func=AF.Copy), r=[bk2], w=[('K1', ('v', tt))])
                kr, krk = qk_norm_rope(bank2, bk2, 2, kg_bc, tt, 2 + tt % 2)
                S.pe(lambda e, kr=kr, pt=pt: e.transpose(out=pt[:, 4, :], in_=kr[:, 0:128], identity=idb[:]), r=krk + ['idb'], w=[ptn])
                S.act(lambda e, tt=tt, pt=pt: e.activation(out=KT[:, tt * 128:(tt + 1) * 128], in_=pt[:, 4, :], func=AF.Copy),
                      r=[ptn], w=[('K1', ('k', tt))])
            thunks = []
            for tt in range(16):
                thunks.append(lambda tt=tt: q_tile(tt))
                thunks.append(lambda tt=tt: k_tile(tt))
            S.interleave(thunks, 4, 'qk')
            S.fence('K0')
            S.fence('K1')
            S.fence('FS')
            S.fence('PT0')
            S.fence('PT1')
            pbuf = [FS[:, i * 256:(i + 1) * 256].bitcast(BF16) for i in range(6)]
            dens = [FS[0:64, 1536:2048], FS[0:64, 2048:2560]]
            itc = [0, 0]

            def core(g, n):
                if True:
                    it = itc[0]
                    c_ = itc[1]
                    gp = slice(g * 64, (g + 1) * 64)
                    ms = [m for m in (n - 1, n, n + 1) if 0 <= m < 16]
                    bO = PS[2 + 2 * (c_ % 2)]
                    bD = PS[3 + 2 * (c_ % 2)]
                    kO = 'P%d' % (2 + 2 * (c_ % 2))
                    kD = 'P%d' % (3 + 2 * (c_ % 2))
                    den = dens[c_ % 2]
                    dk = ('FS', ('den', c_ % 2))
                    itc[1] += 1
                    for mi, m in enumerate(ms):
                        bS = PS[c_ % 2]
                        kS = 'P%d' % (c_ % 2)
                        pb = pbuf[(c_ % 2) * 3 + mi]
                        pk = ('FS', 'pb%d' % ((c_ % 2) * 3 + mi))
                        it += 1
                        itc[0] = it
                        S.pe(lambda e, bS=bS, m=m, n=n, gp=gp: e.matmul(bS[:, :], lhsT=KT[gp, m * 128:(m + 1) * 128], rhs=QT[gp, :, n * 128:(n + 1) * 128],
                                                                        start=True, stop=True), r=['K0', 'K1'], w=[kS])
                        S.act(lambda e, bS=bS, pb=pb: e.activation(out=pb, in_=bS[:, :], func=AF.Exp, scale=0.125), r=[kS], w=[pk])
                        if m != n:
                            msk = trige if m < n else trile
                            S.pool(lambda e, pb=pb, msk=msk: e.tensor_tensor(out=pb.rearrange("p (j q) -> p j q", j=4), in0=pb.rearrange("p (j q) -> p j q", j=4),
                                                                             in1=msk[:].unsqueeze(1).to_broadcast([128, 4, 128]), op=ALU.mult),
                                   r=[pk, 'trige', 'trile'], w=[pk])

                        def mmf(e, pb=pb, m=m, mi=mi, g=g, last=(mi == len(ms) - 1), bO=bO, bD=bD):
                            e.matmul(bO[0:64, :], lhsT=Vat[:, m, g * 64:(g + 1) * 64], rhs=pb, start=(mi == 0), stop=last)
                            return e.matmul(bD[0:64, :], lhsT=onesb[:, 0:64], rhs=pb, start=(mi == 0), stop=last)
                        S.pe(mmf, r=[pk, 'K1', 'onesb'], w=[kO, kD])
                    S.dve(lambda e, g=g, bD=bD, den=den: e.tensor_tensor(out=den.rearrange("p (j q) -> p j q", j=4),
                                                                         in0=bD[0:64, :].rearrange("p (j q) -> p j q", j=4),
                                                                         in1=esink[0:64, g * 4:(g + 1) * 4].unsqueeze(2).to_broadcast([64, 4, 128]), op=ALU.add),
                          r=[kD, 'esink'], w=[dk])
                    S.act(lambda e, den=den: e.activation(out=den, in_=den, func=AF.Ln), r=[dk], w=[dk])
                    S.act(lambda e, den=den: e.activation(out=den, in_=den, func=AF.Exp, scale=-1.0), r=[dk], w=[dk])
                    S.dve(lambda e, g=g, n=n, bO=bO, den=den: e.tensor_tensor(out=y_atT[0:64, g * 4:(g + 1) * 4, n * 128:(n + 1) * 128],
                                                                             in0=bO[0:64, :].rearrange("p (j q) -> p j q", j=4),
                                                                             in1=den.rearrange("p (j q) -> p j q", j=4), op=ALU.mult),
                          r=[kO, dk], w=[('K5', (g, n)), ('K6', (g, n))])
            S.interleave([lambda g=g, n=n: core(g, n) for g in range(2) for n in range(16)], 2, 'core')
            for i in (0, 1, 5, 6):
                S.fence('K%d' % i)
            S.fence('FS')
            if stage == 'attn':
                dump('yat', K(5, 2), [128, 16384], BF16, ['K5', 'K6'])
                return

            mergedT = K(0, 2).rearrange("p (k t) -> p k t", k=8)
            sgs = [[FS[:, (p_ * 6 + i) * 512:(p_ * 6 + i + 1) * 512] for i in range(3)] for p_ in range(2)]
            accs = [FS[:, (p_ * 6 + 3) * 512:(p_ * 6 + 4) * 512] for p_ in range(2)]
            tqs = [FS[:, (p_ * 6 + 4) * 512:(p_ * 6 + 5) * 512] for p_ in range(2)]
            tq2s = [FS[:, (p_ * 6 + 5) * 512:(p_ * 6 + 6) * 512] for p_ in range(2)]
            yT = [K(3).rearrange("p (a t) -> p a t", a=4), y_atT, K(4).rearrange("p (a t) -> p a t", a=4)]
            ykeys = [['K3'], ['K5', 'K6'], ['K4']]
            wbr = din['w_branch'][l]
            S.fence('WP')
            S.fence('WBA')

            def mg_load(dt):
                slot = dt % 2
                mwf = WP[:, slot, :, :].rearrange("p k c -> p (k c)")
                wg = [mwf[:, i * 1024:(i + 1) * 1024].rearrange("p (k c) -> p k c", k=8) for i in range(3)]
                wb_hy = mwf[:, 3072:3584].rearrange("p (k c) -> p k c", k=4)
                wb_gm = mwf[:, 3584:4096].rearrange("p (k c) -> p k c", k=4)
                wb_at = WBA[0:64, slot, :, :]
                dc = slice(dt * 128, (dt + 1) * 128)
                for i in range(3):
                    c0 = COL_GATE + i * 1024 + dt * 128
                    S.dma('pool', lambda e, i=i, c0=c0, wg=wg: e.dma_start(out=wg[i], in_=w_in[:, c0:c0 + 128].rearrange("(k p) n -> p k n", p=128)),
                          w=[('WP', (slot, i))])
                S.dma('pool', lambda e: e.dma_start(out=wb_hy, in_=wbr[0, :, dc].rearrange("(k p) n -> p k n", p=128)), w=[('WP', (slot, 3))])
                S.dma('pool', lambda e: e.dma_start(out=wb_gm, in_=wbr[2, :, dc].rearrange("(k p) n -> p k n", p=128)), w=[('WP', (slot, 4))])
                S.dma('pool', lambda e: e.dma_start(out=wb_at, in_=wbr[1, :, dc].rearrange("(h d) n -> d h n", d=64)), w=[('WBA', slot)])
                return slot, wg, [wb_hy, wb_at, wb_gm]
            mg_next = mg_load(0)
            for dt in range(8):
                slot, wg, wbs = mg_next
                if dt + 1 < 8:
                    mg_next = mg_load(dt + 1)
                mwk = [('WP', (slot, i)) for i in range(5)]
                def mg_tb(tb, dt=dt, slot=slot, wg=wg, wbs=wbs):
                    tcs = slice(tb * 512, (tb + 1) * 512)
                    p_ = tb % 2
                    sg = sgs[p_]
                    acc = accs[p_]
                    tq = tqs[p_]
                    tq2 = tq2s[p_]
                    fk = lambda n_, p_=p_: ('FS', (n_, p_))
                    for i in range(3):
                        uG = 3 * p_ + (2 * i) % 3
                        uB = 3 * p_ + (2 * i + 1) % 3
                        bG = PS[uG]
                        bB = PS[uB]
                        kG = 'P%d' % uG
                        kB_ = 'P%d' % uB

                        def mmg(e, bG=bG, i=i, tb=tb, wg=wg):
                            ins = None
                            for k in range(8):
                                ins = e.matmul(bG[:, :], lhsT=wg[i][:, k, :], rhs=HT[:, k, 1 + tb * 512:1 + (tb + 1) * 512], start=(k == 0), stop=(k == 7))
                            return ins
                        S.pe(mmg, r=[('WP', (slot, i)), 'HT'], w=[kG])
                        S.act(lambda e, bG=bG, i=i, sg=sg: e.activation(out=sg[i], in_=bG[:, :], func=AF.Sigmoid), r=[kG], w=[fk('sg%d' % i)])

                        def mmb(e, bB=bB, i=i, tcs=tcs, wbs=wbs):
                            ins = None
                            if i == 1:
                                for h in range(8):
                                    ins = e.matmul(bB[:, :], lhsT=wbs[1][:, h, :], rhs=y_atT[0:64, h, tcs], start=(h == 0), stop=(h == 7))
                            else:
                                for cc in range(4):
                                    ins = e.matmul(bB[:, :], lhsT=wbs[i][:, cc, :], rhs=yT[i][:, cc, tcs], start=(cc == 0), stop=(cc == 3))
                            return ins
                        S.pe(mmb, r=mwk[3:5] + [('WBA', slot)] + ykeys[i], w=[kB_])
                        if i == 0:
                            S.dve(lambda e, bB=bB, acc=acc, sg=sg: e.tensor_tensor(out=acc, in0=bB[:, :], in1=sg[0], op=ALU.mult), r=[kB_, fk('sg0')], w=[fk('acc')])
                        elif i == 1:
                            S.dve(lambda e, bB=bB, tq=tq, sg=sg: e.tensor_tensor(out=tq, in0=bB[:, :], in1=sg[1], op=ALU.mult), r=[kB_, fk('sg1')], w=[fk('tq')])
                            S.pool(lambda e, acc=acc, tq=tq: e.tensor_tensor(out=acc, in0=acc, in1=tq, op=ALU.add), r=[fk('acc'), fk('tq')], w=[fk('acc')])
                        else:
                            S.dve(lambda e, bB=bB, tq2=tq2, sg=sg: e.tensor_tensor(out=tq2, in0=bB[:, :], in1=sg[2], op=ALU.mult), r=[kB_, fk('sg2')], w=[fk('tq2')])
                            S.pool(lambda e, dt=dt, tcs=tcs, acc=acc, tq2=tq2: e.tensor_tensor(out=mergedT[:, dt, tcs], in0=acc, in1=tq2, op=ALU.add),
                                   r=[fk('acc'), fk('tq2')], w=[('K0', (dt, tcs.start)), ('K1', (dt, tcs.start))])
                S.interleave([lambda tb=tb, mg_tb=mg_tb: mg_tb(tb) for tb in range(4)], 2, 'mg')
            for i in range(7):
                S.fence('K%d' % i)
            S.fence('FS')
            S.fence('HT')
            S.fence('WP')
            S.fence('WBA')
            if stage == 'merge':
                dump('merged', K(0, 2), [128, 16384], BF16, ['K0', 'K1'])
                return

            wout = K(2).rearrange("p (k c) -> p k c", k=8)
            gt1_bc = FS[:, 3072:4096]
            G2_bc = FS[:, 4096:5120]
            SH2_bc = FS[:, 5120:6144]
            h2b = K(3).rearrange("p (s c) -> p s c", s=8)
            h2T = K(4).rearrange("p (s k t) -> p s k t", s=8, k=8)
            for hf in range(2):
                S.dma('pool', lambda e, hf=hf: e.dma_start(out=wout[:, :, hf * 512:(hf + 1) * 512],
                                                           in_=din['w_out'][l, :, hf * 512:(hf + 1) * 512].rearrange("(k p) n -> p k n", p=128)),
                      w=[('K2', hf)])
            S.dma('sp', lambda e: e.dma_start(out=gt1_bc, in_=modd[l, b:b + 1, 2048:3072].partition_broadcast(128)), r=[('modd', l)], w=[('FS', 'gt1')])
            S.dma('sp', lambda e: e.dma_start(out=G2_bc, in_=modd[l, b:b + 1, 4096:5120].partition_broadcast(128)), r=[('modd', l)], w=[('FS', 'G2')])
            S.dma('sp', lambda e: e.dma_start(out=SH2_bc, in_=modd[l, b:b + 1, 3072:4096].partition_broadcast(128)), r=[('modd', l)], w=[('FS', 'SH2')])
            S.dma('sp', lambda e: e.dma_start(out=TMP, in_=din['norm2_g'][l:l + 1, :].partition_broadcast(128)), w=[('FS', 'tmp')])
            S.dve(lambda e: e.scalar_tensor_tensor(out=G2_bc, in0=G2_bc, scalar=1.0, in1=TMP, op0=ALU.add, op1=ALU.mult),
                  r=[('FS', 'G2'), ('FS', 'tmp')], w=[('FS', 'G2')])
            S.dve(lambda e: e.tensor_tensor(out=wout, in0=wout, in1=gt1_bc.unsqueeze(1).to_broadcast([128, 8, 1024]), op=ALU.mult),
                  r=['K2', ('FS', 'gt1')], w=['K2'])
            TMP5 = [FS[:, 2048:3072], FS[:, 3072:4096]]
            TK5 = [('FS', 'tmp'), ('FS', 'gt1')]

            def w5_tile(tt):
                p_ = tt % 2
                TMP = TMP5[p_]
                tk_ = TK5[p_]
                sb_ = 160 + 16 * p_
                xt = XIO[tt % 2]
                xk = ('FS', 'xio%d' % (tt % 2))
                S.dma('sp', lambda e, xt=xt, tt=tt: e.dma_start(out=xt, in_=xin[b, tt * 128:(tt + 1) * 128, :]), r=[xin_key], w=[xk])
                for hf in range(2):
                    bank = PS[(tt * 2 + hf) % 4]
                    bk = 'P%d' % ((tt * 2 + hf) % 4)

                    def mmf(e, bank=bank, tt=tt, hf=hf):
                        ins = None
                        for k in range(8):
                            ins = e.matmul(bank[:, :], lhsT=mergedT[:, k, tt * 128:(tt + 1) * 128], rhs=wout[:, k, hf * 512:(hf + 1) * 512], start=(k == 0), stop=(k == 7))
                        return ins
                    S.pe(mmf, r=['K0', 'K1', 'K2'], w=[bk])
                    S.dve(lambda e, bank=bank, xt=xt, hf=hf: e.tensor_tensor(out=xt[:, hf * 512:(hf + 1) * 512], in0=bank[:, :], in1=xt[:, hf * 512:(hf + 1) * 512], op=ALU.add),
                          r=[bk, xk], w=[xk])
                S.dma('sp', lambda e, xt=xt, tt=tt: e.dma_start(out=xout[b, tt * 128:(tt + 1) * 128, :], in_=xt), r=[xk], w=[(xout_key, (b, tt))])
                S.act(lambda e, xt=xt: e.activation(out=TMP, in_=xt, func=AF.Square, accum_out=small[:, sb_ + 2:sb_ + 3]), r=[xk], w=[tk_, ('small', (2, p_))])
                S.act(lambda e: e.activation(out=small[:, sb_ + 3:sb_ + 4], in_=small[:, sb_ + 2:sb_ + 3], func=AF.Sqrt, scale=1.0 / D, bias=epsb[:, 0:1]),
                      r=[('small', (2, p_)), 'epsb'], w=[('small', (3, p_))])
                S.dve(lambda e: e.reciprocal(out=small[:, sb_ + 3:sb_ + 4], in_=small[:, sb_ + 3:sb_ + 4]), r=[('small', (3, p_))], w=[('small', (3, p_))])
                S.dve(lambda e, xt=xt: e.scalar_tensor_tensor(out=TMP, in0=xt, scalar=small[:, sb_ + 3:sb_ + 4], in1=G2_bc, op0=ALU.mult, op1=ALU.mult),
                      r=[xk, ('small', (3, p_)), ('FS', 'G2')], w=[tk_])
                hs = tt % 2
                S.pool(lambda e, hs=hs: e.tensor_tensor(out=h2b[:, hs, :], in0=TMP, in1=SH2_bc, op=ALU.add), r=[tk_, ('FS', 'SH2')], w=[('K3', hs)])
                S.dma('sp', lambda e, hs=hs, tt=tt: e.dma_start(out=h2d[b, tt * 128:(tt + 1) * 128, :], in_=h2b[:, hs, :]), r=[('K3', hs)], w=[('h2d', (b, tt))])

                pt, ptk = nextpt()

                def trf(e, hs=hs, pt=pt):
                    ins = None
                    for k in range(8):
                        ins = e.transpose(out=pt[:, k, :], in_=h2b[:, hs, k * 128:(k + 1) * 128], identity=idb[:])
                    return ins
                S.pe(trf, r=[('K3', hs), 'idb'], w=[ptk])
                S.act(lambda e, hs=hs, pt=pt: e.activation(out=h2T[:, hs, :, :], in_=pt[:, :, :], func=AF.Copy), r=[ptk], w=[('K4', hs)])
                bank = PS[4 + tt % 2]
                bk = 'P%d' % (4 + tt % 2)

                def mmr(e, bank=bank, hs=hs):
                    ins = None
                    for k in range(8):
                        ins = e.matmul(bank[:, 0:NE], lhsT=h2T[:, hs, k, :], rhs=wr_sb[:, k, :], start=(k == 0), stop=(k == 7))
                    return ins
                S.pe(mmr, r=[('K4', hs), 'wr_sb'], w=[bk])
                S.dve(lambda e, bank=bank: e.tensor_reduce(out=small[:, sb_ + 4:sb_ + 5], in_=bank[:, 0:NE], axis=AX.X, op=ALU.max), r=[bk], w=[('small', (4, p_))])
                S.dve(lambda e: e.tensor_scalar(out=small[:, sb_ + 5:sb_ + 6], in0=small[:, sb_ + 4:sb_ + 5], scalar1=-1.0, scalar2=None, op0=ALU.mult), r=[('small', (4, p_))], w=[('small', (5, p_))])
                S.act(lambda e, bank=bank: e.activation(out=small[:, 32 + sb_:48 + sb_], in_=bank[:, 0:NE], func=AF.Exp, bias=small[:, sb_ + 5:sb_ + 6], accum_out=small[:, sb_ + 6:sb_ + 7]),
                      r=[bk, ('small', (5, p_))], w=[('small', ('e', p_)), ('small', (6, p_))])
                S.dve(lambda e: e.reciprocal(out=small[:, sb_ + 6:sb_ + 7], in_=small[:, sb_ + 6:sb_ + 7]), r=[('small', (6, p_))], w=[('small', (6, p_))])
                S.dve(lambda e, tt=tt: e.tensor_scalar(out=affall[:, tt, b * NE:(b + 1) * NE], in0=small[:, 32 + sb_:48 + sb_], scalar1=small[:, sb_ + 6:sb_ + 7], scalar2=None, op0=ALU.mult),
                      r=[('small', ('e', p_)), ('small', (6, p_))], w=[('affall', (b, tt))])
            S.interleave([lambda tt=tt: w5_tile(tt) for tt in range(16)], 2, 'w5')
            for i in range(7):
                S.fence('K%d' % i)
            S.fence('FS')


        mx8 = sb("mx8", [32, 8], F32)

        def moe(l, xout, xout_key):
            for i in range(7):
                S.fence('K%d' % i)
            S.fence('FS')
            S.fence('WP')
            WPf = WP[:].rearrange("p a k c -> p (a k c)")
            Rall = WPf[:, 2048:4096].rearrange("p (a b c) -> p a b c", a=16, b=32)
            affT = KF[0][0:32, 0:2048]
            work = KF[0][0:32, 2048:4096]
            mask = KF[1][0:32, 0:2048]
            ones_r = KF[1][0:32, 2048:4096]
            keyr = KF[2][0:32, 0:2048]
            for tb in range(4):
                bank = PS[tb % 2]
                bk = 'P%d' % (tb % 2)

                def trf(e, bank=bank, tb=tb):
                    ins = None
                    for j in range(4):
                        ins = e.transpose(out=bank[0:32, j * 128:(j + 1) * 128], in_=affall[:, tb * 4 + j, :], identity=idf[:])
                    return ins
                S.pe(trf, r=['affall', 'idf'], w=[bk])
                S.act(lambda e, bank=bank, tb=tb: e.activation(out=affT[:, tb * 512:(tb + 1) * 512], in_=bank[0:32, :], func=AF.Copy),
                      r=[bk], w=[('K0', ('affT', tb))])
            S.dve(lambda e: e.tensor_copy(out=work, in_=affT), r=['K0'], w=[('K0', 'work')])
            S.pool(lambda e: e.memset(ones_r, 1.0), w=[('K1', 'ones')])
            for r_ in range(CAP // 8):
                S.dve(lambda e: e.max(out=mx8[:], in_=work), r=[('K0', 'work')], w=['mx8'])
                if r_ < CAP // 8 - 1:
                    S.dve(lambda e: e.match_replace(out=work, in_to_replace=mx8[:], in_values=work, imm_value=-1.0),
                          r=[('K0', 'work'), 'mx8'], w=[('K0', 'work')])
            S.dve(lambda e: e.tensor_scalar(out=mask, in0=affT, scalar1=mx8[:, 7:8], scalar2=None, op0=ALU.is_ge), r=['K0', 'mx8'], w=[('K1', 'mask')])
            S.dve(lambda e: e.tensor_tensor_scan(out=keyr, data0=ones_r, data1=mask, initial=0.0, op0=ALU.mult, op1=ALU.add),
                  r=[('K1', 'mask'), ('K1', 'ones')], w=[('K2', 'key')])
            S.dve(lambda e: e.tensor_tensor(out=keyr, in0=keyr, in1=mask, op=ALU.mult), r=[('K2', 'key'), ('K1', 'mask')], w=[('K2', 'key')])

            def trk(e):
                ins = None
                for tt in range(16):
                    ins = e.transpose(out=PS[2][:, tt * 32:(tt + 1) * 32], in_=keyr[:, tt * 128:(tt + 1) * 128], identity=idf[0:32, 0:32])
                return ins
            S.pe(trk, r=[('K2', 'key'), 'idf'], w=['P2'])
            keyb = WPf[:, 4096:4608].rearrange("p (a b) -> p a b", a=16)
            S.act(lambda e: e.activation(out=keyb.rearrange("p a b -> p (a b)"), in_=PS[2][:, :], func=AF.Copy), r=['P2'], w=[('WP', 'keyb')])
            S.dve(lambda e: e.tensor_copy(out=Rall[:, :, :, 0:2], in_=tokidx[:].unsqueeze(2).to_broadcast([128, 16, 32, 2])), r=['tokidx'], w=[('WP', ('Rall', 0))])
            S.dve(lambda e: e.tensor_copy(out=Rall[:, :, :, 2], in_=affall[:]), r=['affall'], w=[('WP', ('Rall', 2))])
            S.dve(lambda e: e.tensor_tensor(out=Rall[:, :, :, 3], in0=affall[:], in1=Rall[:, :, :, 2], op=ALU.subtract),
                  r=['affall', ('WP', ('Rall', 2))], w=[('WP', ('Rall', 3))])
            S.fence('HT')
            HTf = HT[:].rearrange("p k t -> p (k t)")
            Poh = [HTf[:, i * 4096:(i + 1) * 4096].rearrange("p (t c) -> p t c", t=16) for i in range(2)]
            idxf = small[:, 48:56]
            rkeys = [('WP', ('Rall', 0)), ('WP', ('Rall', 2)), ('WP', ('Rall', 3))]

            def route(e_, b_, part):
                if True:
                    be = b_ * NE + e_
                    po = Poh[b_]
                    if part == 0:
                        S.dve(lambda e, po=po, be=be: e.tensor_tensor(out=po, in0=iota1[:].unsqueeze(1).to_broadcast([128, 16, 256]),
                                                                     in1=keyb[:, :, be].unsqueeze(2).to_broadcast([128, 16, 256]), op=ALU.is_equal),
                              r=['iota1', ('WP', 'keyb')], w=[('HT', ('poh', b_))])
                        return
                    for st in range(2):
                        bank = PS[5]
                        bk = 'P5'

                        def mmf(e, bank=bank, po=po, st=st, be=be):
                            ins = None
                            for tt in range(16):
                                ins = e.matmul(bank[:, 0:4], lhsT=po[:, tt, st * 128:(st + 1) * 128], rhs=Rall[:, tt, be, :], start=(tt == 0), stop=(tt == 15))
                            return ins
                        S.pe(mmf, r=[('HT', ('poh', b_))] + rkeys, w=[bk])
                        col = b_ * 2 + st
                        S.dve(lambda e, bank=bank: e.tensor_copy(out=idxf[:, 0:4], in_=bank[:, 0:4]), r=[bk], w=[('small', 'i0')])
                        S.dve(lambda e: e.scalar_tensor_tensor(out=idxf[:, 4:5], in0=idxf[:, 0:1], scalar=128.0, in1=idxf[:, 1:2], op0=ALU.mult, op1=ALU.add),
                              r=[('small', 'i0')], w=[('small', 'i1')])
                        S.dve(lambda e, col=col, b_=b_: e.tensor_scalar(out=idx_sb[:, e_, col:col + 1], in0=idxf[:, 4:5], scalar1=float(T * b_), scalar2=None,
                                                                        op0=ALU.add),
                              r=[('small', 'i1')], w=[('idx', (e_, col))])
                        S.dve(lambda e, col=col: e.tensor_tensor(out=gate_sb[:, e_, col:col + 1], in0=idxf[:, 2:3], in1=idxf[:, 3:4], op=ALU.add),
                              r=[('small', 'i0')], w=[('gate', (e_, col))])
            for i in (0, 1, 2, 3):
                S.fence('K%d' % i)
            if stage == 'route':
                for e_ in range(NE):
                    for b_ in range(2):
                        route(e_, b_, 0)
                        route(e_, b_, 1)
                return
            gt2_bc = FS[:, 0:2048].rearrange("p (b c) -> p b c", b=2)
            ystg = [FS[:, 2048 + i * 1024:2048 + (i + 1) * 1024] for i in range(4)]
            xg = K(4).rearrange("p (s q c) -> p s q c", s=2, q=4)
            xgT = K(5).rearrange("p (s k c) -> p s k c", s=2, k=8)
            actT = K(6).rearrange("p (f c) -> p f c", f=16)
            sgt = [WP[:].rearrange("p a k c -> p (a k c)").bitcast(F32)[:, i * 512:(i + 1) * 512] for i in range(2)]
            for b_ in range(2):
                S.dma('sp', lambda e, b_=b_: e.dma_start(out=gt2_bc[:, b_, :], in_=modd[l, b_:b_ + 1, 5120:6144].partition_broadcast(128)),
                      r=[('modd', l)], w=[('FS', ('gt2', b_))])
            wg_d = din['w_e_gate'][l]
            wu_d = din['w_e_up'][l]
            wd_d = din['w_e_down'][l]
            pieces = [(e_, q) for e_ in range(NE) for q in range(6)]

            def load_piece(pi):
                e_, q = pieces[pi]
                sl = pi % 4
                if q < 4:
                    dst = K(sl).rearrange("p (m k f) -> p m k f", m=2, k=8)
                    S.dma('pool', lambda e: e.dma_start(out=dst[:, 0, :, :], in_=wg_d[e_, :, q * 512:(q + 1) * 512].rearrange("(k p) f -> p k f", p=128)),
                          w=[('K%d' % sl, 'a')])
                    S.dma('pool', lambda e: e.dma_start(out=dst[:, 1, :, :], in_=wu_d[e_, :, q * 512:(q + 1) * 512].rearrange("(k p) f -> p k f", p=128)),
                          w=[('K%d' % sl, 'b')])
                else:
                    h = q - 4
                    dst = K(sl).rearrange("p (f c) -> p f c", f=16)
                    for hh in range(2):
                        S.dma('pool', lambda e, hh=hh: e.dma_start(out=dst[:, hh * 8:(hh + 1) * 8, :],
                                                                   in_=wd_d[e_, hh * 1024:(hh + 1) * 1024, h * 512:(h + 1) * 512].rearrange("(f p) c -> p f c", p=128)),
                              w=[('K%d' % sl, 'a' if hh == 0 else 'b')])

            def gathers(e_):
                par = e_ % 2
                for bs in range(4):
                    b_ = bs // 2
                    S.dma('pool', lambda e, bs=bs, b_=b_: e.indirect_dma_start(out=xg[:, par, bs, :], out_offset=None, in_=h2d.rearrange("b t d -> (b t) d"),
                                                                               in_offset=bass.IndirectOffsetOnAxis(ap=idx_sb[:, e_, bs:bs + 1], axis=0)),
                          r=['h2d', ('idx', (e_, bs))], w=[('K4', (par, bs))])

            def expert(e_):
                par = e_ % 2
                if e_ + 2 < NE:
                    route(e_ + 2, 0, 0)
                if e_ + 1 < NE:
                    gathers(e_ + 1)
                for bs in range(4):
                    pt, ptk = nextpt()

                    def trf(e, bs=bs, pt=pt):
                        ins = None
                        for k in range(8):
                            ins = e.transpose(out=pt[:, k, :], in_=xg[:, par, bs, k * 128:(k + 1) * 128], identity=idb[:])
                        return ins
                    S.pe(trf, r=[('K4', (par, bs)), 'idb'], w=[ptk])
                    S.act(lambda e, bs=bs, pt=pt: e.activation(out=xgT[:, par, :, bs * 128:(bs + 1) * 128], in_=pt[:, :, :], func=AF.Copy),
                          r=[ptk], w=[('K5', (par, bs))])
                for q in range(6):
                    pi = e_ * 6 + q
                    if pi + 3 < len(pieces):
                        load_piece(pi + 3)
                    if e_ + 2 < NE:
                        if q == 2:
                            route(e_ + 2, 0, 1)
                            route(e_ + 2, 1, 0)
                        elif q == 4:
                            route(e_ + 2, 1, 1)
                    sl = pi % 4
                    wk = 'K%d' % sl
                    if q < 4:
                        wsl = K(sl).rearrange("p (m k f) -> p m k f", m=2, k=8)
                        for fl in range(4):
                            ft = q * 4 + fl
                            bA = PS[(ft % 2) * 2]
                            bU = PS[(ft % 2) * 2 + 1]
                            kA = 'P%d' % ((ft % 2) * 2)
                            kU = 'P%d' % ((ft % 2) * 2 + 1)
                            for m, bank, bk in ((0, bA, kA), (1, bU, kU)):
                                def mmf(e, m=m, bank=bank, fl=fl, wsl=wsl):
                                    ins = None
                                    for k in range(8):
                                        ins = e.matmul(bank[:, :], lhsT=wsl[:, m, k, fl * 128:(fl + 1) * 128], rhs=xgT[:, par, k, :], start=(k == 0), stop=(k == 7))
                                    return ins
                                S.pe(mmf, r=[wk, 'K5'], w=[bk])
                            st_ = sgt[ft % 2]
                            S.act(lambda e, bA=bA, st_=st_: e.activation(out=st_, in_=bA[:, :], func=AF.Silu), r=[kA], w=[('WP', ('sg', ft % 2))])
                            S.dve(lambda e, bU=bU, st_=st_, ft=ft: e.tensor_tensor(out=actT[:, ft, :], in0=bU[:, :], in1=st_, op=ALU.mult),
                                  r=[kU, ('WP', ('sg', ft % 2))], w=[('K6', ft)])
                    else:
                        h = q - 4
                        wsl = K(sl).rearrange("p (f c) -> p f c", f=16)
                        for bs in range(4):
                            b_ = bs // 2
                            bank = PS[4 + bs % 2]
                            bk = 'P%d' % (4 + bs % 2)

                            def mmf(e, bank=bank, bs=bs, wsl=wsl):
                                ins = None
                                for ft in range(16):
                                    ins = e.matmul(bank[:, :], lhsT=actT[:, ft, bs * 128:(bs + 1) * 128], rhs=wsl[:, ft, :], start=(ft == 0), stop=(ft == 15))
                                return ins
                            S.pe(mmf, r=[wk, 'K6'], w=[bk])
                            S.dve(lambda e, bank=bank, bs=bs, b_=b_, h=h: e.scalar_tensor_tensor(out=ystg[bs][:, h * 512:(h + 1) * 512], in0=bank[:, :],
                                                                                                  scalar=gate_sb[:, e_, bs:bs + 1],
                                                                                                  in1=gt2_bc[:, b_, h * 512:(h + 1) * 512],
                                                                                                  op0=ALU.mult, op1=ALU.mult),
                                  r=[bk, ('gate', (e_, bs)), ('FS', ('gt2', b_))], w=[('FS', ('y', bs))])
                for bs in range(4):
                    b_ = bs // 2
                    S.dma('pool', lambda e, bs=bs, b_=b_: e.indirect_dma_start(out=xout.rearrange("b t d -> (b t) d"),
                                                                               out_offset=bass.IndirectOffsetOnAxis(ap=idx_sb[:, e_, bs:bs + 1], axis=0),
                                                                               in_=ystg[bs], in_offset=None, compute_op=ALU.add),
                          r=[('FS', ('y', bs)), ('idx', (e_, bs))], w=[xout_key])

            for pi in range(3):
                load_piece(pi)
            for e_ in range(2):
                for b_ in range(2):
                    route(e_, b_, 0)
                    route(e_, b_, 1)
            gathers(0)
            for e_ in range(NE):
                expert(e_)
            S.fence('HT')
            S.dve(lambda e: e.memset(HT[:, :, 0:1], 0.0), w=['HT'])
            S.dve(lambda e: e.memset(HT[:, :, T + 1:T + 2], 0.0), w=['HT'])
            for i in range(7):
                S.fence('K%d' % i)
            S.fence('FS')
            S.fence('WP')

        for l in range(nlayers):
            xin = din['x'] if l == 0 else xs0
            xout = xs0 if (l == 0 and nlayers > 1) else out_d
            xin_key = 'xin%d' % l
            xout_key = 'xin%d' % (l + 1)
            layer_prep(l)
            if stage == 'prep':
                dump('G1T', G1T[:], [128, NB, 8], F32, ['G1T'])
                dump('esink', esink[:], [128, 8], F32, ['esink'])
                break
            filter_phase(l)
            if stage in ('f1', 'f2'):
                break
            if stage == 'filter':
                dump('pqd', pqd, [2, 2, T, 512], F32, ['pqd'])
                break
            stop = False
            for b in range(NB):
                mixer(l, b, xin, xin_key, xout, xout_key)
                if stage in ('ht', 'hyproj', 'hyena', 'gmlp', 'attn', 'merge'):
                    stop = True
                    break
            if stop:
                break
            if stage == 'mixer':
                dump('h2d', h2d, [NB, T, D], BF16, ['h2d'])
                dump('aff', affall[:], [128, 16, 32], F32, ['affall'])
                break
            moe(l, xout, xout_key)
            if stage == 'route':
                dump('idx', idx_sb[:], [128, NE, 4], I32, ['idx'])
                dump('gate', gate_sb[:], [128, NE, 4], F32, ['gate'])
                break
        fk = ['xin%d' % nlayers] + list(dump_d.keys())
        if stage is not None:
            fk += ['pqd', 'h2d', 'xin1', 'idx', 'gate', 'HT', 'affall']
        S.emit(final_keys=fk)
    return nc, dump_d


_NC_CACHE = {}


def kernel(**inputs):
    consts = make_consts()
    if 'full' not in _NC_CACHE:
        _NC_CACHE['full'] = build()
    nc, _ = _NC_CACHE['full']
    shared = {k: np.ascontiguousarray(np.asarray(v, dtype=np.float32)) for k, v in inputs.items() if k not in ('x', 'c')}
    shared.update(consts)
    x = np.asarray(inputs['x'], dtype=np.float32)
    c = np.asarray(inputs['c'], dtype=np.float32)
    in_maps = []
    for i in range(8):
        m = dict(shared)
        m['x'] = np.ascontiguousarray(x[2 * i:2 * i + 2])
        m['c'] = np.ascontiguousarray(c[2 * i:2 * i + 2])
        in_maps.append(m)
    res = run_bass_kernel_spmd(nc, in_maps, core_ids=list(range(8)))
    return np.concatenate([r['out'] for r in res.results], axis=0).astype(np.float32)
```

```python
import math
import contextlib
import numpy as np
import ml_dtypes
import concourse.bass as bass
import concourse.mybir as mybir
from concourse.bass_utils import run_bass_kernel_spmd

F32 = mybir.dt.float32
BF16 = mybir.dt.bfloat16
I32 = mybir.dt.int32
ALU = mybir.AluOpType
AF = mybir.ActivationFunctionType
AX = mybir.AxisListType

T = 2048
D = 1024
NB = 2
NL = 2
NE = 16
CAP = 256
DE = 2048
COL_V, COL_X1, COL_X2, COL_Q, COL_K, COL_GU, COL_GV, COL_GATE = 0, 512, 1024, 1536, 2048, 2304, 2816, 3328
EPS = 1e-6


class Sched:
    NDMASEM = 8

    def __init__(self, nc):
        self.nc = nc
        self.ops = []
        self.st = {}
        self._cap = None

    def _acc(self, key, write, i, deps):
        if isinstance(key, tuple):
            base, sub = key
        else:
            base, sub = key, None
        b = self.st.get(base)
        if b is None:
            b = self.st[base] = {'w': set(), 'r': set(), 'subs': {}}
        if sub is None:
            deps |= b['w']
            for s in b['subs'].values():
                deps |= s['w']
                if write:
                    deps |= s['r']
            if write:
                deps |= b['r']
                b['w'] = {i}
                b['r'] = set()
                b['subs'] = {}
            else:
                b['r'].add(i)
        else:
            s = b['subs'].get(sub)
            if s is None:
                s = b['subs'][sub] = {'w': set(), 'r': set()}
            deps |= b['w']
            deps |= s['w']
            if write:
                deps |= b['r']
                deps |= s['r']
                s['w'] = {i}
                s['r'] = set()
            else:
                s['r'].add(i)

    def fence(self, base):
        assert self._cap is None
        b = self.st.get(base)
        if b is None:
            return
        w = set(b['w']) | set(b['r'])
        for s in b['subs'].values():
            w |= s['w']
            w |= s['r']
        b['w'] = w
        b['r'] = set()
        b['subs'] = {}

    def interleave(self, thunks, group, name=''):
        import os
        on = os.environ.get('IL_ON')
        if on is not None and name not in on.split(','):
            group = 1
        for g0 in range(0, len(thunks), group):
            chains = []
            for th in thunks[g0:g0 + group]:
                self._cap = []
                th()
                chains.append(self._cap)
                self._cap = None
            pos = [0] * len(chains)
            left = sum(len(c) for c in chains)
            while left:
                for ci, c in enumerate(chains):
                    if pos[ci] < len(c):
                        self.add(*c[pos[ci]])
                        pos[ci] += 1
                        left -= 1

    def add(self, eng, fn, reads=(), writes=(), dma=False):
        if self._cap is not None:
            self._cap.append((eng, fn, tuple(reads), tuple(writes), dma))
            return None
        i = len(self.ops)
        deps = set()
        for k in reads:
            kb = k[0] if isinstance(k, tuple) else k
            self._acc(k, isinstance(kb, str) and len(kb) >= 2 and kb[0] == 'P' and (kb[1].isdigit() or kb[1] == 'T'), i, deps)
        for k in writes:
            self._acc(k, True, i, deps)
        deps.discard(i)
        self.ops.append(dict(eng=eng, fn=fn, deps=deps, dma=dma))
        return i

    def pe(self, fn, r=(), w=()):
        return self.add('pe', fn, r, w)

    def act(self, fn, r=(), w=()):
        return self.add('act', fn, r, w)

    def dve(self, fn, r=(), w=()):
        return self.add('dve', fn, r, w)

    def pool(self, fn, r=(), w=()):
        return self.add('pool', fn, r, w)

    def dma(self, q, fn, r=(), w=()):
        return self.add(q, fn, r, w, dma=True)

    def last_writers(self, key):
        deps = set()
        self._acc(key, False, -1, deps)
        return deps

    def emit(self, final_keys=()):
        nc = self.nc
        ops = self.ops
        n = len(ops)
        needed = [False] * n
        for i, o in enumerate(ops):
            for d in o['deps']:
                od = ops[d]
                if od['eng'] == 'pe' and o['eng'] == 'pe' and not o['dma'] and not od['dma']:
                    continue
                needed[d] = True
        finals = set()
        for k in final_keys:
            finals |= self.last_writers(k)
        finals.discard(-1)
        for d in finals:
            needed[d] = True
        engs = ['pe', 'act', 'dve', 'pool', 'sp']
        with contextlib.ExitStack() as es:
            esem = {e: es.enter_context(nc.semaphore('s_' + e)) for e in engs}
            dsem = {e: [es.enter_context(nc.semaphore('d_%s%d' % (e, j))) for j in range(self.NDMASEM)]
                    for e in ('sp', 'act', 'pool')}
            ecount = {e: 0 for e in engs}
            dcount = {e: 0 for e in ('sp', 'act', 'pool')}
            sig = [None] * n
            gate = [None] * n
            for i, o in enumerate(ops):
                e = o['eng']
                if o['dma']:
                    k = dcount[e]
                    dcount[e] += 1
                    s = dsem[e][k % self.NDMASEM]
                    rnd = k // self.NDMASEM
                    sig[i] = (s, 16 * (rnd + 1), 16)
                    if rnd > 0:
                        gate[i] = (s, 16 * rnd)
                elif needed[i]:
                    ecount[e] += 1
                    sig[i] = (esem[e], ecount[e], 1)
            per = {e: [] for e in engs}
            for i, o in enumerate(ops):
                per[o['eng']].append(i)
            self.stats = {e: len(per[e]) for e in engs}
            blk = es.enter_context(nc.Block())

            def run(engname, eobj):
                seen = {}

                def wait(s, v):
                    key = id(s)
                    if seen.get(key, 0) >= v:
                        return
                    seen[key] = v
                    eobj.wait_ge(s, v)

                for i in per[engname]:
                    o = ops[i]
                    for d in sorted(o['deps']):
                        od = ops[d]
                        if od['eng'] == 'pe' and engname == 'pe' and not o['dma'] and not od['dma']:
                            continue
                        s, v, _ = sig[d]
                        wait(s, v)
                    if gate[i] is not None:
                        wait(*gate[i])
                    ins = o['fn'](eobj)
                    if sig[i] is not None:
                        ins.then_inc(sig[i][0], sig[i][2])
                if engname == 'sp':
                    for d in sorted(finals):
                        s, v, _ = sig[d]
                        wait(s, v)
                    for q in ('sp', 'act', 'pool'):
                        for j in range(self.NDMASEM):
                            cnt = (dcount[q] - j + self.NDMASEM - 1) // self.NDMASEM if dcount[q] > j else 0
                            if cnt > 0:
                                wait(dsem[q][j], 16 * cnt)

            @blk.tensor
            def _(e):
                run('pe', e)

            @blk.scalar
            def _(e):
                run('act', e)

            @blk.vector
            def _(e):
                run('dve', e)

            @blk.gpsimd
            def _(e):
                run('pool', e)

            @blk.sync
            def _(e):
                run('sp', e)


_CONSTS = None


def _tile16(M):
    return np.ascontiguousarray(M.reshape(16, 128, 16, 128).transpose(2, 1, 0, 3))


def make_consts():
    global _CONSTS
    if _CONSTS is not None:
        return _CONSTS
    bf = ml_dtypes.bfloat16
    N = 2 * T
    a = np.arange(T, dtype=np.float64)
    ang2 = np.pi * np.outer(2 * a + 1, 2 * a + 1) / (2 * N)
    c = {}
    c['k_c2'] = _tile16(np.cos(ang2)).astype(bf)
    c['k_s2'] = _tile16(np.sin(ang2)).astype(bf)
    angi = np.pi * np.outer(a, 2 * a + 1) / N
    c['k_ci'] = _tile16(np.cos(angi)).astype(bf)
    c['k_si'] = _tile16(-np.sin(angi)).astype(bf)
    del ang2, angi
    pos = np.arange(T, dtype=np.float32)
    inv = (np.float32(10000.0) ** (-(np.arange(0, 64, 2, dtype=np.float32) / np.float32(64)))).astype(np.float32)
    ang = pos[:, None] * inv[None, :]
    c['k_cos'] = np.ascontiguousarray(np.cos(ang).astype(np.float32).reshape(16, 128, 32).transpose(1, 0, 2))
    c['k_sin'] = np.ascontiguousarray(np.sin(ang).astype(np.float32).reshape(16, 128, 32).transpose(1, 0, 2))
    s_i = np.arange(128)[:, None]
    q_i = np.arange(128)[None, :]
    c['k_trige'] = (s_i >= q_i).astype(bf)
    c['k_trile'] = (s_i <= q_i).astype(bf)
    c['k_idb'] = np.eye(128).astype(bf)
    c['k_idf'] = np.eye(128).astype(np.float32)
    t = np.linspace(0.0, 1.0, T, dtype=np.float32)
    w = (2.0 * math.pi * pos / T).astype(np.float32)
    bands = np.linspace(1e-4, 7, 8, dtype=np.float32)
    an = w[:, None] * bands[None, :]
    feats = np.concatenate([t[:, None], np.cos(an), -np.sin(an)], axis=-1).astype(np.float32)
    c['k_featsT'] = np.ascontiguousarray(feats.T)
    deltas = np.abs(np.linspace(math.log(1e-2) / 0.3, math.log(1e-2) / 1.5, 512, dtype=np.float32))
    win = (np.exp(-t[:, None] * deltas[None, :]) + np.float32(0.05)).astype(np.float32)
    c['k_win'] = np.ascontiguousarray(win.reshape(16, 128, 512).transpose(1, 0, 2))
    c['k_iota1'] = np.tile(np.arange(1, 257, dtype=np.float32)[None, :], (128, 1)).astype(bf)
    ti = np.zeros((128, 16, 2), np.float32)
    ti[:, :, 0] = np.arange(16)[None, :]
    ti[:, :, 1] = np.arange(128)[:, None]
    c['k_tokidx'] = ti.astype(bf)
    _CONSTS = c
    return c


CONST_SHAPES = {
    'k_c2': ([16, 128, 16, 128], BF16), 'k_s2': ([16, 128, 16, 128], BF16),
    'k_ci': ([16, 128, 16, 128], BF16), 'k_si': ([16, 128, 16, 128], BF16),
    'k_cos': ([128, 16, 32], F32), 'k_sin': ([128, 16, 32], F32),
    'k_trige': ([128, 128], BF16), 'k_trile': ([128, 128], BF16),
    'k_idb': ([128, 128], BF16), 'k_idf': ([128, 128], F32),
    'k_featsT': ([17, 2048], F32), 'k_win': ([128, 16, 512], F32),
    'k_iota1': ([128, 256], BF16), 'k_tokidx': ([128, 16, 2], BF16),
}

IN_SHAPES = {
    'x': [NB, T, D], 'c': [NB, D], 'w_mod': [NL, D, 6 * D], 'b_mod': [NL, 6 * D],
    'norm1_g': [NL, D], 'norm2_g': [NL, D], 'w_in': [NL, D, 6400], 'hy_conv_w': [NL, 3, 1536],
    'hy_conv_b': [NL, 1536], 'hy_f1_w': [NL, 17, 64], 'hy_f1_b': [NL, 64], 'hy_f1_freq': [NL, 64],
    'hy_f2_w': [NL, 64, 64], 'hy_f2_b': [NL, 64], 'hy_f2_freq': [NL, 64], 'hy_f3_w': [NL, 64, 2048],
    'hy_bias': [NL, 2, 512], 'q_norm_g': [NL, 64], 'k_norm_g': [NL, 64], 'attn_sink': [NL, 8],
    'gm_ln_g': [NL, 512], 'gm_ln_b': [NL, 512], 'gm_ws': [NL, 4, 128, 128], 'gm_b': [NL, 4, 128],
    'w_branch': [NL, 3, 512, D], 'w_out': [NL, D, D], 'w_router': [NL, D, NE],
    'w_e_gate': [NL, NE, D, DE], 'w_e_up': [NL, NE, D, DE], 'w_e_down': [NL, NE, DE, D],
}


def build(nlayers=NL, stage=None, dumps=()):
    nc = bass.Bass("TRN2", target_bir_lowering=False)
    din = {}
    for name, shp in IN_SHAPES.items():
        if stage is not None and name.startswith('w_e_'):
            shp = [NL, 1, 8, 8]
        din[name] = nc.dram_tensor(name, list(shp), F32, kind="ExternalInput").ap()
    for name, (shp, dt) in CONST_SHAPES.items():
        din[name] = nc.dram_tensor(name, list(shp), dt, kind="ExternalInput").ap()
    out_d = nc.dram_tensor("out", [NB, T, D], F32, kind="ExternalOutput").ap()
    xs0 = nc.dram_tensor("xs0", [NB, T, D], F32, kind="Internal").ap()
    h2d = nc.dram_tensor("h2d", [NB, T, D], BF16, kind="Internal").ap()
    modd = nc.dram_tensor("modd", [NL, NB, 6 * D], F32, kind="Internal").ap()
    pqd = nc.dram_tensor("pqd", [2, 2, T, 512], F32, kind="Internal").ap()
    dump_d = {}
    S = Sched(nc)
    PI = math.pi

    with contextlib.ExitStack() as es:
        def sb(name, shape, dt):
            return es.enter_context(nc.sbuf_tensor("s_" + name, list(shape), dt))

        def psum(name, shape, dt):
            return es.enter_context(nc.psum_tensor("p_" + name, list(shape), dt))

        HT = sb("HT", [128, 8, T + 2], BF16)
        KB = sb("KB", [128, 7, 8192], BF16)
        FS = sb("FS", [128, 6144], F32)
        WP = sb("WP", [128, 2, 8, 512], BF16)
        WBA = sb("WBA", [128, 2, 8, 128], BF16)
        idb = sb("idb", [128, 128], BF16)
        idf = sb("idf", [128, 128], F32)
        trige = sb("trige", [128, 128], BF16)
        trile = sb("trile", [128, 128], BF16)
        ropec = sb("ropec", [128, 16, 32], F32)
        ropes = sb("ropes", [128, 16, 32], F32)
        iota1 = sb("iota1", [128, 256], BF16)
        tokidx = sb("tokidx", [128, 16, 2], BF16)
        onesb = sb("onesb", [128, 128], BF16)
        onesf = sb("onesf", [128, 128], F32)
        epsb = sb("epsb", [128, 1], F32)
        condT = sb("condT", [128, NB, 8], BF16)
        cTf = sb("cTf", [128, NB, 8], F32)
        G1T = sb("G1T", [128, NB, 8], F32)
        SH1T = sb("SH1T", [128, NB, 8], F32)
        n1g = sb("n1g", [128, 8], F32)
        small = sb("small", [128, 256], F32)
        affall = sb("affall", [128, 16, 32], F32)
        qg_bc = sb("qg_bc", [128, 64], F32)
        kg_bc = sb("kg_bc", [128, 64], F32)
        esink = sb("esink", [128, 8], F32)
        wr_sb = sb("wr_sb", [128, 8, NE], BF16)
        idx_sb = sb("idx_sb", [128, NE, 4], I32)
        gate_sb = sb("gate_sb", [128, NE, 4], F32)

        PS = [psum("ps%d" % i, [128, 512], F32) for i in range(6)]
        PTs = [psum("pt%d" % i, [128, 8, 128], BF16) for i in range(2)]
        ptc = [0]

        def nextpt():
            i = ptc[0] % 2
            ptc[0] += 1
            return PTs[i], 'PT%d' % i

        def K(i, n=1):
            if n == 1:
                return KB[:, i, :]
            return KB[:, i:i + n, :].rearrange("p a b -> p (a b)")

        def dump(name, ap, shape, dt, rkeys):
            d = nc.dram_tensor("dbg_" + name, list(shape), dt, kind="ExternalOutput").ap()
            dump_d["dbg_" + name] = d
            S.dma('sp', lambda e: e.dma_start(out=d, in_=ap), r=rkeys, w=['dbg_' + name])

        def ld(dst, src, key):
            S.dma('sp', lambda e: e.dma_start(out=dst, in_=src), w=[key])

        ld(idb[:], din['k_idb'][:, :], 'idb')
        ld(idf[:], din['k_idf'][:, :], 'idf')
        ld(trige[:], din['k_trige'][:, :], 'trige')
        ld(trile[:], din['k_trile'][:, :], 'trile')
        ld(ropec[:], din['k_cos'][:, :, :], 'ropec')
        ld(ropes[:], din['k_sin'][:, :, :], 'ropes')
        ld(iota1[:], din['k_iota1'][:, :], 'iota1')
        ld(tokidx[:], din['k_tokidx'][:, :, :], 'tokidx')
        S.dve(lambda e: e.memset(onesb[:], 1.0), w=['onesb'])
        S.dve(lambda e: e.memset(onesf[:], 1.0), w=['onesf'])
        S.dve(lambda e: e.memset(epsb[:], EPS), w=['epsb'])
        S.dve(lambda e: e.memset(HT[:, :, 0:1], 0.0), w=[('HT', 'pad0')])
        S.dve(lambda e: e.memset(HT[:, :, T + 1:T + 2], 0.0), w=[('HT', 'pad1')])

        wp_ctr = [0]

        def wp_load(src_ap, ncols=512):
            slot = wp_ctr[0] % 2
            wp_ctr[0] += 1
            dst = WP[:, slot, :, 0:ncols]
            S.dma('pool', lambda e: e.dma_start(out=dst, in_=src_ap.rearrange("(k p) n -> p k n", p=128)),
                  w=[('WP', slot)])
            return slot

        def mod_thunks():
            MR = HT[:].rearrange("p k t -> p (k t)").bitcast(F32)
            th = []

            def first():
                for b_ in range(NB):
                    S.dma('sp', lambda e, b_=b_: e.dma_start(out=cTf[:, b_, :], in_=din['c'][b_, :].rearrange("(k p) -> p k", p=128),
                                                            allow_slow_non_contiguous=True), w=[('cTf', b_)])
                S.act(lambda e: e.activation(out=condT[:], in_=cTf[:], func=AF.Silu), r=['cTf'], w=['condT'])
            th.append(first)
            for l in range(nlayers):
                th.append(lambda l=l: S.dma('sp', lambda e: e.dma_start(out=MR[0:NB, 0:6144], in_=din['b_mod'][l:l + 1, :].partition_broadcast(NB)),
                                            w=['HT']))
                for j in range(12):
                    def piece(l=l, j=j):
                        slot = wp_load(din['w_mod'][l, :, j * 512:(j + 1) * 512])
                        bank = PS[4 + j % 2]
                        bk = 'P%d' % (4 + j % 2)

                        def mmf(e):
                            ins = None
                            for k in range(8):
                                ins = e.matmul(bank[0:NB, :], lhsT=condT[:, :, k], rhs=WP[:, slot, k, :], start=(k == 0), stop=(k == 7))
                            return ins
                        S.pe(mmf, r=[('WP', slot), 'condT'], w=[bk])
                        S.dve(lambda e: e.tensor_tensor(out=MR[0:NB, j * 512:(j + 1) * 512], in0=bank[0:NB, :],
                                                        in1=MR[0:NB, j * 512:(j + 1) * 512], op=ALU.add),
                              r=[bk, ('HT', ('m', j))], w=[('HT', ('m', j))])
                    th.append(piece)
                th.append(lambda l=l: S.dma('sp', lambda e: e.dma_start(out=modd[l, :, :], in_=MR[0:NB, 0:6144]), r=['HT'], w=[('modd', l)]))

            def last():
                S.fence('HT')
                S.dve(lambda e: e.memset(HT[:, :, 0:1], 0.0), w=['HT'])
                S.dve(lambda e: e.memset(HT[:, :, T + 1:T + 2], 0.0), w=['HT'])
                S.fence('HT')
            th.append(last)
            return th

        def mod_phase():
            S.fence('HT')
            for t_ in mod_thunks():
                t_()
        if stage == 'mod':
            mod_phase()
            dump('modd', modd[0, :, :], [NB, 6 * D], F32, [('modd', 0)])
            S.emit(final_keys=['modd', 'dbg_modd'])
            return nc, dump_d

        fw1 = sb("fw1", [17, 64], F32)
        fw2 = sb("fw2", [64, 64], F32)
        fpar = sb("fpar", [64, 4], F32)
        KF = [KB[:, i, :].bitcast(F32) for i in range(7)]

        def layer_prep(l):
            S.dma('sp', lambda e: e.dma_start(out=n1g[:], in_=din['norm1_g'][l, :].rearrange("(k p) -> p k", p=128),
                                              allow_slow_non_contiguous=True), w=['n1g'])
            for b_ in range(NB):
                S.dma('sp', lambda e, b_=b_: e.dma_start(out=SH1T[:, b_, :], in_=modd[l, b_, 0:1024].rearrange("(k p) -> p k", p=128),
                                                        allow_slow_non_contiguous=True), r=[('modd', l)], w=[('SH1T', b_)])
                S.dma('sp', lambda e, b_=b_: e.dma_start(out=G1T[:, b_, :], in_=modd[l, b_, 1024:2048].rearrange("(k p) -> p k", p=128),
                                                        allow_slow_non_contiguous=True), r=[('modd', l)], w=[('G1T', b_)])
            S.dve(lambda e: e.scalar_tensor_tensor(out=G1T[:], in0=G1T[:], scalar=1.0, in1=n1g[:].unsqueeze(1).to_broadcast([128, NB, 8]),
                                                   op0=ALU.add, op1=ALU.mult), r=['G1T', 'n1g'], w=['G1T'])
            S.dma('sp', lambda e: e.dma_start(out=qg_bc[:], in_=din['q_norm_g'][l:l + 1, :].partition_broadcast(128)), w=['qg_bc'])
            S.dma('sp', lambda e: e.dma_start(out=kg_bc[:], in_=din['k_norm_g'][l:l + 1, :].partition_broadcast(128)), w=['kg_bc'])
            S.dma('sp', lambda e: e.dma_start(out=esink[:], in_=din['attn_sink'][l:l + 1, :].partition_broadcast(128)), w=['esink'])
            S.act(lambda e: e.activation(out=esink[:], in_=esink[:], func=AF.Exp), r=['esink'], w=['esink'])
            S.dma('pool', lambda e: e.dma_start(out=wr_sb[:], in_=din['w_router'][l].rearrange("(k p) n -> p k n", p=128)), w=['wr_sb'])

        def filter_phase(l, hooks=None):
            for i in range(7):
                S.fence('K%d' % i)
            S.fence('FS')
            featsT = FS[0:17, 0:2048]
            a1T = FS[0:64, 2048:4096]
            a2T = FS[0:64, 4096:6144]
            gplus = K(0, 2).rearrange("p (a b) -> p a b", a=16)
            gminus = K(2, 2).rearrange("p (a b) -> p a b", a=16)
            f3w = KF[4][0:64, 0:2048]
            absum = KF[4][:, 2048:3072]
            wint = [KF[4][:, 3072:3584], KF[4][:, 3584:4096]]
            tmpf, tmpb, tab1, tab2 = (KF[5][:, i * 512:(i + 1) * 512] for i in range(4))
            stg = [KF[5][:, 2048 + i * 512:2048 + (i + 1) * 512] for i in range(4)]
            dblk = K(6).rearrange("p (s m k j) -> p s m k j", s=2, m=2, k=16)
            S.dma('sp', lambda e: e.dma_start(out=featsT, in_=din['k_featsT'][:, :]), w=[('FS', 'feats')])
            S.dma('sp', lambda e: e.dma_start(out=fw1[:], in_=din['hy_f1_w'][l, :, :]), w=['fw1'])
            S.dma('sp', lambda e: e.dma_start(out=fw2[:], in_=din['hy_f2_w'][l, :, :]), w=['fw2'])
            for j, nm in enumerate(('hy_f1_b', 'hy_f1_freq', 'hy_f2_b', 'hy_f2_freq')):
                S.dma('sp', lambda e, j=j, nm=nm: e.dma_start(out=fpar[:, j:j + 1], in_=din[nm][l, :].rearrange("(p o) -> p o", o=1)),
                      w=[('fpar', j)])
            S.dma('sp', lambda e: e.dma_start(out=f3w, in_=din['hy_f3_w'][l, :, :]), w=[('K4', 'f3w')])
            S.pool(lambda e: e.memset(absum, 0.0), w=[('K4', 'absum')])

            def sin_mlp(wt, kdim, src, dst, pb, pf, lname):
                for nb in range(4):
                    cs = slice(nb * 512, (nb + 1) * 512)
                    bank = PS[nb % 2]
                    bk = 'P%d' % (nb % 2)
                    S.pe(lambda e, bank=bank, cs=cs: e.matmul(bank[0:64, :], lhsT=wt[0:kdim, :], rhs=src[0:kdim, cs], start=True, stop=True),
                         r=[lname, ('FS', lname + 'src')], w=[bk])
                    S.dve(lambda e, bank=bank, cs=cs: e.tensor_scalar(out=dst[:, cs], in0=bank[0:64, :], scalar1=fpar[:, pb:pb + 1],
                                                                       scalar2=fpar[:, pf:pf + 1], op0=ALU.add, op1=ALU.mult),
                          r=[bk, 'fpar'], w=[('FS', lname + 'dst%d' % nb)])
                    t1 = tmpf[0:64, :]
                    t2 = tmpb[0:64, :]
                    S.act(lambda e, cs=cs: e.activation(out=t1, in_=dst[:, cs], func=AF.Sin, scale=1.0 / 3), r=[('FS', lname + 'dst%d' % nb)], w=[('K5', 't1')])
                    S.act(lambda e: e.activation(out=t2, in_=t1, func=AF.Square), r=[('K5', 't1')], w=[('K5', 't2')])
                    S.dve(lambda e: e.tensor_scalar(out=t2, in0=t2, scalar1=-4.0, scalar2=3.0, op0=ALU.mult, op1=ALU.add), r=[('K5', 't2')], w=[('K5', 't2')])
                    S.dve(lambda e, cs=cs: e.tensor_tensor(out=dst[:, cs], in0=t1, in1=t2, op=ALU.mult), r=[('K5', 't1'), ('K5', 't2')],
                          w=[('FS', lname + 'dst%d' % nb)])
            S.fence('FS')
            sin_mlp(fw1, 17, featsT, a1T, 0, 1, 'fw1')
            S.fence('FS')
            sin_mlp(fw2, 64, a1T, a2T, 2, 3, 'fw2')
            S.fence('FS')
            S.fence('K5')
            if stage == 'f1':
                dump('a2T', FS[0:64, 4096:6144], [64, 2048], F32, ['FS'])
                return
            for tt in range(16):
                wt = wint[tt % 2]
                wk = ('K4', 'win%d' % (tt % 2))
                S.dma('sp', lambda e, wt=wt, tt=tt: e.dma_start(out=wt, in_=din['k_win'][:, tt, :]), w=[wk])
                for cb in range(4):
                    S.pe(lambda e, cb=cb, tt=tt: e.matmul(PS[cb][:, :], lhsT=a2T[:, tt * 128:(tt + 1) * 128], rhs=f3w[:, cb * 512:(cb + 1) * 512],
                                                          start=True, stop=True), r=['FS', ('K4', 'f3w')], w=['P%d' % cb])
                for o in range(2):
                    S.dve(lambda e, o=o, wt=wt: e.tensor_tensor(out=tmpf, in0=PS[o][:, :], in1=wt, op=ALU.mult), r=['P%d' % o, wk], w=[('K5', 'tf')])
                    S.dve(lambda e, o=o, wt=wt: e.tensor_tensor(out=tmpb, in0=PS[2 + o][:, :], in1=wt, op=ALU.mult), r=['P%d' % (2 + o), wk], w=[('K5', 'tb')])
                    if tt == 0:
                        S.dve(lambda e: e.memset(tmpb[0:1, :], 0.0), r=[('K5', 'tb')], w=[('K5', 'tb')])
                    oc = slice(o * 512, (o + 1) * 512)
                    S.dve(lambda e, tt=tt, oc=oc: e.tensor_tensor(out=gplus[:, tt, oc], in0=tmpf, in1=tmpb, op=ALU.add),
                          r=[('K5', 'tf'), ('K5', 'tb')], w=[('K0', tt * 2 + o), ('K1', tt * 2 + o)])
                    S.dve(lambda e, tt=tt, oc=oc: e.tensor_tensor(out=gminus[:, tt, oc], in0=tmpf, in1=tmpb, op=ALU.subtract),
                          r=[('K5', 'tf'), ('K5', 'tb')], w=[('K2', tt * 2 + o), ('K3', tt * 2 + o)])
                    S.act(lambda e: e.activation(out=tab1, in_=tmpf, func=AF.Abs), r=[('K5', 'tf')], w=[('K5', 'a1')])
                    S.act(lambda e: e.activation(out=tab2, in_=tmpb, func=AF.Abs), r=[('K5', 'tb')], w=[('K5', 'a2')])
                    S.pool(lambda e: e.tensor_tensor(out=tab1, in0=tab1, in1=tab2, op=ALU.add), r=[('K5', 'a1'), ('K5', 'a2')], w=[('K5', 'a1')])
                    S.pool(lambda e, oc=oc: e.tensor_tensor(out=absum[:, oc], in0=absum[:, oc], in1=tab1, op=ALU.add),
                           r=[('K5', 'a1'), ('K4', 'absum')], w=[('K4', 'absum')])
            S.fence('FS')
            rn_bc = FS[:, 0:1024]
            hb_bc = FS[:, 1024:2048]
            S.dma('sp', lambda e: e.dma_start(out=hb_bc, in_=din['hy_bias'][l:l + 1, :, :].rearrange("a o c -> a (o c)").partition_broadcast(128)),
                  w=[('FS', 'hb_bc')])
            S.dve(lambda e: e.tensor_scalar(out=hb_bc, in0=hb_bc, scalar1=2.0 / (2 * T), scalar2=None, op0=ALU.mult), r=[('FS', 'hb_bc')], w=[('FS', 'hb_bc')])
            for o in range(2):
                oc = slice(o * 512, (o + 1) * 512)
                S.pe(lambda e, o=o, oc=oc: e.matmul(PS[4 + o][:, :], lhsT=onesf[:], rhs=absum[:, oc], start=True, stop=True),
                     r=[('K4', 'absum'), 'onesf'], w=['P%d' % (4 + o)])
                S.dve(lambda e, o=o, oc=oc: e.reciprocal(out=rn_bc[:, oc], in_=PS[4 + o][:, :]), r=['P%d' % (4 + o)], w=[('FS', ('rn', o))])
                S.dve(lambda e, oc=oc: e.tensor_scalar(out=rn_bc[:, oc], in0=rn_bc[:, oc], scalar1=2.0 / (2 * T), scalar2=None, op0=ALU.mult),
                      r=[('FS', ('rn', o))], w=[('FS', ('rn', o))])
            for i in range(6):
                S.fence('K%d' % i)
            if stage == 'f2':
                dump('rn', FS[:, 0:2048], [128, 2048], F32, ['FS'])
                dump('gplus', K(0, 2), [128, 16384], BF16, ['K0', 'K1'])
                return
            def fl_load(ft):
                sl = ft % 2
                S.dma('sp', lambda e: e.dma_start(out=dblk[:, sl, 0, :, :], in_=din['k_ci'][ft, :, :, :]), w=[('K6', (sl, 0))])
                S.dma('sp', lambda e: e.dma_start(out=dblk[:, sl, 1, :, :], in_=din['k_si'][ft, :, :, :]), w=[('K6', (sl, 1))])
            fl_load(0)
            for ft in range(16):
                sl = ft % 2
                if ft + 1 < 16:
                    fl_load(ft + 1)
                if hooks:
                    for _ in range(2):
                        if hooks:
                            hooks.pop(0)()
                for o in range(2):
                    oc = slice(o * 512, (o + 1) * 512)
                    for m, gsrc, gk, gk2 in ((0, gplus, 'K0', 'K1'), (1, gminus, 'K2', 'K3')):
                        bank = PS[m * 2 + o]

                        def mmf(e, bank=bank, sl=sl, m=m, gsrc=gsrc, oc=oc):
                            ins = None
                            for jc in range(16):
                                ins = e.matmul(bank[:, :], lhsT=dblk[:, sl, m, jc, :], rhs=gsrc[:, jc, oc], start=(jc == 0), stop=(jc == 15))
                            return ins
                        S.pe(mmf, r=[('K6', (sl, m)), gk, gk2], w=['P%d' % (m * 2 + o)])
                        st_ = stg[m * 2 + o]
                        sk = ('K5', 'stg%d' % (m * 2 + o))
                        S.dve(lambda e, bank=bank, st_=st_, oc=oc: e.tensor_tensor(out=st_, in0=bank[:, :], in1=rn_bc[:, oc], op=ALU.mult),
                              r=['P%d' % (m * 2 + o), ('FS', ('rn', o))], w=[sk])
                        if m == 0:
                            S.dve(lambda e, st_=st_, oc=oc: e.tensor_tensor(out=st_, in0=st_, in1=hb_bc[:, oc], op=ALU.add), r=[sk, ('FS', 'hb_bc')], w=[sk])
                        S.dma('sp', lambda e, st_=st_, m=m, o=o, ft=ft: e.dma_start(out=pqd[m, o, ft * 128:(ft + 1) * 128, :], in_=st_),
                              r=[sk], w=[('pqd', (m, o, ft))])
            for i in range(7):
                S.fence('K%d' % i)
            S.fence('FS')

        def mixer(l, b, xin, xin_key, xout, xout_key):
            XIO = [FS[:, 0:1024], FS[:, 1024:2048]]
            TMP = FS[:, 2048:3072]
            w_in = din['w_in'][l]
            TMPs = [FS[:, 2048:3072], FS[:, 3072:4096]]
            def ht_tile(tt):
                p_ = tt % 2
                xt = XIO[p_]
                xk = ('FS', 'xio%d' % p_)
                tmp_ = TMPs[p_]
                tk = ('FS', 'tmp%d' % p_)
                c_ssq = small[:, 4 * p_:4 * p_ + 1]
                c_rstd = small[:, 4 * p_ + 1:4 * p_ + 2]
                sk0 = ('small', ('a0', p_))
                sk1 = ('small', ('a1', p_))
                S.dma('sp', lambda e, xt=xt, tt=tt: e.dma_start(out=xt, in_=xin[b, tt * 128:(tt + 1) * 128, :]), r=[xin_key], w=[xk])
                S.act(lambda e, xt=xt, tmp_=tmp_, c_ssq=c_ssq: e.activation(out=tmp_, in_=xt, func=AF.Square, accum_out=c_ssq), r=[xk], w=[tk, sk0])
                S.act(lambda e, c_ssq=c_ssq, c_rstd=c_rstd: e.activation(out=c_rstd, in_=c_ssq, func=AF.Sqrt, scale=1.0 / D, bias=epsb[:, 0:1]),
                      r=[sk0, 'epsb'], w=[sk1])
                S.dve(lambda e, c_rstd=c_rstd: e.reciprocal(out=c_rstd, in_=c_rstd), r=[sk1], w=[sk1])
                xn = tmp_.bitcast(BF16)[:, 0:1024]
                S.dve(lambda e, xt=xt, xn=xn, c_rstd=c_rstd: e.tensor_scalar(out=xn, in0=xt, scalar1=c_rstd, scalar2=None, op0=ALU.mult),
                      r=[xk, sk1, tk], w=[tk])
                pt, ptk = nextpt()

                def trf(e, xn=xn, pt=pt):
                    ins = None
                    for k in range(8):
                        ins = e.transpose(out=pt[:, k, :], in_=xn[:, k * 128:(k + 1) * 128], identity=idb[:])
                    return ins
                S.pe(trf, r=[tk, 'idb'], w=[ptk])
                for k in range(8):
                    if tt % 2 == 0:
                        S.act(lambda e, k=k, tt=tt, pt=pt: e.activation(out=HT[:, k, 1 + tt * 128:1 + (tt + 1) * 128], in_=pt[:, k, :], func=AF.Identity,
                                                                        scale=G1T[:, b, k:k + 1], bias=SH1T[:, b, k:k + 1]),
                              r=[ptk, 'G1T', 'SH1T'], w=[('HT', (tt, k))])
                    else:
                        S.dve(lambda e, k=k, tt=tt, pt=pt: e.tensor_scalar(out=HT[:, k, 1 + tt * 128:1 + (tt + 1) * 128], in0=pt[:, k, :],
                                                                           scalar1=G1T[:, b, k:k + 1], scalar2=SH1T[:, b, k:k + 1], op0=ALU.mult, op1=ALU.add),
                              r=[ptk, 'G1T', 'SH1T'], w=[('HT', (tt, k))])
            S.interleave([lambda tt=tt: ht_tile(tt) for tt in range(16)], 2, 'ht')
            S.fence('HT')
            S.fence('FS')
            if stage == 'ht':
                return

            hy_tm = [K(0).rearrange("p (a c) -> p a c", a=16), K(1).rearrange("p (a c) -> p a c", a=16), K(2).rearrange("p (a c) -> p a c", a=16)]
            zTs = [K(3)[:, 0:T + 2], K(3)[:, 4096:4096 + T + 2]]
            cTs = [K(4)[:, 0:T], K(4)[:, 4096:4096 + T]]
            cwT = FS[:, 3072:3108].rearrange("p (j s) -> p j s", j=3)
            cbT = FS[:, 3112:3124]
            for i in (3, 4):
                S.fence('K%d' % i)
            S.dma('sp', lambda e: e.dma_start(out=cwT, in_=din['hy_conv_w'][l, :, :].rearrange("j (s p) -> p j s", p=128), allow_slow_non_contiguous=True),
                  w=[('FS', 'cwT')])
            S.dma('sp', lambda e: e.dma_start(out=cbT, in_=din['hy_conv_b'][l, :].rearrange("(s p) -> p s", p=128), allow_slow_non_contiguous=True),
                  w=[('FS', 'cbT')])
            for p_ in range(2):
                S.dve(lambda e, p_=p_: e.memset(zTs[p_][:, 0:1], 0.0), w=[('K3', ('pad0', p_))])
                S.dve(lambda e, p_=p_: e.memset(zTs[p_][:, T + 1:T + 2], 0.0), w=[('K3', ('pad1', p_))])
            slots = [wp_load(w_in[:, seg * 512:(seg + 1) * 512]) if seg < 2 else None for seg in range(3)]

            def hy_chain(sc):
                seg, cc = sc // 4, sc % 4
                p_ = sc % 2
                zT = zTs[p_]
                cT = cTs[p_]
                zk = ('K3', ('z', p_))
                ck = ('K4', ('c', p_))
                slot = slots[seg]
                for tb in range(4):
                    bank = PS[2 * p_ + tb % 2]
                    bk = 'P%d' % (2 * p_ + tb % 2)

                    def mmf(e, bank=bank, tb=tb):
                        ins = None
                        for k in range(8):
                            ins = e.matmul(bank[:, :], lhsT=WP[:, slot, k, cc * 128:(cc + 1) * 128], rhs=HT[:, k, 1 + tb * 512:1 + (tb + 1) * 512],
                                           start=(k == 0), stop=(k == 7))
                        return ins
                    S.pe(mmf, r=[('WP', slot), 'HT'], w=[bk])
                    S.act(lambda e, bank=bank, tb=tb: e.activation(out=zT[:, 1 + tb * 512:1 + (tb + 1) * 512], in_=bank[:, :], func=AF.Copy),
                          r=[bk], w=[('K3', ('z', p_, tb))])
                zr = [('K3', ('z', p_, tb)) for tb in range(4)] + [('K3', ('pad0', p_)), ('K3', ('pad1', p_))]
                S.dve(lambda e: e.tensor_scalar(out=cT, in0=zT[:, 1:T + 1], scalar1=cwT[:, 1, sc:sc + 1], scalar2=cbT[:, sc:sc + 1], op0=ALU.mult, op1=ALU.add),
                      r=zr + [('FS', 'cwT'), ('FS', 'cbT')], w=[ck])
                S.dve(lambda e: e.scalar_tensor_tensor(out=cT, in0=zT[:, 0:T], scalar=cwT[:, 0, sc:sc + 1], in1=cT, op0=ALU.mult, op1=ALU.add),
                      r=zr + [('FS', 'cwT'), ck], w=[ck])
                S.dve(lambda e: e.scalar_tensor_tensor(out=cT, in0=zT[:, 2:T + 2], scalar=cwT[:, 2, sc:sc + 1], in1=cT, op0=ALU.mult, op1=ALU.add),
                      r=zr + [('FS', 'cwT'), ck], w=[ck])
                for hb in range(2):
                    pt, ptk = PTs[p_], 'PT%d' % p_

                    def trf(e, pt=pt, hb=hb):
                        ins = None
                        for j in range(8):
                            tt = hb * 8 + j
                            ins = e.transpose(out=pt[:, j, :], in_=cT[:, tt * 128:(tt + 1) * 128], identity=idb[:])
                        return ins
                    S.pe(trf, r=[ck, 'idb'], w=[ptk])
                    dst = hy_tm[seg][:, hb * 8:(hb + 1) * 8, cc * 128:(cc + 1) * 128]
                    if hb == 0:
                        S.act(lambda e, pt=pt, dst=dst: e.activation(out=dst, in_=pt[:, :, :], func=AF.Copy), r=[ptk], w=[('K%d' % seg, (cc, hb))])
                    else:
                        S.dve(lambda e, pt=pt, dst=dst: e.tensor_copy(out=dst, in_=pt[:, :, :]), r=[ptk], w=[('K%d' % seg, (cc, hb))])
            S.interleave([lambda sc=sc: hy_chain(sc) for sc in range(8)], 2, 'hy')
            slots[2] = wp_load(w_in[:, 1024:1536])
            S.interleave([lambda sc=sc: hy_chain(sc) for sc in range(8, 12)], 2, 'hy')
            for i in range(3):
                S.fence('K%d' % i)
            for i in (3, 4, 5, 6):
                S.fence('K%d' % i)
            S.fence('FS')
            if stage == 'hyproj':
                dump('v', K(0), [128, 8192], BF16, ['K0'])
                dump('x1', K(1), [128, 8192], BF16, ['K1'])
                dump('x2', K(2), [128, 8192], BF16, ['K2'])
                return

            Wr = K(3).rearrange("p (a c) -> p a c", a=16)
            Wi = K(4).rearrange("p (a c) -> p a c", a=16)
            dblk = K(5).rearrange("p (s m k j) -> p s m k j", s=2, m=2, k=16)
            pqs = FS[:, 3072:5120].rearrange("p (s m c) -> p s m c", s=2, m=2)
            t1 = FS[:, 0:512]
            t2 = FS[:, 512:1024]
            blk_ctr = [0]

            def load_blk(i):
                sl = blk_ctr[0] % 2
                blk_ctr[0] += 1
                S.dma('sp', lambda e: e.dma_start(out=dblk[:, sl, 0, :, :], in_=din['k_c2'][i, :, :, :]), w=[('K5', (sl, 0))])
                S.dma('sp', lambda e: e.dma_start(out=dblk[:, sl, 1, :, :], in_=din['k_s2'][i, :, :, :]), w=[('K5', (sl, 1))])
                return sl
            for s_ in range(2):
                u = hy_tm[0]
                gsrc = hy_tm[1 + s_]
                for ft in range(16):
                    sl = load_blk(ft)
                    ps_ = ft % 2
                    for m in range(2):
                        S.dma('sp', lambda e, m=m, ft=ft, ps_=ps_, s_=s_: e.dma_start(out=pqs[:, ps_, m, :], in_=pqd[m, s_, ft * 128:(ft + 1) * 128, :]),
                              r=[('pqd', (m, s_, ft))], w=[('FS', ('pq', ps_, m))])
                    bA = PS[(ft % 2) * 2]
                    bB = PS[(ft % 2) * 2 + 1]
                    kA = 'P%d' % ((ft % 2) * 2)
                    kB = 'P%d' % ((ft % 2) * 2 + 1)
                    for m, bank, bk in ((0, bA, kA), (1, bB, kB)):
                        def mmf(e, bank=bank, sl=sl, m=m):
                            ins = None
                            for tc in range(16):
                                ins = e.matmul(bank[:, :], lhsT=dblk[:, sl, m, tc, :], rhs=u[:, tc, :], start=(tc == 0), stop=(tc == 15))
                            return ins
                        S.pe(mmf, r=[('K5', (sl, m)), 'K0'], w=[bk])
                    Pt = pqs[:, ps_, 0, :]
                    Qt = pqs[:, ps_, 1, :]
                    pk = ('FS', ('pq', ps_, 0))
                    qk = ('FS', ('pq', ps_, 1))
                    S.dve(lambda e, bA=bA, Pt=Pt: e.tensor_tensor(out=t1, in0=bA[:, :], in1=Pt, op=ALU.mult), r=[kA, pk], w=[('FS', 't1')])
                    S.dve(lambda e, bB=bB, Qt=Qt: e.tensor_tensor(out=t2, in0=bB[:, :], in1=Qt, op=ALU.mult), r=[kB, qk], w=[('FS', 't2')])
                    S.pool(lambda e, ft=ft: e.tensor_tensor(out=Wr[:, ft, :], in0=t1, in1=t2, op=ALU.add), r=[('FS', 't1'), ('FS', 't2')], w=[('K3', ft)])
                    t3 = FS[:, 1024:1536]
                    t4 = FS[:, 1536:2048]
                    S.dve(lambda e, bB=bB, Pt=Pt, t3=t3: e.tensor_tensor(out=t3, in0=bB[:, :], in1=Pt, op=ALU.mult), r=[kB, pk], w=[('FS', 't3')])
                    S.dve(lambda e, bA=bA, Qt=Qt, t4=t4: e.tensor_tensor(out=t4, in0=bA[:, :], in1=Qt, op=ALU.mult), r=[kA, qk], w=[('FS', 't4')])
                    S.pool(lambda e, ft=ft, t3=t3, t4=t4: e.tensor_tensor(out=Wi[:, ft, :], in0=t3, in1=t4, op=ALU.subtract),
                           r=[('FS', 't3'), ('FS', 't4')], w=[('K4', ft)])
                S.fence('K3')
                S.fence('K4')
                S.fence('K0')
                for tt in range(16):
                    sl = load_blk(tt)
                    bank = PS[4 + tt % 2]
                    bk = 'P%d' % (4 + tt % 2)

                    def mmf(e, bank=bank, sl=sl):
                        ins = None
                        for fc in range(16):
                            e.matmul(bank[:, :], lhsT=dblk[:, sl, 0, fc, :], rhs=Wr[:, fc, :], start=(fc == 0), stop=False)
                            ins = e.matmul(bank[:, :], lhsT=dblk[:, sl, 1, fc, :], rhs=Wi[:, fc, :], start=False, stop=(fc == 15))
                        return ins
                    S.pe(mmf, r=[('K5', (sl, 0)), ('K5', (sl, 1)), 'K3', 'K4'], w=[bk])
                    S.dve(lambda e, bank=bank, tt=tt, gsrc=gsrc: e.tensor_tensor(out=u[:, tt, :], in0=bank[:, :], in1=gsrc[:, tt, :], op=ALU.mult),
                          r=[bk, ('K%d' % (1 + s_), tt)], w=[('K0', tt)])
                S.fence('K0')
            y_hyT = K(3).rearrange("p (a t) -> p a t", a=4)
            S.fence('K3')
            for tt in range(16):
                pt, ptk = nextpt()

                def trf(e, tt=tt, pt=pt):
                    ins = None
                    for cc in range(4):
                        ins = e.transpose(out=pt[:, cc, :], in_=hy_tm[0][:, tt, cc * 128:(cc + 1) * 128], identity=idb[:])
                    return ins
                S.pe(trf, r=['K0', 'idb'], w=[ptk])
                S.act(lambda e, tt=tt, pt=pt: e.activation(out=y_hyT[:, :, tt * 128:(tt + 1) * 128], in_=pt[:, 0:4, :], func=AF.Copy),
                      r=[ptk], w=[('K3', tt)])
            S.fence('K3')
            S.fence('FS')
            if stage == 'hyena':
                dump('yhy', K(3), [128, 8192], BF16, ['K3'])
                return

            uT = K(0).rearrange("p (a t) -> p a t", a=4)
            vln = K(1).rearrange("p (a c) -> p a c", a=16)
            y_gmT = K(4).rearrange("p (a t) -> p a t", a=4)
            wsT = K(2).rearrange("p (g q) -> p g q", g=64)[:, 0:4, :]
            lng_bc = FS[:, 3072:3584]
            lnb_bc = FS[:, 3584:4096]
            gmb_bc = FS[:, 4096:4608]
            gt = [FS[:, 512 + i * 512:1024 + i * 512] for i in range(4)]
            wsf = FS[:, 0:512].rearrange("p (g q) -> p g q", g=4)
            for i in (0, 1, 2, 4):
                S.fence('K%d' % i)
            S.dma('sp', lambda e: e.dma_start(out=lng_bc, in_=din['gm_ln_g'][l:l + 1, :].partition_broadcast(128)), w=[('FS', 'lng')])
            S.dma('sp', lambda e: e.dma_start(out=lnb_bc, in_=din['gm_ln_b'][l:l + 1, :].partition_broadcast(128)), w=[('FS', 'lnb')])
            S.dma('sp', lambda e: e.dma_start(out=gmb_bc, in_=din['gm_b'][l:l + 1, :, :].rearrange("a g p -> a (g p)").partition_broadcast(128)),
                  w=[('FS', 'gmb')])
            S.dma('sp', lambda e: e.dma_start(out=wsf, in_=din['gm_ws'][l].rearrange("g p q -> p g q")), w=[('FS', 'wsf')])
            for g in range(4):
                S.pe(lambda e, g=g: e.transpose(out=PS[5][:, g * 128:(g + 1) * 128], in_=wsf[:, g, :], identity=idf[:]), r=[('FS', 'wsf'), 'idf'], w=['P5'])
            S.act(lambda e: e.activation(out=wsT, in_=PS[5][:, :].rearrange("p (g q) -> p g q", g=4), func=AF.Copy), r=['P5'], w=[('K2', 'wsT')])
            slot = wp_load(w_in[:, COL_GU:COL_GU + 512])
            for cc in range(4):
                for tb in range(4):
                    bank = PS[(cc * 4 + tb) % 4]
                    bk = 'P%d' % ((cc * 4 + tb) % 4)

                    def mmf(e, bank=bank, cc=cc, tb=tb, slot=slot):
                        ins = None
                        for k in range(8):
                            ins = e.matmul(bank[:, :], lhsT=WP[:, slot, k, cc * 128:(cc + 1) * 128], rhs=HT[:, k, 1 + tb * 512:1 + (tb + 1) * 512],
                                           start=(k == 0), stop=(k == 7))
                        return ins
                    S.pe(mmf, r=[('WP', slot), 'HT'], w=[bk])
                    S.act(lambda e, bank=bank, cc=cc, tb=tb: e.activation(out=uT[:, cc, tb * 512:(tb + 1) * 512], in_=bank[:, :], func=AF.Gelu),
                          r=[bk], w=[('K0', (cc, tb))])
            slot = wp_load(w_in[:, COL_GV:COL_GV + 512])
            def gv_tile(tt):
                bank = PS[tt % 4]
                bk = 'P%d' % (tt % 4)
                g_ = gt[tt % 4]
                gk = ('FS', 'g%d' % (tt % 4))

                def mmf(e, bank=bank, tt=tt, slot=slot):
                    ins = None
                    for k in range(8):
                        ins = e.matmul(bank[:, :], lhsT=HT[:, k, 1 + tt * 128:1 + (tt + 1) * 128], rhs=WP[:, slot, k, :], start=(k == 0), stop=(k == 7))
                    return ins
                S.pe(mmf, r=[('WP', slot), 'HT'], w=[bk])
                S.act(lambda e, bank=bank, g_=g_: e.activation(out=g_, in_=bank[:, :], func=AF.Gelu), r=[bk], w=[gk])
                p_ = tt % 4
                bn6 = small[:, 64 + 8 * p_:64 + 8 * p_ + 6]
                mv_ = small[:, 128 + 2 * p_:128 + 2 * p_ + 2]
                bnk = ('small', ('bn', p_))
                mvk = ('small', ('mv', p_))
                S.dve(lambda e, g_=g_, bn6=bn6: e.bn_stats(out=bn6, in_=g_), r=[gk], w=[bnk])
                S.dve(lambda e, bn6=bn6, mv_=mv_: e.bn_aggr(out=mv_, in_=bn6), r=[bnk], w=[mvk])
                S.act(lambda e, mv_=mv_: e.activation(out=mv_[:, 1:2], in_=mv_[:, 1:2], func=AF.Sqrt, bias=epsb[:, 0:1]), r=[mvk, 'epsb'], w=[mvk])
                S.dve(lambda e, mv_=mv_: e.reciprocal(out=mv_[:, 1:2], in_=mv_[:, 1:2]), r=[mvk], w=[mvk])
                S.dve(lambda e, g_=g_, mv_=mv_: e.scalar_tensor_tensor(out=g_, in0=g_, scalar=mv_[:, 0:1], in1=lng_bc, op0=ALU.subtract, op1=ALU.mult),
                      r=[gk, mvk, ('FS', 'lng')], w=[gk])
                S.dve(lambda e, g_=g_, mv_=mv_, tt=tt: e.scalar_tensor_tensor(out=vln[:, tt, :], in0=g_, scalar=mv_[:, 1:2], in1=lnb_bc, op0=ALU.mult, op1=ALU.add),
                      r=[gk, mvk, ('FS', 'lnb')], w=[('K1', tt)])
            S.interleave([lambda tt=tt: gv_tile(tt) for tt in range(16)], 4, 'gv')

            def sp_tile(n):
                bank = PS[4 + n % 2]
                bk = 'P%d' % (4 + n % 2)

                def mmf(e, bank=bank, n=n):
                    ins = None
                    for g in range(4):
                        ins = e.matmul(bank[:, g * 128:(g + 1) * 128], lhsT=vln[:, n, g * 128:(g + 1) * 128], rhs=wsT[:, g, :], start=True, stop=True)
                    return ins
                S.pe(mmf, r=[('K1', n), ('K2', 'wsT')], w=[bk])
                g_ = gt[n % 4]
                gk = ('FS', 'g%d' % (n % 4))
                S.dve(lambda e, bank=bank, g_=g_: e.tensor_tensor(out=g_, in0=bank[:, :], in1=gmb_bc, op=ALU.add), r=[bk, ('FS', 'gmb')], w=[gk])
                S.dve(lambda e, g_=g_, n=n: e.tensor_tensor(out=y_gmT[:, :, n * 128:(n + 1) * 128], in0=g_.rearrange("p (g q) -> p g q", g=4),
                                                           in1=uT[:, :, n * 128:(n + 1) * 128], op=ALU.mult), r=[gk, 'K0'], w=[('K4', n)])
            S.interleave([lambda n=n: sp_tile(n) for n in range(16)], 2, 'sp')
            for i in (0, 1, 2, 4):
                S.fence('K%d' % i)
            S.fence('FS')
            if stage == 'gmlp':
                dump('ygm', K(4), [128, 8192], BF16, ['K4'])
                return

            QT = K(0).rearrange("p (j t) -> p j t", j=4)
            KT = K(1)[:, 0:2048]
            Vat = K(1)[:, 2048:4096].rearrange("p (a c) -> p a c", a=16)
            y_atT = K(5, 2).rearrange("p (h t) -> p h t", h=8)
            for i in (0, 1, 5, 6):
                S.fence('K%d' % i)
            slotq = wp_ctr[0] % 2
            wp_ctr[0] += 1
            for j in range(4):
                for a_ in range(2):
                    hc = COL_Q + (a_ * 4 + j) * 64
                    S.dma('pool', lambda e, j=j, a_=a_, hc=hc: e.dma_start(out=WP[:, slotq, :, j * 128 + a_ * 64:j * 128 + (a_ + 1) * 64],
                                                                          in_=w_in[:, hc:hc + 64].rearrange("(k p) d -> p k d", p=128)),
                          w=[('WP', slotq)])
            slotk = wp_load(w_in[:, COL_K:COL_K + 256], 256)

            def qk_norm_rope(bank, bk, nh, gbc, tt, p_):
                w_ = nh * 64
                h_ = nh * 32
                o_ = p_ * 2304 if p_ < 2 else 4608 + (p_ - 2) * 576
                sq = FS[:, o_:o_ + w_]
                qn = FS[:, o_ + w_:o_ + 2 * w_]
                qr = FS[:, o_ + 2 * w_:o_ + 2 * w_ + h_].bitcast(BF16)
                tA, tB, tC, tD = (FS[:, o_ + 2 * w_ + h_ + i * h_:o_ + 2 * w_ + h_ + (i + 1) * h_] for i in range(4))
                st_ = small[:, 96 + 8 * p_:96 + 8 * p_ + nh]
                kq = lambda n_: ('FS', (n_, p_))
                sk = ('small', ('qs', p_))
                S.act(lambda e: e.activation(out=sq[:, 0:w_], in_=bank[:, 0:w_], func=AF.Square), r=[bk], w=[kq('sq')])
                S.dve(lambda e: e.tensor_reduce(out=st_, in_=sq[:, 0:w_].rearrange("p (h d) -> p h d", h=nh), axis=AX.X, op=ALU.add),
                      r=[kq('sq')], w=[sk])
                S.act(lambda e: e.activation(out=st_, in_=st_, func=AF.Sqrt, scale=1.0 / 64, bias=epsb[:, 0:1]), r=[sk, 'epsb'], w=[sk])
                S.dve(lambda e: e.reciprocal(out=st_, in_=st_), r=[sk], w=[sk])
                q3 = qn[:, 0:w_].rearrange("p (h d) -> p h d", h=nh)
                S.dve(lambda e: e.tensor_tensor(out=q3, in0=bank[:, 0:w_].rearrange("p (h d) -> p h d", h=nh),
                                                in1=st_.unsqueeze(2).to_broadcast([128, nh, 64]), op=ALU.mult),
                      r=[bk, sk], w=[kq('qn')])
                S.pool(lambda e: e.tensor_tensor(out=q3, in0=q3, in1=gbc[:].unsqueeze(1).to_broadcast([128, nh, 64]), op=ALU.mult),
                       r=[kq('qn'), 'qg_bc', 'kg_bc'], w=[kq('qn')])
                cb_ = ropec[:, tt, :].unsqueeze(1).to_broadcast([128, nh, 32])
                sb_ = ropes[:, tt, :].unsqueeze(1).to_broadcast([128, nh, 32])
                x1_ = q3[:, :, 0:32]
                x2_ = q3[:, :, 32:64]
                r3 = qr[:, 0:w_].rearrange("p (h d) -> p h d", h=nh)
                v = lambda t_: t_[:, 0:nh * 32].rearrange("p (h d) -> p h d", h=nh)
                S.dve(lambda e: e.tensor_tensor(out=v(tA), in0=x1_, in1=cb_, op=ALU.mult), r=[kq('qn'), 'ropec'], w=[kq('tA')])
                S.pool(lambda e: e.tensor_tensor(out=v(tB), in0=x2_, in1=sb_, op=ALU.mult), r=[kq('qn'), 'ropes'], w=[kq('tB')])
                S.dve(lambda e: e.tensor_tensor(out=v(tC), in0=x2_, in1=cb_, op=ALU.mult), r=[kq('qn'), 'ropec'], w=[kq('tC')])
                S.pool(lambda e: e.tensor_tensor(out=v(tD), in0=x1_, in1=sb_, op=ALU.mult), r=[kq('qn'), 'ropes'], w=[kq('tD')])
                S.dve(lambda e: e.tensor_tensor(out=r3[:, :, 0:32], in0=v(tA), in1=v(tB), op=ALU.subtract), r=[kq('tA'), kq('tB')], w=[kq('qr')])
                S.pool(lambda e: e.tensor_tensor(out=r3[:, :, 32:64], in0=v(tC), in1=v(tD), op=ALU.add), r=[kq('tC'), kq('tD')], w=[kq('qr2')])
                return qr, [kq('qr'), kq('qr2')]

            def q_tile(tt):
                bank = PS[tt % 2]
                bk = 'P%d' % (tt % 2)
                pt = PTs[tt % 2]
                ptn = 'PT%d' % (tt % 2)

                def mmf(e, bank=bank, tt=tt):
                    ins = None
                    for k in range(8):
                        ins = e.matmul(bank[:, :], lhsT=HT[:, k, 1 + tt * 128:1 + (tt + 1) * 128], rhs=WP[:, slotq, k, :], start=(k == 0), stop=(k == 7))
                    return ins
                S.pe(mmf, r=[('WP', slotq), 'HT'], w=[bk])
                qr, qrk = qk_norm_rope(bank, bk, 8, qg_bc, tt, tt % 2)

                def trf(e, qr=qr, pt=pt):
                    ins = None
                    for j in range(4):
                        ins = e.transpose(out=pt[:, j, :], in_=qr[:, j * 128:(j + 1) * 128], identity=idb[:])
                    return ins
                S.pe(trf, r=qrk + ['idb'], w=[ptn])
                S.act(lambda e, tt=tt, pt=pt: e.activation(out=QT[:, :, tt * 128:(tt + 1) * 128], in_=pt[:, 0:4, :], func=AF.Copy),
                      r=[ptn], w=[('K0', tt)])

            def k_tile(tt):
                pt = PTs[(tt + 1) % 2]
                ptn = 'PT%d' % ((tt + 1) % 2)
                bank2 = PS[2 + tt % 2]
                bk2 = 'P%d' % (2 + tt % 2)

                def mmf2(e, bank2=bank2, tt=tt):
                    ins = None
                    for k in range(8):
                        ins = e.matmul(bank2[:, 0:256], lhsT=HT[:, k, 1 + tt * 128:1 + (tt + 1) * 128], rhs=WP[:, slotk, k, 0:256], start=(k == 0), stop=(k == 7))
                    return ins
                S.pe(mmf2, r=[('WP', slotk), 'HT'], w=[bk2])
                S.act(lambda e, bank2=bank2, tt=tt: e.activation(out=Vat[:, tt, :], in_=bank2[:, 128:256], func=AF.Copy), r=[bk2], w=[('K1', ('v', tt))])
                kr, krk = qk_norm_rope(bank2, bk2, 2, kg_bc, tt, 2 + tt % 2)
                S.pe(lambda e, kr=kr, pt=pt: e.transpose(out=pt[:, 4, :], in_=kr[:, 0:128], identity=idb[:]), r=krk + ['idb'], w=[ptn])
                S.act(lambda e, tt=tt, pt=pt: e.activation(out=KT[:, tt * 128:(tt + 1) * 128], in_=pt[:, 4, :], func=AF.Copy),
                      r=[ptn], w=[('K1', ('k', tt))])
            thunks = []
            for tt in range(16):
                thunks.append(lambda tt=tt: q_tile(tt))
                thunks.append(lambda tt=tt: k_tile(tt))
            S.interleave(thunks, 4, 'qk')
            S.fence('K0')
            S.fence('K1')
            S.fence('FS')
            S.fence('PT0')
            S.fence('PT1')
            pbuf = [FS[:, i * 256:(i + 1) * 256].bitcast(BF16) for i in range(6)]
            dens = [FS[0:64, 1536:2048], FS[0:64, 2048:2560]]
            itc = [0, 0]

            def core(g, n):
                if True:
                    it = itc[0]
                    c_ = itc[1]
                    gp = slice(g * 64, (g + 1) * 64)
                    ms = [m for m in (n - 1, n, n + 1) if 0 <= m < 16]
                    bO = PS[2 + 2 * (c_ % 2)]
                    bD = PS[3 + 2 * (c_ % 2)]
                    kO = 'P%d' % (2 + 2 * (c_ % 2))
                    kD = 'P%d' % (3 + 2 * (c_ % 2))
                    den = dens[c_ % 2]
                    dk = ('FS', ('den', c_ % 2))
                    itc[1] += 1
                    for mi, m in enumerate(ms):
                        bS = PS[c_ % 2]
                        kS = 'P%d' % (c_ % 2)
                        pb = pbuf[(c_ % 2) * 3 + mi]
                        pk = ('FS', 'pb%d' % ((c_ % 2) * 3 + mi))
                        it += 1
                        itc[0] = it
                        S.pe(lambda e, bS=bS, m=m, n=n, gp=gp: e.matmul(bS[:, :], lhsT=KT[gp, m * 128:(m + 1) * 128], rhs=QT[gp, :, n * 128:(n + 1) * 128],
                                                                        start=True, stop=True), r=['K0', 'K1'], w=[kS])
                        S.act(lambda e, bS=bS, pb=pb: e.activation(out=pb, in_=bS[:, :], func=AF.Exp, scale=0.125), r=[kS], w=[pk])
                        if m != n:
                            msk = trige if m < n else trile
                            S.pool(lambda e, pb=pb, msk=msk: e.tensor_tensor(out=pb.rearrange("p (j q) -> p j q", j=4), in0=pb.rearrange("p (j q) -> p j q", j=4),
                                                                             in1=msk[:].unsqueeze(1).to_broadcast([128, 4, 128]), op=ALU.mult),
                                   r=[pk, 'trige', 'trile'], w=[pk])

                        def mmf(e, pb=pb, m=m, mi=mi, g=g, last=(mi == len(ms) - 1), bO=bO, bD=bD):
                            e.matmul(bO[0:64, :], lhsT=Vat[:, m, g * 64:(g + 1) * 64], rhs=pb, start=(mi == 0), stop=last)
                            return e.matmul(bD[0:64, :], lhsT=onesb[:, 0:64], rhs=pb, start=(mi == 0), stop=last)
                        S.pe(mmf, r=[pk, 'K1', 'onesb'], w=[kO, kD])
                    S.dve(lambda e, g=g, bD=bD, den=den: e.tensor_tensor(out=den.rearrange("p (j q) -> p j q", j=4),
                                                                         in0=bD[0:64, :].rearrange("p (j q) -> p j q", j=4),
                                                                         in1=esink[0:64, g * 4:(g + 1) * 4].unsqueeze(2).to_broadcast([64, 4, 128]), op=ALU.add),
                          r=[kD, 'esink'], w=[dk])
                    S.act(lambda e, den=den: e.activation(out=den, in_=den, func=AF.Ln), r=[dk], w=[dk])
                    S.act(lambda e, den=den: e.activation(out=den, in_=den, func=AF.Exp, scale=-1.0), r=[dk], w=[dk])
                    S.dve(lambda e, g=g, n=n, bO=bO, den=den: e.tensor_tensor(out=y_atT[0:64, g * 4:(g + 1) * 4, n * 128:(n + 1) * 128],
                                                                             in0=bO[0:64, :].rearrange("p (j q) -> p j q", j=4),
                                                                             in1=den.rearrange("p (j q) -> p j q", j=4), op=ALU.mult),
                          r=[kO, dk], w=[('K5', (g, n)), ('K6', (g, n))])
            S.interleave([lambda g=g, n=n: core(g, n) for g in range(2) for n in range(16)], 2, 'core')
            for i in (0, 1, 5, 6):
                S.fence('K%d' % i)
            S.fence('FS')
            if stage == 'attn':
                dump('yat', K(5, 2), [128, 16384], BF16, ['K5', 'K6'])
                return

            mergedT = K(0, 2).rearrange("p (k t) -> p k t", k=8)
            sgs = [[FS[:, (p_ * 6 + i) * 512:(p_ * 6 + i + 1) * 512] for i in range(3)] for p_ in range(2)]
            accs = [FS[:, (p_ * 6 + 3) * 512:(p_ * 6 + 4) * 512] for p_ in range(2)]
            tqs = [FS[:, (p_ * 6 + 4) * 512:(p_ * 6 + 5) * 512] for p_ in range(2)]
            tq2s = [FS[:, (p_ * 6 + 5) * 512:(p_ * 6 + 6) * 512] for p_ in range(2)]
            yT = [K(3).rearrange("p (a t) -> p a t", a=4), y_atT, K(4).rearrange("p (a t) -> p a t", a=4)]
            ykeys = [['K3'], ['K5', 'K6'], ['K4']]
            wbr = din['w_branch'][l]
            S.fence('WP')
            S.fence('WBA')

            def mg_load(dt):
                slot = dt % 2
                mwf = WP[:, slot, :, :].rearrange("p k c -> p (k c)")
                wg = [mwf[:, i * 1024:(i + 1) * 1024].rearrange("p (k c) -> p k c", k=8) for i in range(3)]
                wb_hy = mwf[:, 3072:3584].rearrange("p (k c) -> p k c", k=4)
                wb_gm = mwf[:, 3584:4096].rearrange("p (k c) -> p k c", k=4)
                wb_at = WBA[0:64, slot, :, :]
                dc = slice(dt * 128, (dt + 1) * 128)
                for i in range(3):
                    c0 = COL_GATE + i * 1024 + dt * 128
                    S.dma('pool', lambda e, i=i, c0=c0, wg=wg: e.dma_start(out=wg[i], in_=w_in[:, c0:c0 + 128].rearrange("(k p) n -> p k n", p=128)),
                          w=[('WP', (slot, i))])
                S.dma('pool', lambda e: e.dma_start(out=wb_hy, in_=wbr[0, :, dc].rearrange("(k p) n -> p k n", p=128)), w=[('WP', (slot, 3))])
                S.dma('pool', lambda e: e.dma_start(out=wb_gm, in_=wbr[2, :, dc].rearrange("(k p) n -> p k n", p=128)), w=[('WP', (slot, 4))])
                S.dma('pool', lambda e: e.dma_start(out=wb_at, in_=wbr[1, :, dc].rearrange("(h d) n -> d h n", d=64)), w=[('WBA', slot)])
                return slot, wg, [wb_hy, wb_at, wb_gm]
            mg_next = mg_load(0)
            for dt in range(8):
                slot, wg, wbs = mg_next
                if dt + 1 < 8:
                    mg_next = mg_load(dt + 1)
                mwk = [('WP', (slot, i)) for i in range(5)]
                def mg_tb(tb, dt=dt, slot=slot, wg=wg, wbs=wbs):
                    tcs = slice(tb * 512, (tb + 1) * 512)
                    p_ = tb % 2
                    sg = sgs[p_]
                    acc = accs[p_]
                    tq = tqs[p_]
                    tq2 = tq2s[p_]
                    fk = lambda n_, p_=p_: ('FS', (n_, p_))
                    for i in range(3):
                        uG = 3 * p_ + (2 * i) % 3
                        uB = 3 * p_ + (2 * i + 1) % 3
                        bG = PS[uG]
                        bB = PS[uB]
                        kG = 'P%d' % uG
                        kB_ = 'P%d' % uB

                        def mmg(e, bG=bG, i=i, tb=tb, wg=wg):
                            ins = None
                            for k in range(8):
                                ins = e.matmul(bG[:, :], lhsT=wg[i][:, k, :], rhs=HT[:, k, 1 + tb * 512:1 + (tb + 1) * 512], start=(k == 0), stop=(k == 7))
                            return ins
                        S.pe(mmg, r=[('WP', (slot, i)), 'HT'], w=[kG])
                        S.act(lambda e, bG=bG, i=i, sg=sg: e.activation(out=sg[i], in_=bG[:, :], func=AF.Sigmoid), r=[kG], w=[fk('sg%d' % i)])

                        def mmb(e, bB=bB, i=i, tcs=tcs, wbs=wbs):
                            ins = None
                            if i == 1:
                                for h in range(8):
                                    ins = e.matmul(bB[:, :], lhsT=wbs[1][:, h, :], rhs=y_atT[0:64, h, tcs], start=(h == 0), stop=(h == 7))
                            else:
                                for cc in range(4):
                                    ins = e.matmul(bB[:, :], lhsT=wbs[i][:, cc, :], rhs=yT[i][:, cc, tcs], start=(cc == 0), stop=(cc == 3))
                            return ins
                        S.pe(mmb, r=mwk[3:5] + [('WBA', slot)] + ykeys[i], w=[kB_])
                        if i == 0:
                            S.dve(lambda e, bB=bB, acc=acc, sg=sg: e.tensor_tensor(out=acc, in0=bB[:, :], in1=sg[0], op=ALU.mult), r=[kB_, fk('sg0')], w=[fk('acc')])
                        elif i == 1:
                            S.dve(lambda e, bB=bB, tq=tq, sg=sg: e.tensor_tensor(out=tq, in0=bB[:, :], in1=sg[1], op=ALU.mult), r=[kB_, fk('sg1')], w=[fk('tq')])
                            S.pool(lambda e, acc=acc, tq=tq: e.tensor_tensor(out=acc, in0=acc, in1=tq, op=ALU.add), r=[fk('acc'), fk('tq')], w=[fk('acc')])
                        else:
                            S.dve(lambda e, bB=bB, tq2=tq2, sg=sg: e.tensor_tensor(out=tq2, in0=bB[:, :], in1=sg[2], op=ALU.mult), r=[kB_, fk('sg2')], w=[fk('tq2')])
                            S.pool(lambda e, dt=dt, tcs=tcs, acc=acc, tq2=tq2: e.tensor_tensor(out=mergedT[:, dt, tcs], in0=acc, in1=tq2, op=ALU.add),
                                   r=[fk('acc'), fk('tq2')], w=[('K0', (dt, tcs.start)), ('K1', (dt, tcs.start))])
                S.interleave([lambda tb=tb, mg_tb=mg_tb: mg_tb(tb) for tb in range(4)], 2, 'mg')
            for i in range(7):
                S.fence('K%d' % i)
            S.fence('FS')
            S.fence('HT')
            S.fence('WP')
            S.fence('WBA')
            if stage == 'merge':
                dump('merged', K(0, 2), [128, 16384], BF16, ['K0', 'K1'])
                return

            wout = K(2).rearrange("p (k c) -> p k c", k=8)
            gt1_bc = FS[:, 3072:4096]
            G2_bc = FS[:, 4096:5120]
            SH2_bc = FS[:, 5120:6144]
            h2b = K(3).rearrange("p (s c) -> p s c", s=8)
            h2T = K(4).rearrange("p (s k t) -> p s k t", s=8, k=8)
            for hf in range(2):
                S.dma('pool', lambda e, hf=hf: e.dma_start(out=wout[:, :, hf * 512:(hf + 1) * 512],
                                                           in_=din['w_out'][l, :, hf * 512:(hf + 1) * 512].rearrange("(k p) n -> p k n", p=128)),
                      w=[('K2', hf)])
            S.dma('sp', lambda e: e.dma_start(out=gt1_bc, in_=modd[l, b:b + 1, 2048:3072].partition_broadcast(128)), r=[('modd', l)], w=[('FS', 'gt1')])
            S.dma('sp', lambda e: e.dma_start(out=G2_bc, in_=modd[l, b:b + 1, 4096:5120].partition_broadcast(128)), r=[('modd', l)], w=[('FS', 'G2')])
            S.dma('sp', lambda e: e.dma_start(out=SH2_bc, in_=modd[l, b:b + 1, 3072:4096].partition_broadcast(128)), r=[('modd', l)], w=[('FS', 'SH2')])
            S.dma('sp', lambda e: e.dma_start(out=TMP, in_=din['norm2_g'][l:l + 1, :].partition_broadcast(128)), w=[('FS', 'tmp')])
            S.dve(lambda e: e.scalar_tensor_tensor(out=G2_bc, in0=G2_bc, scalar=1.0, in1=TMP, op0=ALU.add, op1=ALU.mult),
                  r=[('FS', 'G2'), ('FS', 'tmp')], w=[('FS', 'G2')])
            S.dve(lambda e: e.tensor_tensor(out=wout, in0=wout, in1=gt1_bc.unsqueeze(1).to_broadcast([128, 8, 1024]), op=ALU.mult),
                  r=['K2', ('FS', 'gt1')], w=['K2'])
            TMP5 = [FS[:, 2048:3072], FS[:, 3072:4096]]
            TK5 = [('FS', 'tmp'), ('FS', 'gt1')]

            def w5_tile(tt):
                p_ = tt % 2
                TMP = TMP5[p_]
                tk_ = TK5[p_]
                sb_ = 160 + 16 * p_
                xt = XIO[tt % 2]
                xk = ('FS', 'xio%d' % (tt % 2))
                S.dma('sp', lambda e, xt=xt, tt=tt: e.dma_start(out=xt, in_=xin[b, tt * 128:(tt + 1) * 128, :]), r=[xin_key], w=[xk])
                for hf in range(2):
                    bank = PS[(tt * 2 + hf) % 4]
                    bk = 'P%d' % ((tt * 2 + hf) % 4)

                    def mmf(e, bank=bank, tt=tt, hf=hf):
                        ins = None
                        for k in range(8):
                            ins = e.matmul(bank[:, :], lhsT=mergedT[:, k, tt * 128:(tt + 1) * 128], rhs=wout[:, k, hf * 512:(hf + 1) * 512], start=(k == 0), stop=(k == 7))
                        return ins
                    S.pe(mmf, r=['K0', 'K1', 'K2'], w=[bk])
                    S.dve(lambda e, bank=bank, xt=xt, hf=hf: e.tensor_tensor(out=xt[:, hf * 512:(hf + 1) * 512], in0=bank[:, :], in1=xt[:, hf * 512:(hf + 1) * 512], op=ALU.add),
                          r=[bk, xk], w=[xk])
                S.dma('sp', lambda e, xt=xt, tt=tt: e.dma_start(out=xout[b, tt * 128:(tt + 1) * 128, :], in_=xt), r=[xk], w=[(xout_key, (b, tt))])
                S.act(lambda e, xt=xt: e.activation(out=TMP, in_=xt, func=AF.Square, accum_out=small[:, sb_ + 2:sb_ + 3]), r=[xk], w=[tk_, ('small', (2, p_))])
                S.act(lambda e: e.activation(out=small[:, sb_ + 3:sb_ + 4], in_=small[:, sb_ + 2:sb_ + 3], func=AF.Sqrt, scale=1.0 / D, bias=epsb[:, 0:1]),
                      r=[('small', (2, p_)), 'epsb'], w=[('small', (3, p_))])
                S.dve(lambda e: e.reciprocal(out=small[:, sb_ + 3:sb_ + 4], in_=small[:, sb_ + 3:sb_ + 4]), r=[('small', (3, p_))], w=[('small', (3, p_))])
                S.dve(lambda e, xt=xt: e.scalar_tensor_tensor(out=TMP, in0=xt, scalar=small[:, sb_ + 3:sb_ + 4], in1=G2_bc, op0=ALU.mult, op1=ALU.mult),
                      r=[xk, ('small', (3, p_)), ('FS', 'G2')], w=[tk_])
                hs = tt % 2
                S.pool(lambda e, hs=hs: e.tensor_tensor(out=h2b[:, hs, :], in0=TMP, in1=SH2_bc, op=ALU.add), r=[tk_, ('FS', 'SH2')], w=[('K3', hs)])
                S.dma('sp', lambda e, hs=hs, tt=tt: e.dma_start(out=h2d[b, tt * 128:(tt + 1) * 128, :], in_=h2b[:, hs, :]), r=[('K3', hs)], w=[('h2d', (b, tt))])

                pt, ptk = nextpt()

                def trf(e, hs=hs, pt=pt):
                    ins = None
                    for k in range(8):
                        ins = e.transpose(out=pt[:, k, :], in_=h2b[:, hs, k * 128:(k + 1) * 128], identity=idb[:])
                    return ins
                S.pe(trf, r=[('K3', hs), 'idb'], w=[ptk])
                S.act(lambda e, hs=hs, pt=pt: e.activation(out=h2T[:, hs, :, :], in_=pt[:, :, :], func=AF.Copy), r=[ptk], w=[('K4', hs)])
                bank = PS[4 + tt % 2]
                bk = 'P%d' % (4 + tt % 2)

                def mmr(e, bank=bank, hs=hs):
                    ins = None
                    for k in range(8):
                        ins = e.matmul(bank[:, 0:NE], lhsT=h2T[:, hs, k, :], rhs=wr_sb[:, k, :], start=(k == 0), stop=(k == 7))
                    return ins
                S.pe(mmr, r=[('K4', hs), 'wr_sb'], w=[bk])
                S.dve(lambda e, bank=bank: e.tensor_reduce(out=small[:, sb_ + 4:sb_ + 5], in_=bank[:, 0:NE], axis=AX.X, op=ALU.max), r=[bk], w=[('small', (4, p_))])
                S.dve(lambda e: e.tensor_scalar(out=small[:, sb_ + 5:sb_ + 6], in0=small[:, sb_ + 4:sb_ + 5], scalar1=-1.0, scalar2=None, op0=ALU.mult), r=[('small', (4, p_))], w=[('small', (5, p_))])
                S.act(lambda e, bank=bank: e.activation(out=small[:, 32 + sb_:48 + sb_], in_=bank[:, 0:NE], func=AF.Exp, bias=small[:, sb_ + 5:sb_ + 6], accum_out=small[:, sb_ + 6:sb_ + 7]),
                      r=[bk, ('small', (5, p_))], w=[('small', ('e', p_)), ('small', (6, p_))])
                S.dve(lambda e: e.reciprocal(out=small[:, sb_ + 6:sb_ + 7], in_=small[:, sb_ + 6:sb_ + 7]), r=[('small', (6, p_))], w=[('small', (6, p_))])
                S.dve(lambda e, tt=tt: e.tensor_scalar(out=affall[:, tt, b * NE:(b + 1) * NE], in0=small[:, 32 + sb_:48 + sb_], scalar1=small[:, sb_ + 6:sb_ + 7], scalar2=None, op0=ALU.mult),
                      r=[('small', ('e', p_)), ('small', (6, p_))], w=[('affall', (b, tt))])
            S.interleave([lambda tt=tt: w5_tile(tt) for tt in range(16)], 2, 'w5')
            for i in range(7):
                S.fence('K%d' % i)
            S.fence('FS')


        mx8 = sb("mx8", [32, 8], F32)

        def moe(l, xout, xout_key):
            for i in range(7):
                S.fence('K%d' % i)
            S.fence('FS')
            S.fence('WP')
            WPf = WP[:].rearrange("p a k c -> p (a k c)")
            Rall = WPf[:, 2048:4096].rearrange("p (a b c) -> p a b c", a=16, b=32)
            affT = KF[0][0:32, 0:2048]
            work = KF[0][0:32, 2048:4096]
            mask = KF[1][0:32, 0:2048]
            ones_r = KF[1][0:32, 2048:4096]
            keyr = KF[2][0:32, 0:2048]
            for tb in range(4):
                bank = PS[tb % 2]
                bk = 'P%d' % (tb % 2)

                def trf(e, bank=bank, tb=tb):
                    ins = None
                    for j in range(4):
                        ins = e.transpose(out=bank[0:32, j * 128:(j + 1) * 128], in_=affall[:, tb * 4 + j, :], identity=idf[:])
                    return ins
                S.pe(trf, r=['affall', 'idf'], w=[bk])
                S.act(lambda e, bank=bank, tb=tb: e.activation(out=affT[:, tb * 512:(tb + 1) * 512], in_=bank[0:32, :], func=AF.Copy),
                      r=[bk], w=[('K0', ('affT', tb))])
            S.dve(lambda e: e.tensor_copy(out=work, in_=affT), r=['K0'], w=[('K0', 'work')])
            S.pool(lambda e: e.memset(ones_r, 1.0), w=[('K1', 'ones')])
            for r_ in range(CAP // 8):
                S.dve(lambda e: e.max(out=mx8[:], in_=work), r=[('K0', 'work')], w=['mx8'])
                if r_ < CAP // 8 - 1:
                    S.dve(lambda e: e.match_replace(out=work, in_to_replace=mx8[:], in_values=work, imm_value=-1.0),
                          r=[('K0', 'work'), 'mx8'], w=[('K0', 'work')])
            S.dve(lambda e: e.tensor_scalar(out=mask, in0=affT, scalar1=mx8[:, 7:8], scalar2=None, op0=ALU.is_ge), r=['K0', 'mx8'], w=[('K1', 'mask')])
            S.dve(lambda e: e.tensor_tensor_scan(out=keyr, data0=ones_r, data1=mask, initial=0.0, op0=ALU.mult, op1=ALU.add),
                  r=[('K1', 'mask'), ('K1', 'ones')], w=[('K2', 'key')])
            S.dve(lambda e: e.tensor_tensor(out=keyr, in0=keyr, in1=mask, op=ALU.mult), r=[('K2', 'key'), ('K1', 'mask')], w=[('K2', 'key')])

            def trk(e):
                ins = None
                for tt in range(16):
                    ins = e.transpose(out=PS[2][:, tt * 32:(tt + 1) * 32], in_=keyr[:, tt * 128:(tt + 1) * 128], identity=idf[0:32, 0:32])
                return ins
            S.pe(trk, r=[('K2', 'key'), 'idf'], w=['P2'])
            keyb = WPf[:, 4096:4608].rearrange("p (a b) -> p a b", a=16)
            S.act(lambda e: e.activation(out=keyb.rearrange("p a b -> p (a b)"), in_=PS[2][:, :], func=AF.Copy), r=['P2'], w=[('WP', 'keyb')])
            S.dve(lambda e: e.tensor_copy(out=Rall[:, :, :, 0:2], in_=tokidx[:].unsqueeze(2).to_broadcast([128, 16, 32, 2])), r=['tokidx'], w=[('WP', ('Rall', 0))])
            S.dve(lambda e: e.tensor_copy(out=Rall[:, :, :, 2], in_=affall[:]), r=['affall'], w=[('WP', ('Rall', 2))])
            S.dve(lambda e: e.tensor_tensor(out=Rall[:, :, :, 3], in0=affall[:], in1=Rall[:, :, :, 2], op=ALU.subtract),
                  r=['affall', ('WP', ('Rall', 2))], w=[('WP', ('Rall', 3))])
            S.fence('HT')
            HTf = HT[:].rearrange("p k t -> p (k t)")
            Poh = [HTf[:, i * 4096:(i + 1) * 4096].rearrange("p (t c) -> p t c", t=16) for i in range(2)]
            idxf = small[:, 48:56]
            rkeys = [('WP', ('Rall', 0)), ('WP', ('Rall', 2)), ('WP', ('Rall', 3))]

            def route(e_, b_, part):
                if True:
                    be = b_ * NE + e_
                    po = Poh[b_]
                    if part == 0:
                        S.dve(lambda e, po=po, be=be: e.tensor_tensor(out=po, in0=iota1[:].unsqueeze(1).to_broadcast([128, 16, 256]),
                                                                     in1=keyb[:, :, be].unsqueeze(2).to_broadcast([128, 16, 256]), op=ALU.is_equal),
                              r=['iota1', ('WP', 'keyb')], w=[('HT', ('poh', b_))])
                        return
                    for st in range(2):
                        bank = PS[5]
                        bk = 'P5'

                        def mmf(e, bank=bank, po=po, st=st, be=be):
                            ins = None
                            for tt in range(16):
                                ins = e.matmul(bank[:, 0:4], lhsT=po[:, tt, st * 128:(st + 1) * 128], rhs=Rall[:, tt, be, :], start=(tt == 0), stop=(tt == 15))
                            return ins
                        S.pe(mmf, r=[('HT', ('poh', b_))] + rkeys, w=[bk])
                        col = b_ * 2 + st
                        S.dve(lambda e, bank=bank: e.tensor_copy(out=idxf[:, 0:4], in_=bank[:, 0:4]), r=[bk], w=[('small', 'i0')])
                        S.dve(lambda e: e.scalar_tensor_tensor(out=idxf[:, 4:5], in0=idxf[:, 0:1], scalar=128.0, in1=idxf[:, 1:2], op0=ALU.mult, op1=ALU.add),
                              r=[('small', 'i0')], w=[('small', 'i1')])
                        S.dve(lambda e, col=col, b_=b_: e.tensor_scalar(out=idx_sb[:, e_, col:col + 1], in0=idxf[:, 4:5], scalar1=float(T * b_), scalar2=None,
                                                                        op0=ALU.add),
                              r=[('small', 'i1')], w=[('idx', (e_, col))])
                        S.dve(lambda e, col=col: e.tensor_tensor(out=gate_sb[:, e_, col:col + 1], in0=idxf[:, 2:3], in1=idxf[:, 3:4], op=ALU.add),
                              r=[('small', 'i0')], w=[('gate', (e_, col))])
            for i in (0, 1, 2, 3):
                S.fence('K%d' % i)
            if stage == 'route':
                for e_ in range(NE):
                    for b_ in range(2):
                        route(e_, b_, 0)
                        route(e_, b_, 1)
                return
            gt2_bc = FS[:, 0:2048].rearrange("p (b c) -> p b c", b=2)
            ystg = [FS[:, 2048 + i * 1024:2048 + (i + 1) * 1024] for i in range(4)]
            xg = K(4).rearrange("p (s q c) -> p s q c", s=2, q=4)
            xgT = K(5).rearrange("p (s k c) -> p s k c", s=2, k=8)
            actT = K(6).rearrange("p (f c) -> p f c", f=16)
            sgt = [WP[:].rearrange("p a k c -> p (a k c)").bitcast(F32)[:, i * 512:(i + 1) * 512] for i in range(2)]
            for b_ in range(2):
                S.dma('sp', lambda e, b_=b_: e.dma_start(out=gt2_bc[:, b_, :], in_=modd[l, b_:b_ + 1, 5120:6144].partition_broadcast(128)),
                      r=[('modd', l)], w=[('FS', ('gt2', b_))])
            wg_d = din['w_e_gate'][l]
            wu_d = din['w_e_up'][l]
            wd_d = din['w_e_down'][l]
            pieces = [(e_, q) for e_ in range(NE) for q in range(6)]

            def load_piece(pi):
                e_, q = pieces[pi]
                sl = pi % 4
                if q < 4:
                    dst = K(sl).rearrange("p (m k f) -> p m k f", m=2, k=8)
                    S.dma('pool', lambda e: e.dma_start(out=dst[:, 0, :, :], in_=wg_d[e_, :, q * 512:(q + 1) * 512].rearrange("(k p) f -> p k f", p=128)),
                          w=[('K%d' % sl, 'a')])
                    S.dma('pool', lambda e: e.dma_start(out=dst[:, 1, :, :], in_=wu_d[e_, :, q * 512:(q + 1) * 512].rearrange("(k p) f -> p k f", p=128)),
                          w=[('K%d' % sl, 'b')])
                else:
                    h = q - 4
                    dst = K(sl).rearrange("p (f c) -> p f c", f=16)
                    for hh in range(2):
                        S.dma('pool', lambda e, hh=hh: e.dma_start(out=dst[:, hh * 8:(hh + 1) * 8, :],
                                                                   in_=wd_d[e_, hh * 1024:(hh + 1) * 1024, h * 512:(h + 1) * 512].rearrange("(f p) c -> p f c", p=128)),
                              w=[('K%d' % sl, 'a' if hh == 0 else 'b')])

            def gathers(e_):
                par = e_ % 2
                for bs in range(4):
                    b_ = bs // 2
                    S.dma('pool', lambda e, bs=bs, b_=b_: e.indirect_dma_start(out=xg[:, par, bs, :], out_offset=None, in_=h2d.rearrange("b t d -> (b t) d"),
                                                                               in_offset=bass.IndirectOffsetOnAxis(ap=idx_sb[:, e_, bs:bs + 1], axis=0)),
                          r=['h2d', ('idx', (e_, bs))], w=[('K4', (par, bs))])

            def expert(e_):
                par = e_ % 2
                if e_ + 2 < NE:
                    route(e_ + 2, 0, 0)
                if e_ + 1 < NE:
                    gathers(e_ + 1)
                for bs in range(4):
                    pt, ptk = nextpt()

                    def trf(e, bs=bs, pt=pt):
                        ins = None
                        for k in range(8):
                            ins = e.transpose(out=pt[:, k, :], in_=xg[:, par, bs, k * 128:(k + 1) * 128], identity=idb[:])
                        return ins
                    S.pe(trf, r=[('K4', (par, bs)), 'idb'], w=[ptk])
                    S.act(lambda e, bs=bs, pt=pt: e.activation(out=xgT[:, par, :, bs * 128:(bs + 1) * 128], in_=pt[:, :, :], func=AF.Copy),
                          r=[ptk], w=[('K5', (par, bs))])
                for q in range(6):
                    pi = e_ * 6 + q
                    if pi + 3 < len(pieces):
                        load_piece(pi + 3)
                    if e_ + 2 < NE:
                        if q == 2:
                            route(e_ + 2, 0, 1)
                            route(e_ + 2, 1, 0)
                        elif q == 4:
                            route(e_ + 2, 1, 1)
                    sl = pi % 4
                    wk = 'K%d' % sl
                    if q < 4:
                        wsl = K(sl).rearrange("p (m k f) -> p m k f", m=2, k=8)
                        for fl in range(4):
                            ft = q * 4 + fl
                            bA = PS[(ft % 2) * 2]
                            bU = PS[(ft % 2) * 2 + 1]
                            kA = 'P%d' % ((ft % 2) * 2)
                            kU = 'P%d' % ((ft % 2) * 2 + 1)
                            for m, bank, bk in ((0, bA, kA), (1, bU, kU)):
                                def mmf(e, m=m, bank=bank, fl=fl, wsl=wsl):
                                    ins = None
                                    for k in range(8):
                                        ins = e.matmul(bank[:, :], lhsT=wsl[:, m, k, fl * 128:(fl + 1) * 128], rhs=xgT[:, par, k, :], start=(k == 0), stop=(k == 7))
                                    return ins
                                S.pe(mmf, r=[wk, 'K5'], w=[bk])
                            st_ = sgt[ft % 2]
                            S.act(lambda e, bA=bA, st_=st_: e.activation(out=st_, in_=bA[:, :], func=AF.Silu), r=[kA], w=[('WP', ('sg', ft % 2))])
                            S.dve(lambda e, bU=bU, st_=st_, ft=ft: e.tensor_tensor(out=actT[:, ft, :], in0=bU[:, :], in1=st_, op=ALU.mult),
                                  r=[kU, ('WP', ('sg', ft % 2))], w=[('K6', ft)])
                    else:
                        h = q - 4
                        wsl = K(sl).rearrange("p (f c) -> p f c", f=16)
                        for bs in range(4):
                            b_ = bs // 2
                            bank = PS[4 + bs % 2]
                            bk = 'P%d' % (4 + bs % 2)

                            def mmf(e, bank=bank, bs=bs, wsl=wsl):
                                ins = None
                                for ft in range(16):
                                    ins = e.matmul(bank[:, :], lhsT=actT[:, ft, bs * 128:(bs + 1) * 128], rhs=wsl[:, ft, :], start=(ft == 0), stop=(ft == 15))
                                return ins
                            S.pe(mmf, r=[wk, 'K6'], w=[bk])
                            S.dve(lambda e, bank=bank, bs=bs, b_=b_, h=h: e.scalar_tensor_tensor(out=ystg[bs][:, h * 512:(h + 1) * 512], in0=bank[:, :],
                                                                                                  scalar=gate_sb[:, e_, bs:bs + 1],
                                                                                                  in1=gt2_bc[:, b_, h * 512:(h + 1) * 512],
                                                                                                  op0=ALU.mult, op1=ALU.mult),
                                  r=[bk, ('gate', (e_, bs)), ('FS', ('gt2', b_))], w=[('FS', ('y', bs))])
                for bs in range(4):
                    b_ = bs // 2
                    S.dma('pool', lambda e, bs=bs, b_=b_: e.indirect_dma_start(out=xout.rearrange("b t d -> (b t) d"),
                                                                               out_offset=bass.IndirectOffsetOnAxis(ap=idx_sb[:, e_, bs:bs + 1], axis=0),
                                                                               in_=ystg[bs], in_offset=None, compute_op=ALU.add),
                          r=[('FS', ('y', bs)), ('idx', (e_, bs))], w=[xout_key])

            for pi in range(3):
                load_piece(pi)
            for e_ in range(2):
                for b_ in range(2):
                    route(e_, b_, 0)
                    route(e_, b_, 1)
            gathers(0)
            for e_ in range(NE):
                expert(e_)
            S.fence('HT')
            S.dve(lambda e: e.memset(HT[:, :, 0:1], 0.0), w=['HT'])
            S.dve(lambda e: e.memset(HT[:, :, T + 1:T + 2], 0.0), w=['HT'])
            for i in range(7):
                S.fence('K%d' % i)
            S.fence('FS')
            S.fence('WP')

        for l in range(nlayers):
            xin = din['x'] if l == 0 else xs0
            xout = xs0 if (l == 0 and nlayers > 1) else out_d
            xin_key = 'xin%d' % l
            xout_key = 'xin%d' % (l + 1)
            if l == 0:
                S.fence('HT')
                hooks = mod_thunks()
                filter_phase(0, hooks)
                while hooks:
                    hooks.pop(0)()
                layer_prep(0)
            else:
                layer_prep(l)
                filter_phase(l)
            if stage == 'prep':
                dump('G1T', G1T[:], [128, NB, 8], F32, ['G1T'])
                dump('esink', esink[:], [128, 8], F32, ['esink'])
                break
            if stage in ('f1', 'f2'):
                break
            if stage == 'filter':
                dump('pqd', pqd, [2, 2, T, 512], F32, ['pqd'])
                break
            stop = False
            for b in range(NB):
                mixer(l, b, xin, xin_key, xout, xout_key)
                if stage in ('ht', 'hyproj', 'hyena', 'gmlp', 'attn', 'merge'):
                    stop = True
                    break
            if stop:
                break
            if stage == 'mixer':
                dump('h2d', h2d, [NB, T, D], BF16, ['h2d'])
                dump('aff', affall[:], [128, 16, 32], F32, ['affall'])
                break
            moe(l, xout, xout_key)
            if stage == 'route':
                dump('idx', idx_sb[:], [128, NE, 4], I32, ['idx'])
                dump('gate', gate_sb[:], [128, NE, 4], F32, ['gate'])
                break
        fk = ['xin%d' % nlayers] + list(dump_d.keys())
        if stage is not None:
            fk += ['pqd', 'h2d', 'xin1', 'idx', 'gate', 'HT', 'affall']
        S.emit(final_keys=fk)
    return nc, dump_d


_NC_CACHE = {}


def kernel(**inputs):
    consts = make_consts()
    if 'full' not in _NC_CACHE:
        _NC_CACHE['full'] = build()
    nc, _ = _NC_CACHE['full']
    shared = {k: np.ascontiguousarray(np.asarray(v, dtype=np.float32)) for k, v in inputs.items() if k not in ('x', 'c')}
    shared.update(consts)
    x = np.asarray(inputs['x'], dtype=np.float32)
    c = np.asarray(inputs['c'], dtype=np.float32)
    in_maps = []
    for i in range(8):
        m = dict(shared)
        m['x'] = np.ascontiguousarray(x[2 * i:2 * i + 2])
        m['c'] = np.ascontiguousarray(c[2 * i:2 * i + 2])
        in_maps.append(m)
    res = run_bass_kernel_spmd(nc, in_maps, core_ids=list(range(8)))
    return np.concatenate([r['out'] for r in res.results], axis=0).astype(np.float32)
```
